# Optimizing a Trainium2 kernel written in Bass

```python
import jax
import jax.numpy as jnp
from jax import lax
import numpy as np

D_MODEL = 2048
BATCH = 2
SEQ = 4096
DEPTH = 1

HEAD_DIM = 128
GDN_HEADS = 8
NSA_HEADS = 8
NSA_KV_HEADS = 2
NSA_GROUP = NSA_HEADS // NSA_KV_HEADS
GDN_WIDTH = GDN_HEADS * HEAD_DIM
NSA_WIDTH = NSA_HEADS * HEAD_DIM
NSA_KV_WIDTH = NSA_KV_HEADS * HEAD_DIM
D_MIX = GDN_WIDTH + NSA_WIDTH
CONV_WIDTH = 4
GDN_CHUNK = 64
CMP_BLOCK = 32
CMP_STRIDE = 16
CMP_HIDDEN = 256
SEL_BLOCK = 64
SEL_TOPK = 16
N_LOCAL_BLOCKS = 2
WINDOW = 512
Q_BLOCK = 128
N_EXPERTS = 64
TOP_K = 8
EXPERT_FF = 512
SHARED_FF = 512
ROUTED_SCALE = 2.5
N_MOD = 6
EPS = 1e-6
IN_SPLIT_SIZES = (3 * GDN_WIDTH, GDN_HEADS, GDN_HEADS, GDN_WIDTH, NSA_WIDTH, 6 * NSA_KV_WIDTH, 3 * NSA_HEADS)
IN_COLS = 4 * GDN_WIDTH + 2 * GDN_HEADS + NSA_WIDTH + 6 * NSA_KV_WIDTH + 3 * NSA_HEADS

kernel_name = "hybrid_gdn_nsa_moe_block"


def rms_norm(x, w):
    xf = x.astype(jnp.float32)
    y = xf * lax.rsqrt(jnp.mean(xf * xf, axis=-1, keepdims=True) + EPS)
    return (y * w.astype(jnp.float32)).astype(x.dtype)


def l2_normalize(x):
    return x * lax.rsqrt(jnp.sum(x * x, axis=-1, keepdims=True) + EPS)


def masked_softmax(s, valid):
    s = jnp.where(valid, s.astype(jnp.float32), -jnp.inf)
    m = jnp.max(s, axis=-1, keepdims=True)
    m = jnp.where(jnp.isfinite(m), m, 0.0)
    e = jnp.exp(s - m)
    return e / jnp.maximum(jnp.sum(e, axis=-1, keepdims=True), 1e-30)


def causal_depthwise_conv(x, w):
    t = x.shape[1]
    xp = jnp.pad(x, ((0, 0), (CONV_WIDTH - 1, 0), (0, 0)))
    return sum(xp[:, i:i + t] * w[i] for i in range(CONV_WIDTH))


def gated_delta_rule(q, k, v, g, beta):
    b, t, h, dk = q.shape
    dv = v.shape[-1]
    c = GDN_CHUNK
    n = t // c

    def to_chunks(a):
        return a.reshape(b, n, c, h, -1).transpose(0, 3, 1, 2, 4)

    q = to_chunks(q) * dk ** -0.5
    k = to_chunks(k)
    v = to_chunks(v)
    beta = beta.reshape(b, n, c, h).transpose(0, 3, 1, 2)
    g_cum = jnp.cumsum(g.reshape(b, n, c, h).transpose(0, 3, 1, 2), axis=-1)
    causal = jnp.tril(jnp.ones((c, c), dtype=bool))
    strict = jnp.tril(jnp.ones((c, c), dtype=bool), -1)
    decay = jnp.exp(jnp.where(causal, g_cum[..., :, None] - g_cum[..., None, :], -jnp.inf))
    k_beta = k * beta[..., None]
    a_strict = jnp.where(strict, jnp.einsum('bhncd,bhnsd->bhncs', k_beta, k) * decay, 0.0)
    lhs = a_strict + jnp.eye(c, dtype=q.dtype)
    rhs = jnp.concatenate([v * beta[..., None], k_beta * jnp.exp(g_cum)[..., None]], axis=-1)
    sol = lax.linalg.triangular_solve(lhs, rhs, left_side=True, lower=True, unit_diagonal=True)
    u, w = sol[..., :dv], sol[..., dv:]
    qk = jnp.einsum('bhncd,bhnsd->bhncs', q, k) * decay
    q_dec = q * jnp.exp(g_cum)[..., None]
    g_last = g_cum[..., -1]
    k_dec = k * jnp.exp(g_last[..., None] - g_cum)[..., None]

    def chunk_step(state, xs):
        q_c, qk_c, u_c, w_c, k_c, gl = xs
        v_new = u_c - jnp.einsum('bhcd,bhde->bhce', w_c, state)
        o_c = jnp.einsum('bhcd,bhde->bhce', q_c, state) + jnp.einsum('bhcs,bhse->bhce', qk_c, v_new)
        state = state * jnp.exp(gl)[..., None, None] + jnp.einsum('bhcd,bhce->bhde', k_c, v_new)
        return state, o_c

    xs = tuple(jnp.moveaxis(a, 2, 0) for a in (q_dec, qk, u, w, k_dec, g_last))
    state0 = jnp.zeros((b, h, dk, dv), jnp.float32)
    _, o = lax.scan(chunk_step, state0, xs)
    return o.transpose(1, 0, 3, 2, 4).reshape(b, t, h, dv)


def gated_deltanet(qkv, beta_logit, a_logit, gate, conv_w, a_log, dt_bias, norm_w):
    b, t, _ = qkv.shape
    dtype = qkv.dtype
    f32 = jnp.float32
    qkv = jax.nn.silu(causal_depthwise_conv(qkv.astype(f32), conv_w.astype(f32)))
    q, k, v = jnp.split(qkv, 3, axis=-1)
    q = l2_normalize(q.reshape(b, t, GDN_HEADS, HEAD_DIM))
    k = l2_normalize(k.reshape(b, t, GDN_HEADS, HEAD_DIM))
    v = v.reshape(b, t, GDN_HEADS, HEAD_DIM)
    beta = jax.nn.sigmoid(beta_logit.astype(f32))
    g = -jnp.exp(a_log.astype(f32)) * jax.nn.softplus(a_logit.astype(f32) + dt_bias.astype(f32))
    o = gated_delta_rule(q, k, v, g, beta)
    o = rms_norm(o, norm_w) * jax.nn.silu(gate.astype(f32).reshape(b, t, GDN_HEADS, HEAD_DIM))
    return o.reshape(b, t, GDN_WIDTH).astype(dtype)


def compress_blocks(x, pe, w1, w2):
    b, t, hk, d = x.shape
    nc = (t - CMP_BLOCK) // CMP_STRIDE + 1
    idx = np.arange(nc)[:, None] * CMP_STRIDE + np.arange(CMP_BLOCK)[None, :]
    blocks = x[:, idx] + pe[:, None, :]
    flat = blocks.transpose(0, 1, 3, 2, 4).reshape(b, nc, hk, CMP_BLOCK * d)
    return jax.nn.silu(flat @ w1) @ w2


def native_sparse_attention(q, k_cmp, v_cmp, k_slc, v_slc, k_win, v_win, gate_logits,
                            pe_k, w1_k, w2_k, pe_v, w1_v, w2_v):
    b, t, _ = q.shape
    dtype = q.dtype
    f32 = jnp.float32
    scale = HEAD_DIM ** -0.5
    qg = q.reshape(b, t, NSA_KV_HEADS, NSA_GROUP, HEAD_DIM)
    k_cmp, v_cmp, k_slc, v_slc, k_win, v_win = [
        a.reshape(b, t, NSA_KV_HEADS, HEAD_DIM) for a in (k_cmp, v_cmp, k_slc, v_slc, k_win, v_win)]
    pos = jnp.arange(t)

    kc = compress_blocks(k_cmp, pe_k, w1_k, w2_k)
    vc = compress_blocks(v_cmp, pe_v, w1_v, w2_v)
    nc = kc.shape[1]
    c_end = np.arange(nc) * CMP_STRIDE + CMP_BLOCK - 1
    s_cmp = jnp.einsum('btkgd,bckd->bkgtc', qg, kc) * scale
    p_cmp = masked_softmax(s_cmp, c_end[None, :] <= pos[:, None])
    o_cmp = jnp.einsum('bkgtc,bckd->btkgd', p_cmp, vc.astype(f32)).reshape(b, t, NSA_HEADS, HEAD_DIM)

    nsel = t // SEL_BLOCK
    n_top = min(SEL_TOPK, nsel)
    c_start = np.arange(nc)[:, None] * CMP_STRIDE
    s_start = np.arange(nsel)[None, :] * SEL_BLOCK
    overlap = ((c_start < s_start + SEL_BLOCK) & (c_start + CMP_BLOCK > s_start)).astype(np.float32)
    importance = jnp.einsum('bkgtc,cj->bktj', p_cmp, overlap)
    cur = pos // SEL_BLOCK
    blk = jnp.arange(nsel)
    dist = cur[:, None] - blk[None, :]
    forced = (blk[None, :] == 0) | ((dist >= 0) & (dist < N_LOCAL_BLOCKS))
    importance = jnp.where(forced, jnp.inf, jnp.where(dist >= 0, importance, -jnp.inf))
    _, sel_idx = lax.top_k(importance, n_top)

    nq = t // Q_BLOCK
    q_blocks = qg.reshape(b, nq, Q_BLOCK, NSA_KV_HEADS, NSA_GROUP, HEAD_DIM).transpose(1, 0, 3, 2, 4, 5)
    idx_blocks = sel_idx.reshape(b, NSA_KV_HEADS, nq, Q_BLOCK, n_top).transpose(2, 0, 1, 3, 4)
    starts = jnp.arange(nq) * Q_BLOCK
    ks_blk = k_slc.reshape(b, nsel, SEL_BLOCK, NSA_KV_HEADS, HEAD_DIM).transpose(0, 3, 1, 2, 4)
    vs_blk = v_slc.reshape(b, nsel, SEL_BLOCK, NSA_KV_HEADS, HEAD_DIM).transpose(0, 3, 1, 2, 4)
    pad = ((0, 0), (0, 0), (WINDOW, 0), (0, 0))
    kw_pad = jnp.pad(k_win.transpose(0, 2, 1, 3), pad)
    vw_pad = jnp.pad(v_win.transpose(0, 2, 1, 3), pad)
    bi = jnp.arange(b)[:, None, None, None]
    hi = jnp.arange(NSA_KV_HEADS)[None, :, None, None]
    n_keys = n_top * SEL_BLOCK

    def query_block(args):
        qb, ib, start = args
        tq = start + jnp.arange(Q_BLOCK)
        ksel = ks_blk[bi, hi, ib].reshape(b, NSA_KV_HEADS, Q_BLOCK, n_keys, HEAD_DIM)
        vsel = vs_blk[bi, hi, ib].reshape(b, NSA_KV_HEADS, Q_BLOCK, n_keys, HEAD_DIM)
        kpos = (ib[..., None] * SEL_BLOCK + jnp.arange(SEL_BLOCK)).reshape(b, NSA_KV_HEADS, Q_BLOCK, n_keys)
        s_sel = jnp.einsum('bkqgd,bkqsd->bkqgs', qb, ksel) * scale
        p_sel = masked_softmax(s_sel, (kpos <= tq[:, None])[:, :, :, None, :])
        o_sel = jnp.einsum('bkqgs,bkqsd->bkqgd', p_sel, vsel.astype(f32))
        kw = lax.dynamic_slice_in_dim(kw_pad, start, Q_BLOCK + WINDOW, axis=2)
        vw = lax.dynamic_slice_in_dim(vw_pad, start, Q_BLOCK + WINDOW, axis=2)
        wpos = start - WINDOW + jnp.arange(Q_BLOCK + WINDOW)
        valid = (wpos[None, :] <= tq[:, None]) & (wpos[None, :] > tq[:, None] - WINDOW) & (wpos[None, :] >= 0)
        s_win = jnp.einsum('bkqgd,bksd->bkqgs', qb, kw) * scale
        p_win = masked_softmax(s_win, valid[:, None, :])
        o_win = jnp.einsum('bkqgs,bksd->bkqgd', p_win, vw.astype(f32))
        return o_sel, o_win

    o_sel, o_win = lax.map(query_block, (q_blocks, idx_blocks, starts))

    def unblock(o):
        return o.transpose(1, 0, 3, 2, 4, 5).reshape(b, t, NSA_HEADS, HEAD_DIM)

    gates = jax.nn.sigmoid(gate_logits.astype(f32)).reshape(b, t, 3, NSA_HEADS)[..., None]
    o = gates[:, :, 0] * o_cmp + gates[:, :, 1] * unblock(o_sel) + gates[:, :, 2] * unblock(o_win)
    return o.reshape(b, t, NSA_WIDTH).astype(dtype)


def moe_ffn(h, router_w, router_bias, w_gate, w_up, w_down, ws_gate, ws_up, ws_down):
    b, t, d = h.shape
    x = h.reshape(b * t, d)
    n_tok = b * t
    scores = jax.nn.sigmoid((x @ router_w).astype(jnp.float32))
    _, top_idx = lax.top_k(scores + router_bias.astype(jnp.float32), TOP_K)
    s_sel = jnp.take_along_axis(scores, top_idx, axis=-1)
    weights = s_sel / jnp.sum(s_sel, axis=-1, keepdims=True) * ROUTED_SCALE
    combine = jnp.zeros((n_tok, N_EXPERTS), jnp.float32).at[jnp.arange(n_tok)[:, None], top_idx].set(weights)

    def expert_step(acc, args):
        wg, wu, wd, cw = args
        y = (jax.nn.silu(x @ wg) * (x @ wu)) @ wd
        return acc + cw[:, None].astype(y.dtype) * y, None

    routed, _ = lax.scan(expert_step, jnp.zeros_like(x), (w_gate, w_up, w_down, combine.T))
    shared = (jax.nn.silu(x @ ws_gate) * (x @ ws_up)) @ ws_down
    return (routed + shared).reshape(b, t, d)


def setup_inputs(seed: int = 0) -> dict:
    key = jax.random.key(seed)
    ks = jax.random.split(key, 32)
    f32 = jnp.float32
    nrm = lambda k, shape, s: jax.random.normal(k, shape, f32) * s
    dt = jnp.exp(jax.random.uniform(ks[8], (DEPTH, GDN_HEADS), f32, np.log(1e-3), np.log(1e-1)))
    return {
        "x": nrm(ks[0], (BATCH, SEQ, D_MODEL), 1.0),
        "c": nrm(ks[1], (BATCH, D_MODEL), 1.0),
        "w_ada": nrm(ks[2], (DEPTH, D_MODEL, N_MOD * D_MODEL), 0.5 * D_MODEL ** -0.5),
        "b_ada": nrm(ks[3], (DEPTH, N_MOD * D_MODEL), 0.01),
        "norm_attn_w": 1.0 + nrm(ks[4], (DEPTH, D_MODEL), 0.02),
        "norm_ffn_w": 1.0 + nrm(ks[5], (DEPTH, D_MODEL), 0.02),
        "norm_final_w": 1.0 + nrm(ks[6], (D_MODEL,), 0.02),
        "w_in": nrm(ks[7], (DEPTH, D_MODEL, IN_COLS), D_MODEL ** -0.5),
        "gdn_conv_w": nrm(ks[9], (DEPTH, CONV_WIDTH, 3 * GDN_WIDTH), CONV_WIDTH ** -0.5),
        "gdn_a_log": jnp.log(jax.random.uniform(ks[10], (DEPTH, GDN_HEADS), f32, 1.0, 16.0)),
        "gdn_dt_bias": dt + jnp.log(-jnp.expm1(-dt)),
        "gdn_norm_w": 1.0 + nrm(ks[11], (DEPTH, HEAD_DIM), 0.02),
        "cmp_pe_k": nrm(ks[12], (DEPTH, CMP_BLOCK, HEAD_DIM), 0.02),
        "cmp_w1_k": nrm(ks[13], (DEPTH, CMP_BLOCK * HEAD_DIM, CMP_HIDDEN), (CMP_BLOCK * HEAD_DIM) ** -0.5),
        "cmp_w2_k": nrm(ks[14], (DEPTH, CMP_HIDDEN, HEAD_DIM), CMP_HIDDEN ** -0.5),
        "cmp_pe_v": nrm(ks[15], (DEPTH, CMP_BLOCK, HEAD_DIM), 0.02),
        "cmp_w1_v": nrm(ks[16], (DEPTH, CMP_BLOCK * HEAD_DIM, CMP_HIDDEN), (CMP_BLOCK * HEAD_DIM) ** -0.5),
        "cmp_w2_v": nrm(ks[17], (DEPTH, CMP_HIDDEN, HEAD_DIM), CMP_HIDDEN ** -0.5),
        "w_out": nrm(ks[18], (DEPTH, D_MIX, D_MODEL), D_MIX ** -0.5),
        "router_w": nrm(ks[19], (DEPTH, D_MODEL, N_EXPERTS), D_MODEL ** -0.5),
        "router_bias": nrm(ks[20], (DEPTH, N_EXPERTS), 0.01),
        "expert_w_gate": nrm(ks[21], (DEPTH, N_EXPERTS, D_MODEL, EXPERT_FF), D_MODEL ** -0.5),
        "expert_w_up": nrm(ks[22], (DEPTH, N_EXPERTS, D_MODEL, EXPERT_FF), D_MODEL ** -0.5),
        "expert_w_down": nrm(ks[23], (DEPTH, N_EXPERTS, EXPERT_FF, D_MODEL), EXPERT_FF ** -0.5),
        "shared_w_gate": nrm(ks[24], (DEPTH, D_MODEL, SHARED_FF), D_MODEL ** -0.5),
        "shared_w_up": nrm(ks[25], (DEPTH, D_MODEL, SHARED_FF), D_MODEL ** -0.5),
        "shared_w_down": nrm(ks[26], (DEPTH, SHARED_FF, D_MODEL), SHARED_FF ** -0.5),
    }


def reference(x, c, w_ada, b_ada, norm_attn_w, norm_ffn_w, norm_final_w, w_in, gdn_conv_w, gdn_a_log,
              gdn_dt_bias, gdn_norm_w, cmp_pe_k, cmp_w1_k, cmp_w2_k, cmp_pe_v, cmp_w1_v, cmp_w2_v, w_out,
              router_w, router_bias, expert_w_gate, expert_w_up, expert_w_down, shared_w_gate, shared_w_up,
              shared_w_down):
    cond = jax.nn.silu(c)
    split_points = [int(p) for p in np.cumsum(IN_SPLIT_SIZES)[:-1]]
    for l in range(DEPTH):
        mod = cond @ w_ada[l] + b_ada[l]
        shift_a, scale_a, gate_a, shift_m, scale_m, gate_m = [m[:, None, :] for m in jnp.split(mod, N_MOD, axis=-1)]
        h = rms_norm(x, norm_attn_w[l]) * (1.0 + scale_a) + shift_a
        proj = h @ w_in[l]
        qkv_g, beta_g, a_g, gate_g, q_n, kv_n, gate_n = jnp.split(proj, split_points, axis=-1)
        k_cmp, v_cmp, k_slc, v_slc, k_win, v_win = jnp.split(kv_n, 6, axis=-1)
        y_gdn = gated_deltanet(qkv_g, beta_g, a_g, gate_g, gdn_conv_w[l], gdn_a_log[l], gdn_dt_bias[l], gdn_norm_w[l])
        y_nsa = native_sparse_attention(q_n, k_cmp, v_cmp, k_slc, v_slc, k_win, v_win, gate_n,
                                        cmp_pe_k[l], cmp_w1_k[l], cmp_w2_k[l], cmp_pe_v[l], cmp_w1_v[l], cmp_w2_v[l])
        x = x + gate_a * (jnp.concatenate([y_gdn, y_nsa], axis=-1) @ w_out[l])
        h = rms_norm(x, norm_ffn_w[l]) * (1.0 + scale_m) + shift_m
        x = x + gate_m * moe_ffn(h, router_w[l], router_bias[l], expert_w_gate[l], expert_w_up[l], expert_w_down[l],
                                 shared_w_gate[l], shared_w_up[l], shared_w_down[l])
    return rms_norm(x, norm_final_w)
```

```python
import numpy as np
from contextlib import ExitStack
import ml_dtypes
import concourse.bass as bass
import concourse.mybir as mybir
from concourse.bass_utils import run_bass_kernel_spmd

F32 = mybir.dt.float32
F32R = mybir.dt.float32r
BF16 = mybir.dt.bfloat16
AF = mybir.ActivationFunctionType
ALU = mybir.AluOpType

D = 2048
T = 4096
NEG = -10000.0
EPS = 1e-6
SCALE = 128 ** -0.5


class Buf:
    __slots__ = ("w", "r", "name", "excl")

    def __init__(self, name="", excl=False):
        self.w = None
        self.r = {}
        self.name = name
        self.excl = excl


class Ctx:
    NS = 6

    def __init__(self, nc, es):
        self.nc = nc
        self.es = es
        self.E = {"pe": nc.tensor, "dve": nc.vector, "act": nc.scalar, "pool": nc.gpsimd, "sp": nc.sync}
        self.sem = {}
        self.cnt = {}
        for k in self.E:
            self.sem[k] = es.enter_context(nc.semaphore("s_" + k))
            self.cnt[k] = 0
        self.dq = {}
        for q in ("sp", "pool"):
            sl = []
            for i in range(self.NS):
                key = "d_%s%d" % (q, i)
                self.sem[key] = es.enter_context(nc.semaphore(key))
                self.cnt[key] = 0
                sl.append(key)
            self.dq[q] = [sl, 0]
        self.sem["d_coll"] = es.enter_context(nc.semaphore("d_coll"))
        self.cnt["d_coll"] = 0
        self.seen = {k: {} for k in self.E}
        self.uid = 0

    def sb(self, name, shape, dt):
        self.uid += 1
        return self.es.enter_context(self.nc.sbuf_tensor("%s_%d" % (name, self.uid), list(shape), dt))

    def ps(self, name, shape, dt=F32):
        self.uid += 1
        return self.es.enter_context(self.nc.psum_tensor("%s_%d" % (name, self.uid), list(shape), dt))

    def _wait(self, eng, tok):
        if tok is None:
            return
        key, val = tok
        if key == eng and eng == "pe":
            return
        if self.seen[eng].get(key, 0) >= val:
            return
        self.seen[eng][key] = val
        self.E[eng].wait_ge(self.sem[key], val)

    def _deps(self, eng, reads, writes):
        for b in reads:
            self._wait(eng, b.w)
            if b.excl:
                for t in list(b.r.items()):
                    if t[0] != eng:
                        self._wait(eng, t)
        for b in writes:
            self._wait(eng, b.w)
            for t in list(b.r.items()):
                self._wait(eng, t)

    def _mark(self, tok, reads, writes):
        for b in reads:
            if b.r.get(tok[0], 0) < tok[1]:
                b.r[tok[0]] = tok[1]
        for b in writes:
            b.w = tok
            b.r = {}

    def op(self, eng, fn, R=(), W=(), inc=True):
        self._deps(eng, R, W)
        ins = fn(self.E[eng])
        if inc:
            self.cnt[eng] += 1
            ins.then_inc(self.sem[eng], 1)
            tok = (eng, self.cnt[eng])
        else:
            tok = (eng, self.cnt[eng] + 1)
        self._mark(tok, R, W)
        return ins

    def dma(self, q, out, in_, R=(), W=(), **kw):
        sl, k = self.dq[q]
        key = sl[k % self.NS]
        self.dq[q][1] = k + 1
        if self.cnt[key]:
            self._wait(q, (key, self.cnt[key]))
        self._deps(q, R, W)
        ins = self.E[q].dma_start(out=out, in_=in_, **kw)
        self.cnt[key] += 16
        ins.then_inc(self.sem[key], 16)
        tok = (key, self.cnt[key])
        self._mark(tok, R, W)
        return tok

    def coll(self, fn, R=(), W=()):
        self._deps("pool", R, W)
        ins = fn(self.E["pool"])
        self.cnt["d_coll"] += 16
        ins.then_inc(self.sem["d_coll"], 16)
        tok = ("d_coll", self.cnt["d_coll"])
        self._mark(tok, R, W)
        return ins

    def barrier(self):
        for e in self.E:
            for k, v in self.cnt.items():
                if v and k != e:
                    self._wait(e, (k, v))

    def drain(self):
        for k, v in self.cnt.items():
            if v:
                self._wait("sp", (k, v))

    def finish(self, bufs):
        for b in bufs:
            self._wait("sp", b.w)

    def mm(self, out, lhsT, rhs, R, W, start=True, stop=True, inc=True):
        return self.op("pe", lambda e: e.matmul(out, lhsT, rhs, start=start, stop=stop), R, W, inc=inc)

    def tr(self, out, in_, ident, R, W):
        return self.op("pe", lambda e: e.transpose(out, in_, ident), R, W)

    def act(self, out, in_, func, R, W, **kw):
        return self.op("act", lambda e: e.activation(out=out, in_=in_, func=func, **kw), R, W)

    def tt(self, out, in0, in1, op, R, W, eng="dve"):
        return self.op(eng, lambda e: e.tensor_tensor(out=out, in0=in0, in1=in1, op=op), R, W)

    def ts(self, out, in0, s1, s2, op0, op1, R, W, eng="dve"):
        if op1 is None:
            return self.op(eng, lambda e: e.tensor_scalar(out=out, in0=in0, scalar1=s1, scalar2=None, op0=op0), R, W)
        return self.op(eng, lambda e: e.tensor_scalar(out=out, in0=in0, scalar1=s1, scalar2=s2, op0=op0, op1=op1), R, W)

    def stt(self, out, in0, scalar, in1, op0, op1, R, W):
        return self.op("dve", lambda e: e.scalar_tensor_tensor(out=out, in0=in0, scalar=scalar, in1=in1, op0=op0, op1=op1), R, W)

    def cp(self, out, in_, R, W, eng="dve"):
        if eng == "act":
            return self.op("act", lambda e: e.copy(out=out, in_=in_), R, W)
        return self.op(eng, lambda e: e.tensor_copy(out=out, in_=in_), R, W)


class PSlots:
    def __init__(self, c, name, nbanks, per, width=None):
        self.slots = []
        w = width or 512 // per
        for b in range(nbanks):
            t = c.ps(name, [128, 512])
            bb = Buf(excl=True)
            for s in range(per):
                self.slots.append((t[:, s * w:(s + 1) * w], bb))
        self.i = 0

    def get(self):
        s = self.slots[self.i % len(self.slots)]
        self.i += 1
        return s


def r32(ap):
    return ap


def build(debug=False, n_exp=64, stop=None):
    nc = bass.Bass("TRN2", target_bir_lowering=False)

    def din(name, shape, dt=F32):
        return nc.dram_tensor(name, list(shape), dt, kind="ExternalInput").ap()

    xb = din("xb", [T, D])
    xq = din("xq", [1024, D])
    cvec = din("cvec", [128, 16])
    w_ada = din("w_ada", [D, 6 * D])
    b_ada = din("b_ada", [6 * D])
    nrm_a = din("nrm_a", [128, 16])
    nrm_m = din("nrm_m", [128, 16])
    nrm_f = din("nrm_f", [D])
    wA_fm = din("wA_fm", [D, 768])
    wA_tm = din("wA_tm", [D, 260])
    convw = din("convw", [128, 6, 4])
    alog = din("alog", [128, 2])
    dtb = din("dtb", [128, 2])
    gnw = din("gnw", [128, 128])
    wB_fm = din("wB_fm", [D, 1024])
    wB_tm = din("wB_tm", [D, 262])
    w1k = din("w1k", [4096, 256])
    w1v = din("w1v", [4096, 256])
    w2k = din("w2k", [256, 128])
    w2v = din("w2v", [256, 128])
    pek = din("pek", [128, 32])
    pev = din("pev", [128, 32])
    wout = din("wout", [512, D])
    rw = din("rw", [D, 64])
    rbias = din("rbias", [128, 64])
    if n_exp:
        ewg = din("ewg", [64, D, 512])
        ewu = din("ewu", [64, D, 512])
        ewd = din("ewd", [64, 512, D])
    swg = din("swg", [D, 512])
    swu = din("swu", [D, 512])
    swd = din("swd", [512, D])
    k_identf = din("k_identf", [128, 128])
    k_identb = din("k_identb", [128, 128], BF16)
    k_U = din("k_U", [128, 128])
    k_SL = din("k_SL", [128, 128])
    k_ILT = din("k_ILT", [128, 128])
    k_ones = din("k_ones", [128, 128])
    k_triU = din("k_triU", [128, 256], BF16)
    k_triL = din("k_triL", [128, 256], BF16)
    k_E = din("k_E", [64, 32, 128], BF16)
    k_ovl = din("k_ovl", [128, 2, 65])
    k_cb0 = din("k_cb0", [128, 17, 128], BF16)
    k_fb = din("k_fb", [128, 32, 64], BF16)

    y = nc.dram_tensor("y", [1024, D], F32, kind="ExternalOutput").ap()

    dk = dict(kind="ExternalOutput") if debug else {}
    hT_d = nc.dram_tensor("hT_d", [128, 16, T], BF16, **dk).ap()
    modD = nc.dram_tensor("modD", [96, 128], F32, **dk).ap()
    partial = nc.dram_tensor("partial", [T, D], F32)
    rs_out = nc.dram_tensor("rs_out", [1024, D], F32)
    fence_in = nc.dram_tensor("fence_in", [128, 128], F32)
    fence_out = nc.dram_tensor("fence_out", [128, 128], F32)
    x1_d = nc.dram_tensor("x1_d", [1024, D], F32, **dk).ap()
    if debug:
        yTg_d = nc.dram_tensor("yTg_d", [128, 2, T], BF16, kind="ExternalOutput").ap()
        yTn_d = nc.dram_tensor("yTn_d", [128, 2, T], BF16, kind="ExternalOutput").ap()
        part_d = nc.dram_tensor("part_d", [T, D], F32, kind="ExternalOutput").ap()

    with ExitStack() as es0:
        c = Ctx(nc, es0)

        def load_const(name, src, shape, dt, q="sp"):
            t = c.sb(name, shape, dt)
            b = Buf(name)
            c.dma(q, t[:], src, W=[b])
            return t, b

        identf, b_identf = load_const("identf", k_identf, [128, 128], F32)
        identb, b_identb = load_const("identb", k_identb, [128, 128], BF16)
        nrmA, b_nrmA = load_const("nrmA", nrm_a, [128, 16], F32)
        nrmM, b_nrmM = load_const("nrmM", nrm_m, [128, 16], F32)
        modT = c.sb("modT", [128, 96], F32)
        b_modT = Buf()
        A1 = c.sb("A1", [128, 16], F32)
        A2 = c.sb("A2", [128, 16], F32)
        b_A = Buf()

        b_modD = Buf()

        def ada_part(j0, j1, last):
            with ExitStack() as es:
                c.es = es
                cv = c.sb("cv", [128, 16], F32)
                b_cv = Buf()
                c.dma("sp", cv[:], cvec, W=[b_cv])
                cond = c.sb("cond", [128, 16, 2], F32)
                b_cond = Buf()
                for j in range(2):
                    c.act(cond[:, :, j], cv[:], AF.Silu, [b_cv], [b_cond])
                wt = [c.sb("wada", [128, 16, 512], F32) for _ in range(2)]
                bw = [Buf(), Buf()]
                prow = [c.ps("prow", [128, 512]) for _ in range(2)]
                b_prow = [Buf(excl=True), Buf(excl=True)]
                pmT = c.ps("pmT", [128, 512])
                b_pmT = Buf(excl=True)
                rowsb = [c.sb("rowsb", [2, 512], F32) for _ in range(2)]
                b_rowsb = [Buf(), Buf()]
                bad = c.sb("bad", [128, 96], F32)
                b_bad = Buf()
                c.dma("sp", bad[:], b_ada.rearrange("(j p) -> p j", p=128), W=[b_bad], allow_slow_non_contiguous=True)
                for blk in range(j0 // 4, j1 // 4):
                    k2 = blk % 2
                    c.dma("sp", wt[k2][:], w_ada[:, blk * 512:(blk + 1) * 512].rearrange("(kc p) n -> p kc n", p=128), W=[bw[k2]])
                    for kc in range(16):
                        c.mm(prow[k2][0:2, :], cond[:, kc, :], wt[k2][:, kc, :], [bw[k2], b_cond], [b_prow[k2]],
                             start=(kc == 0), stop=(kc == 15), inc=(kc == 15))
                    c.cp(rowsb[k2][:], prow[k2][0:2, :], [b_prow[k2]], [b_rowsb[k2]], eng="act")
                    for nn in range(4):
                        jj = blk * 4 + nn - j0
                        c.tr(pmT[:, 2 * jj:2 * jj + 2], rowsb[k2][0:2, nn * 128:(nn + 1) * 128], identf[0:2, 0:2],
                             [b_rowsb[k2], b_identf], [b_pmT])
                nj = j1 - j0
                c.tt(modT[:, j0:j1], pmT[:, 0:2 * nj].rearrange("p (j two) -> p j two", two=2)[:, :, 0], bad[:, j0:j1], ALU.add,
                     [b_pmT, b_bad], [b_modT])
                if not last:
                    c.stt(A1[:], modT[:, 16:32], 1.0, nrmA[:], ALU.add, ALU.mult, [b_modT, b_nrmA], [b_A])
                else:
                    c.stt(A2[:], modT[:, 64:80], 1.0, nrmM[:], ALU.add, ALU.mult, [b_modT, b_nrmM], [b_A])
                    ptm = c.ps("ptm", [128, 512])
                    b_ptm = Buf(excl=True)
                    c.tr(ptm[0:96, 0:128], modT[:], identf[:], [b_modT, b_identf], [b_ptm])
                    mrow = c.sb("mrow", [96, 128], F32)
                    b_mrow = Buf()
                    c.cp(mrow[:], ptm[0:96, 0:128], [b_ptm], [b_mrow])
                    c.dma("sp", modD, mrow[:], R=[b_mrow], W=[b_modD])
            c.barrier()
            c.es = es0

        ada_part(0, 32, False)
        if stop == "p0all":
            ada_part(32, 96, True)
            c.drain()
            return nc
        if stop == "p0":
            c.drain()
            return nc
        B1 = modT[:, 0:16]
        B2 = modT[:, 48:64]

        def norm_tile(xt, b_xt, Acol, Bcol, hT, b_hT, col0, tmp, ptr):
            sq, ssq, rstd, xn = tmp["sq"], tmp["ss"], tmp["rstd"], tmp["xn"]
            b = tmp["b"]
            c.act(sq[:], xt[:], AF.Square, [b_xt], [b["sq"], b["ss"]], accum_out=ssq[:])
            c.ts(rstd[:], ssq[:], 1.0 / D, EPS, ALU.mult, ALU.add, [b["ss"]], [b["rstd"]])
            c.act(rstd[:], rstd[:], AF.Sqrt, [b["rstd"]], [b["rstd"]])
            c.op("dve", lambda e: e.reciprocal(out=rstd[:], in_=rstd[:]), [b["rstd"]], [b["rstd"]])
            c.ts(xn[:], xt[:], rstd[:, 0:1], None, ALU.mult, None, [b_xt, b["rstd"]], [b["xn"]])
            for k4 in range(4):
                pt, b_pt = ptr.get()
                ptb = pt.bitcast(BF16)
                for i in range(4):
                    kc = k4 * 4 + i
                    c.tr(ptb[:, i * 128:(i + 1) * 128], xn[:, kc * 128:(kc + 1) * 128], identb[:], [b["xn"], b_identb], [b_pt])
                for i in range(4):
                    kc = k4 * 4 + i
                    c.act(hT[:, kc, col0:col0 + 128], ptb[:, i * 128:(i + 1) * 128], AF.Identity, [b_pt, b_A, b_modT], [b_hT],
                          scale=Acol[:, kc:kc + 1], bias=Bcol[:, kc:kc + 1])

        def norm_tmp():
            return {"sq": c.sb("sq", [128, D], BF16), "ss": c.sb("ss", [128, 1], F32), "rstd": c.sb("rstd", [128, 1], F32),
                    "xn": c.sb("xn", [128, D], BF16), "b": {k: Buf() for k in ("sq", "ss", "rstd", "xn")}}

        yTg = c.sb("yTg", [128, 2, T], BF16)
        b_yTg = [Buf() for _ in range(8)]
        b_hTd = [Buf() for _ in range(8)]
        b_parts = []
        dbg_bufs = []

        with ExitStack() as es:
            c.es = es
            wfm = c.sb("wAfm", [128, 16, 768], BF16)
            wtm = c.sb("wAtm", [128, 16, 260], BF16)
            b_wfm, b_wtm = Buf(), Buf()
            c.dma("pool", wfm[:], wA_fm.rearrange("(kc p) n -> p kc n", p=128), W=[b_wfm])
            c.dma("pool", wtm[:], wA_tm.rearrange("(kc p) n -> p kc n", p=128), W=[b_wtm])
            cw, b_cw = load_const("cw", convw, [128, 6, 4], F32)
            Uc, b_U = load_const("Uc", k_U, [128, 128], F32)
            SLc, b_SL = load_const("SLc", k_SL, [128, 128], F32)
            ILTc, b_ILT = load_const("ILTc", k_ILT, [128, 128], F32)
            onesc, b_ones = load_const("onesc", k_ones, [128, 128], F32)
            al, b_al = load_const("al", alog, [128, 2], F32)
            dtbc, b_dtb = load_const("dtbc", dtb, [128, 2], F32)
            gnwr = c.sb("gnwr", [128, 128], F32)
            b_gnw = Buf()
            c.dma("sp", gnwr[:], gnw, W=[b_gnw])
            nega = c.sb("nega", [128, 2], F32)
            b_nega = Buf()
            c.act(nega[:], al[:], AF.Exp, [b_al], [b_nega])
            c.ts(nega[:], nega[:], -1.0, None, ALU.mult, None, [b_nega], [b_nega])

            hT = c.sb("hT", [128, 16, 512], BF16)
            b_hT = Buf()
            xts = [c.sb("xt", [128, D], F32) for _ in range(2)]
            b_xts = [Buf(), Buf()]
            ntmp = norm_tmp()
            ptr = PSlots(c, "ptr", 1, 1)
            pproj = PSlots(c, "pproj", 2, 1)
            pg = PSlots(c, "pg", 5, 1, 128)
            for ap_, bb_ in ptr.slots + pproj.slots:
                pg.slots.append((ap_[:, 0:128], bb_))

            raw = c.sb("raw", [128, 6, 515], F32)
            b_raw = [Buf() for _ in range(6)]
            for u in range(6):
                c.op("pool", lambda e: e.memset(raw[:, u, 0:3], 0.0), [], [b_raw[u]])
            qkv = c.sb("qkvs", [128, 6, 512], F32)
            b_qkv = [Buf() for _ in range(6)]
            sqt = c.sb("sqt", [128, 512], F32)
            b_sqt = Buf()
            rn = c.sb("rn", [128, 512], F32)
            b_rn = Buf()
            gsil = c.sb("gsil", [128, 4, 256], BF16)
            b_gsil = [Buf() for _ in range(4)]
            gb = c.sb("gb", [128, 4, 4], F32)
            b_gb = [Buf() for _ in range(4)]
            sp1 = c.sb("sp1", [128, 2], F32)
            b_sp1 = Buf()
            S = [c.sb("S", [128, 128], F32) for _ in range(2)]
            b_S = [Buf(), Buf()]
            for h in range(2):
                c.op("pool", lambda e: e.memset(S[h][:], 0.0), [], [b_S[h]])

            def unit_tmp():
                t = {}
                for n in ("Gs", "Dm", "DTm", "A", "AT", "Pa", "PaT", "Pb", "PbT", "FT", "QK", "WT", "kdec", "o1s", "o", "vnew"):
                    t[n] = c.sb("u_" + n, [128, 128], F32)
                for n in ("Xa", "Xb"):
                    t[n] = c.sb("u_" + n, [128, 256], F32)
                for n in ("bcol", "eb", "ebl", "edec", "bek", "ss2", "rs2"):
                    t[n] = c.sb("u_" + n, [128, 1], F32)
                t["y"] = c.sb("u_y", [128, 128], BF16)
                t["b"] = {}
                return t
            units = [[unit_tmp() for _ in range(2)] for _ in range(2)]

            def B(u, n):
                if n not in u["b"]:
                    u["b"][n] = Buf(n)
                return u["b"][n]

            for blk in range(8):
                for tt in range(4):
                    ti = blk * 4 + tt
                    xt, b_xt = xts[ti % 2], b_xts[ti % 2]
                    c.dma("sp", xt[:], xb[ti * 128:(ti + 1) * 128, :], W=[b_xt])
                    norm_tile(xt, b_xt, A1, B1, hT, b_hT, tt * 128, ntmp, ptr)
                c.dma("sp", hT_d[:, :, blk * 512:(blk + 1) * 512], hT[:], R=[b_hT], W=[b_hTd[blk]])
                if stop == "A1" and blk == 0:
                    c.drain()
                    return nc
                for u in range(6):
                    pp, b_pp = pproj.get()
                    for kc in range(16):
                        c.mm(pp, wfm[:, kc, u * 128:(u + 1) * 128], hT[:, kc, :], [b_wfm, b_hT], [b_pp],
                             start=(kc == 0), stop=(kc == 15), inc=(kc == 15))
                    c.cp(raw[:, u, 3:515], pp, [b_pp], [b_raw[u]], eng="act")
                    c.ts(qkv[:, u, :], raw[:, u, 0:512], cw[:, u, 0:1], None, ALU.mult, None, [b_raw[u], b_cw], [b_qkv[u]])
                    for i in range(1, 4):
                        c.stt(qkv[:, u, :], raw[:, u, i:i + 512], cw[:, u, i:i + 1], qkv[:, u, :], ALU.mult, ALU.add,
                              [b_raw[u], b_cw, b_qkv[u]], [b_qkv[u]])
                    c.cp(raw[:, u, 0:3], raw[:, u, 512:515], [b_raw[u]], [b_raw[u]], eng="pool")
                    c.act(qkv[:, u, :], qkv[:, u, :], AF.Silu, [b_qkv[u]], [b_qkv[u]])
                if stop == "A2" and blk == 0:
                    c.drain()
                    return nc
                for u in range(4):
                    c.tt(sqt[:], qkv[:, u, :], qkv[:, u, :], ALU.mult, [b_qkv[u]], [b_sqt])
                    pp, b_pp = pproj.get()
                    c.mm(pp, r32(onesc[:]), r32(sqt[:]), [b_ones, b_sqt], [b_pp])
                    c.ts(rn[:], pp, EPS, None, ALU.add, None, [b_pp], [b_rn])
                    c.act(rn[:], rn[:], AF.Sqrt, [b_rn], [b_rn])
                    c.op("dve", lambda e: e.reciprocal(out=rn[:], in_=rn[:]), [b_rn], [b_rn])
                    c.stt(qkv[:, u, :], qkv[:, u, :], (SCALE if u < 2 else 1.0), rn[:], ALU.mult, ALU.mult,
                          [b_qkv[u], b_rn], [b_qkv[u]])
                for tt in range(4):
                    pp, b_pp = pproj.get()
                    for kc in range(16):
                        c.mm(pp[:, 0:260], hT[:, kc, tt * 128:(tt + 1) * 128], wtm[:, kc, :], [b_wtm, b_hT], [b_pp],
                             start=(kc == 0), stop=(kc == 15), inc=(kc == 15))
                    c.act(gsil[:, tt, :], pp[:, 0:256], AF.Silu, [b_pp], [b_gsil[tt]])
                    c.act(gb[:, tt, 0:2], pp[:, 256:258], AF.Sigmoid, [b_pp], [b_gb[tt]])
                    for h in range(2):
                        c.act(sp1[:, h:h + 1], pp[:, 258 + h:259 + h], AF.Exp, [b_pp, b_dtb], [b_sp1], bias=dtbc[:, h:h + 1], scale=1.0)
                    c.act(sp1[:], sp1[:], AF.Ln, [b_sp1], [b_sp1], bias=1.0, scale=1.0)
                    c.tt(gb[:, tt, 2:4], sp1[:], nega[:], ALU.mult, [b_sp1, b_nega], [b_gb[tt]])

                if stop == "A2b" and blk == 0:
                    c.drain()
                    return nc
                def tile_ctx(tt):
                    ti = blk * 4 + tt
                    return ti, slice(tt * 128, (tt + 1) * 128), [units[ti % 2][h] for h in range(2)], b_gb[tt]

                def g_pre(tt, h):
                    ti, tsl, us, bg = tile_ctx(tt)
                    u = us[h]
                    beta = gb[:, tt, h:h + 1]
                    g = gb[:, tt, 2 + h:3 + h]
                    yield
                    p1, bp1 = pg.get()
                    c.mm(p1[:, 0:2], Uc[:], gb[:, tt, 2:4], [b_U, bg], [bp1])
                    c.cp(u["bcol"][:], p1[:, h:h + 1], [bp1], [B(u, "bcol")])
                    yield
                    p2, bp2 = pg.get()
                    c.mm(p2[:, 0:2], onesc[:], gb[:, tt, 2:4], [b_ones, bg], [bp2])
                    c.act(u["ebl"][:], p2[:, h:h + 1], AF.Exp, [bp2], [B(u, "ebl")])
                    c.cp(u["ss2"][:], p2[:, h:h + 1], [bp2], [B(u, "ss2")])
                    c.act(u["edec"][:], u["bcol"][:], AF.Exp, [B(u, "bcol"), B(u, "ss2")], [B(u, "edec")], scale=-1.0, bias=u["ss2"][:, 0:1])
                    c.act(u["eb"][:], u["bcol"][:], AF.Exp, [B(u, "bcol")], [B(u, "eb")])
                    c.tt(u["bek"][:], u["eb"][:], beta, ALU.mult, [B(u, "eb"), bg], [B(u, "bek")])
                    c.ts(u["Gs"][:], SLc[:], g, None, ALU.mult, None, [b_SL, bg], [B(u, "Gs")])
                    yield
                    p3, bp3 = pg.get()
                    c.mm(p3, Uc[:], u["Gs"][:], [b_U, B(u, "Gs")], [bp3])
                    c.act(u["Dm"][:], p3, AF.Exp, [bp3], [B(u, "Dm")])
                    c.stt(u["Dm"][:], u["Dm"][:], beta, SLc[:], ALU.mult, ALU.mult, [B(u, "Dm"), bg, b_SL], [B(u, "Dm")])
                    yield
                    p4, bp4 = pg.get()
                    c.mm(p4, u["Gs"][:], Uc[:], [b_U, B(u, "Gs")], [bp4])
                    c.act(u["DTm"][:], p4, AF.Exp, [bp4], [B(u, "DTm")])
                    c.tt(u["DTm"][:], u["DTm"][:], ILTc[:], ALU.mult, [B(u, "DTm"), b_ILT], [B(u, "DTm")])
                    u = us[h]
                    beta = gb[:, tt, h:h + 1]
                    KT = qkv[:, 2 + h, tsl]
                    QT = qkv[:, h, tsl]
                    VT = qkv[:, 4 + h, tsl]
                    yield
                    p1, bp1 = pg.get()
                    c.mm(p1, r32(KT), r32(KT), [b_qkv[2 + h]], [bp1])
                    c.tt(u["A"][:], p1, u["Dm"][:], ALU.mult, [bp1, B(u, "Dm")], [B(u, "A")])
                    yield
                    p2, bp2 = pg.get()
                    c.mm(p2, r32(KT), r32(QT), [b_qkv[2 + h], b_qkv[h]], [bp2])
                    c.tt(u["QK"][:], p2, u["DTm"][:], ALU.mult, [bp2, B(u, "DTm")], [B(u, "QK")])
                    yield
                    p3, bp3 = pg.get()
                    c.tr(p3, u["A"][:], identf[:], [B(u, "A"), b_identf], [bp3])
                    c.cp(u["AT"][:], p3, [bp3], [B(u, "AT")], eng="act")
                    yield
                    p4, bp4 = pg.get()
                    c.tr(p4, VT, identf[:], [b_qkv[4 + h], b_identf], [bp4])
                    c.ts(u["Xa"][:, 0:128], p4, beta, None, ALU.mult, None, [bp4, bg], [B(u, "Xa")])
                    yield
                    p5, bp5 = pg.get()
                    c.tr(p5, KT, identf[:], [b_qkv[2 + h], b_identf], [bp5])
                    c.ts(u["Xa"][:, 128:256], p5, u["bek"][:, 0:1], None, ALU.mult, None, [bp5, B(u, "bek")], [B(u, "Xa")])
                    c.act(u["kdec"][:], p5, AF.Identity, [bp5, B(u, "edec")], [B(u, "kdec")], scale=u["edec"][:, 0:1])
                    u = us[h]
                    X = [(u["Xa"], B(u, "Xa")), (u["Xb"], B(u, "Xb"))]
                    c.stt(u["FT"][:], u["AT"][:], -1.0, identf[:], ALU.mult, ALU.add, [B(u, "AT"), b_identf], [B(u, "FT")])
                    xi = 0
                    Pc, PcT, bPc, bPcT = u["A"], u["AT"], B(u, "A"), B(u, "AT")
                    pp_ = [(u["Pa"], u["PaT"], "Pa", "PaT"), (u["Pb"], u["PbT"], "Pb", "PbT")]
                    for lvl in range(7):
                        Xc, bXc = X[xi]
                        Xn, bXn = X[1 - xi]
                        yield
                        pa, bpa = pg.get()
                        pa2, bpa2 = pg.get()
                        c.mm(pa, r32(u["FT"][:]), r32(Xc[:, 0:128]), [B(u, "FT"), bXc], [bpa])
                        c.mm(pa2, r32(u["FT"][:]), r32(Xc[:, 128:256]), [B(u, "FT"), bXc], [bpa2])
                        c.cp(Xn[:, 0:128], pa, [bpa], [bXn], eng="act")
                        c.cp(Xn[:, 128:256], pa2, [bpa2], [bXn])
                        xi = 1 - xi
                        if lvl == 6:
                            break
                        Pn, PnT, nPn, nPnT = pp_[lvl % 2]
                        yield
                        pq, bpq = pg.get()
                        c.mm(pq, r32(Pc[:]), r32(PcT[:]), [bPc, bPcT], [bpq])
                        c.cp(PnT[:], pq, [bpq], [B(u, nPnT)], eng="act")
                        c.tt(u["FT"][:], pq, identf[:], ALU.add, [bpq, b_identf], [B(u, "FT")])
                        if lvl < 5:
                            yield
                            pq2, bpq2 = pg.get()
                            c.mm(pq2, r32(PcT[:]), r32(Pc[:]), [bPc, bPcT], [bpq2])
                            c.cp(Pn[:], pq2, [bpq2], [B(u, nPn)])
                        Pc, PcT, bPc, bPcT = Pn, PnT, B(u, nPn), B(u, nPnT)
                    u["Xf"], u["bXf"] = X[xi]
                    yield
                    pw, bpw = pg.get()
                    c.tr(pw, u["Xf"][:, 128:256], identf[:], [u["bXf"], b_identf], [bpw])
                    c.cp(u["WT"][:], pw, [bpw], [B(u, "WT")], eng="act")

                def g_seq(tt, h):
                    ti, tsl, us, bg = tile_ctx(tt)
                    u = us[h]
                    QT = qkv[:, h, tsl]
                    Xf, bXf = u["Xf"], u["bXf"]
                    yield
                    p1, bp1 = pg.get()
                    c.mm(p1, r32(u["WT"][:]), r32(S[h][:]), [B(u, "WT"), b_S[h]], [bp1])
                    c.tt(u["vnew"][:], Xf[:, 0:128], p1, ALU.subtract, [bXf, bp1], [B(u, "vnew")])
                    yield
                    p2, bp2 = pg.get()
                    c.mm(p2, r32(QT), r32(S[h][:]), [b_qkv[h], b_S[h]], [bp2])
                    c.act(u["o1s"][:], p2, AF.Identity, [bp2, B(u, "eb")], [B(u, "o1s")], scale=u["eb"][:, 0:1])
                    yield
                    p3, bp3 = pg.get()
                    c.mm(p3, r32(u["QK"][:]), r32(u["vnew"][:]), [B(u, "QK"), B(u, "vnew")], [bp3])
                    c.tt(u["o"][:], u["o1s"][:], p3, ALU.add, [B(u, "o1s"), bp3], [B(u, "o")])
                    yield
                    p4, bp4 = pg.get()
                    c.mm(p4, r32(u["kdec"][:]), r32(u["vnew"][:]), [B(u, "kdec"), B(u, "vnew")], [bp4])
                    c.stt(S[h][:], S[h][:], u["ebl"][:, 0:1], p4, ALU.mult, ALU.add, [b_S[h], B(u, "ebl"), bp4], [b_S[h]])
                    c.act(u["o1s"][:], u["o"][:], AF.Square, [B(u, "o")], [B(u, "o1s"), B(u, "ss2")], accum_out=u["ss2"][:])
                    c.ts(u["rs2"][:], u["ss2"][:], 1.0 / 128, EPS, ALU.mult, ALU.add, [B(u, "ss2")], [B(u, "rs2")])
                    c.act(u["rs2"][:], u["rs2"][:], AF.Sqrt, [B(u, "rs2")], [B(u, "rs2")])
                    c.op("dve", lambda e: e.reciprocal(out=u["rs2"][:], in_=u["rs2"][:]), [B(u, "rs2")], [B(u, "rs2")])
                    c.stt(u["o"][:], u["o"][:], u["rs2"][:, 0:1], gnwr[:], ALU.mult, ALU.mult, [B(u, "o"), B(u, "rs2"), b_gnw], [B(u, "o")])
                    c.tt(u["y"][:], u["o"][:], gsil[:, tt, h * 128:(h + 1) * 128], ALU.mult, [B(u, "o"), b_gsil[tt]], [B(u, "y")])
                    yield
                    py, bpy = pg.get()
                    pyb = py.bitcast(BF16)
                    c.tr(pyb[:, 0:128], u["y"][:], identb[:], [B(u, "y"), b_identb], [bpy])
                    c.cp(yTg[:, h, ti * 128:(ti + 1) * 128], pyb[:, 0:128], [bpy], [b_yTg[blk]], eng="act")

                def run_rr(gens):
                    gens = list(gens)
                    while gens:
                        for g_ in list(gens):
                            try:
                                next(g_)
                            except StopIteration:
                                gens.remove(g_)

                for step in range(5):
                    gens = []
                    if step < 4:
                        gens += [g_pre(step, 0), g_pre(step, 1)]
                    if step > 0:
                        gens += [g_seq(step - 1, 0), g_seq(step - 1, 1)]
                    run_rr(gens)
            if debug:
                bd = Buf(); dbg_bufs.append(bd)
                c.dma("sp", yTg_d, yTg[:], R=b_yTg, W=[bd])
        c.barrier()
        c.es = es0
        if stop == "A":
            c.drain()
            return nc

        with ExitStack() as es:
            c.es = es
            wfm = c.sb("wBfm", [128, 16, 1024], BF16)
            wtm = c.sb("wBtm", [128, 16, 262], BF16)
            b_wfm, b_wtm = Buf(), Buf()
            c.dma("pool", wfm[:], wB_fm.rearrange("(kc p) n -> p kc n", p=128), W=[b_wfm])
            c.dma("pool", wtm[:], wB_tm.rearrange("(kc p) n -> p kc n", p=128), W=[b_wtm])
            w1 = [c.sb("w1", [128, 32, 256], BF16) for _ in range(2)]
            b_w1 = [Buf(), Buf()]
            for i, src in enumerate((w1k, w1v)):
                for half in range(2):
                    c.dma("pool", w1[i][:, half * 16:(half + 1) * 16, :],
                          src[half * 2048:(half + 1) * 2048, :].rearrange("(l p) n -> p l n", p=128), W=[b_w1[i]])
            w2 = [c.sb("w2", [128, 2, 128], BF16) for _ in range(2)]
            b_w2 = [Buf(), Buf()]
            for i, src in enumerate((w2k, w2v)):
                c.dma("pool", w2[i][:], src.rearrange("(a p) n -> p a n", p=128), W=[b_w2[i]])
            pe_ = [c.sb("pe", [128, 32], BF16) for _ in range(2)]
            b_pe = [Buf(), Buf()]
            for i, src in enumerate((pek, pev)):
                c.dma("pool", pe_[i][:], src, W=[b_pe[i]])
            wo = c.sb("wo", [128, 4, D], BF16)
            b_wo = Buf()
            c.dma("pool", wo[:], wout.rearrange("(a p) n -> p a n", p=128), W=[b_wo])
            triU, b_triU = load_const("triU", k_triU, [128, 256], BF16)
            triL, b_triL = load_const("triL", k_triL, [128, 256], BF16)
            Ec, b_E = load_const("Ec", k_E, [64, 32, 128], BF16)
            cb0, b_cb0 = load_const("cb0", k_cb0, [128, 17, 128], BF16)
            fb, b_fb = load_const("fb", k_fb, [128, 32, 64], BF16)
            ovl, b_ovl = load_const("ovl", k_ovl, [128, 2, 65], F32)

            kcmp = [c.sb("kcmp", [128, 16 + T], BF16) for _ in range(2)]
            b_kcmp = [Buf(), Buf()]
            for i in range(2):
                c.op("pool", lambda e: e.memset(kcmp[i][:], 0.0), [], [b_kcmp[i]])
            kslc = c.sb("kslc", [128, T], BF16)
            kwin = c.sb("kwin", [128, T], BF16)
            b_kslc, b_kwin = Buf(), Buf()
            vslc = c.sb("vslc", [128, 32, 130], BF16)
            vwin = c.sb("vwin", [128, 32, 130], BF16)
            b_vslc, b_vwin = Buf(), Buf()
            c.op("pool", lambda e: e.memset(vslc[:, :, 128:130], 1.0), [], [b_vslc])
            c.op("pool", lambda e: e.memset(vwin[:, :, 128:130], 1.0), [], [b_vwin])
            kcT = c.sb("kcT", [128, 256], BF16)
            b_kcT = Buf()
            c.op("pool", lambda e: e.memset(kcT[:], 0.0), [], [b_kcT])
            vcx = c.sb("vcx", [128, 2, 194], BF16)
            b_vcx = Buf()
            c.op("pool", lambda e: e.memset(vcx[:], 0.0), [], [b_vcx])
            c.cp(vcx[:, :, 128:193], ovl[:], [b_ovl, b_vcx], [b_vcx])

            hT = c.sb("hTb", [128, 16, 512], BF16)
            b_hT = Buf()
            qT = c.sb("qT", [128, 4, 512], BF16)
            b_qT = Buf()
            gn = c.sb("gn", [128, 4, 6], F32)
            b_gn = [Buf() for _ in range(4)]
            hs = [c.sb("hs", [128, 2, 32], BF16) for _ in range(2)]
            b_hs = [Buf(), Buf()]
            peb = [c.sb("peb", [128, 2], F32) for _ in range(2)]
            b_peb = [Buf(), Buf()]
            NET = 5
            ET = [c.sb("ET", [128, 256], BF16) for _ in range(NET)]
            b_ET = [Buf() for _ in range(NET)]
            eti = [0]
            imp = c.sb("imp", [128, 64], F32)
            imp2 = c.sb("imp2", [128, 64], F32)
            mx = c.sb("mx", [128, 16], F32)
            b_imp, b_imp2, b_mx = Buf(), Buf(), Buf()
            selb = c.sb("selb", [128, 64], BF16)
            b_selb = Buf()
            selbT = c.sb("selbT", [64, 2, 128], BF16)
            b_selbT = Buf()
            oacc = c.sb("oacc", [128, 2, 128], F32)
            b_oacc = Buf()
            rsm = c.sb("rsm", [128, 8], F32)
            b_rsm = Buf()
            ynb = c.sb("ynb", [128, 2, 128], BF16)
            b_ynb = Buf()
            yTn = c.sb("yTn", [128, 2, 512], BF16)
            b_yTn = Buf()
            pout = [c.sb("pout", [128, 512], F32) for _ in range(2)]
            b_pout = [Buf(), Buf()]

            pproj = PSlots(c, "pprojB", 2, 1)
            pst = PSlots(c, "pst", 2, 1)
            pst_attn = PSlots(c, "pst_attn", 0, 1)
            pst_attn.slots = [pst.slots[0], pproj.slots[0], pst.slots[1], pproj.slots[1]]
            pacc = [c.ps("pacc", [128, 512]) for _ in range(4)]
            b_pacc = [Buf(excl=True) for _ in range(4)]

            for i in range(2):
                for half in range(2):
                    pp, b_pp = pproj.get()
                    for l in range(32):
                        c.mm(pp[:, 0:1], w1[i][:, l, half * 128:(half + 1) * 128], pe_[i][:, l:l + 1],
                             [b_w1[i], b_pe[i]], [b_pp], start=(l == 0), stop=(l == 31), inc=(l == 31))
                    c.cp(peb[i][:, half:half + 1], pp[:, 0:1], [b_pp], [b_peb[i]])

            for blk in range(8):
                c.dma("sp", hT[:], hT_d[:, :, blk * 512:(blk + 1) * 512], R=[b_hTd[blk]], W=[b_hT])
                for u in range(8):
                    pp, b_pp = pproj.get()
                    for kc in range(16):
                        c.mm(pp, wfm[:, kc, u * 128:(u + 1) * 128], hT[:, kc, :], [b_wfm, b_hT], [b_pp],
                             start=(kc == 0), stop=(kc == 15), inc=(kc == 15))
                    eng = "act" if u % 2 else "dve"
                    if u < 4:
                        c.cp(qT[:, u, :], pp, [b_pp], [b_qT], eng=eng)
                    elif u < 6:
                        c.cp(kcmp[u - 4][:, 16 + blk * 512:16 + (blk + 1) * 512], pp, [b_pp], [b_kcmp[u - 4]], eng=eng)
                    elif u == 6:
                        c.cp(kslc[:, blk * 512:(blk + 1) * 512], pp, [b_pp], [b_kslc], eng=eng)
                    else:
                        c.cp(kwin[:, blk * 512:(blk + 1) * 512], pp, [b_pp], [b_kwin], eng=eng)
                for tt in range(4):
                    ti = blk * 4 + tt
                    pp, b_pp = pproj.get()
                    for kc in range(16):
                        c.mm(pp[:, 0:262], hT[:, kc, tt * 128:(tt + 1) * 128], wtm[:, kc, :], [b_wtm, b_hT], [b_pp],
                             start=(kc == 0), stop=(kc == 15), inc=(kc == 15))
                    c.cp(vslc[:, ti, 0:128], pp[:, 0:128], [b_pp], [b_vslc], eng="act")
                    c.cp(vwin[:, ti, 0:128], pp[:, 128:256], [b_pp], [b_vwin])
                    c.act(gn[:, tt, :], pp[:, 256:262], AF.Sigmoid, [b_pp], [b_gn[tt]])
                m0 = 32 * blk
                for i in range(2):
                    for half in range(2):
                        pp, b_pp = pproj.get()
                        for l in range(32):
                            rhs = kcmp[i][:, 16 * m0 + l: 16 * m0 + l + 16 * 31 + 1: 16]
                            c.mm(pp[:, 0:32], w1[i][:, l, half * 128:(half + 1) * 128], rhs, [b_w1[i], b_kcmp[i]], [b_pp],
                                 start=(l == 0), stop=(l == 31), inc=(l == 31))
                        c.act(hs[i][:, half, :], pp[:, 0:32], AF.Silu, [b_pp, b_peb[i]], [b_hs[i]], bias=peb[i][:, half:half + 1], scale=1.0)
                pp, b_pp = pproj.get()
                for half in range(2):
                    c.mm(pp[:, 0:32], w2[0][:, half, :], hs[0][:, half, :], [b_w2[0], b_hs[0]], [b_pp], start=(half == 0), stop=(half == 1), inc=(half == 1))
                c.cp(kcT[:, m0:m0 + 32], pp[:, 0:32], [b_pp], [b_kcT])
                pp, b_pp = pproj.get()
                for half in range(2):
                    c.mm(pp[0:32, 0:128], hs[1][:, half, :], w2[1][:, half, :], [b_w2[1], b_hs[1]], [b_pp], start=(half == 0), stop=(half == 1), inc=(half == 1))
                mc_, mp = m0 // 128, m0 % 128
                c.cp(vcx[mp:mp + 32, mc_, 0:128], pp[0:32, 0:128], [b_pp], [b_vcx], eng="act")
                if blk == 0:
                    c.op("pool", lambda e: e.memset(vcx[0:1, 0, :], 0.0), [], [b_vcx])

                for tt in range(4):
                    qb = blk * 4 + tt
                    tsl = slice(tt * 128, (tt + 1) * 128)
                    pairs = []
                    SK = 3

                    def mk_s1(lhs_k, rb_k, ncol, rhs_q, extra, pre=None):
                        def s1(slot):
                            if pre is not None:
                                pre()
                            st, b_st = pst_attn.get()
                            c.mm(st[:, 0:ncol], lhs_k, rhs_q, rb_k + [b_qT], [b_st],
                                 start=True, stop=(len(extra) == 0), inc=(len(extra) == 0))
                            for xi, (o_, l_, r_, rb) in enumerate(extra):
                                last = xi == len(extra) - 1
                                c.mm(o_(st), l_, r_, rb, [b_st], start=False, stop=last, inc=last)
                            e = eti[0] % NET
                            eti[0] += 1
                            slot["e"] = e
                            c.act(ET[e][:, 0:ncol], st[:, 0:ncol], AF.Exp, [b_st], [b_ET[e]], scale=SCALE)
                        return s1

                    mcs = [0] + ([1] if qb >= 16 else [])

                    def fin_cmp():
                        for g in range(4):
                            pa = pacc[g]
                            c.ts(rsm[:, g:g + 1], pa[:, 192:193], 1e-30, None, ALU.max, None, [b_pacc[g]], [b_rsm])
                            c.op("dve", lambda e: e.reciprocal(out=rsm[:, g:g + 1], in_=rsm[:, g:g + 1]), [b_rsm], [b_rsm])
                            c.stt(imp[:], pa[:, 128:192], rsm[:, g:g + 1], (fb[:, qb, :] if g == 0 else imp[:]), ALU.mult, ALU.add,
                                  [b_pacc[g], b_rsm, b_fb, b_imp], [b_imp])
                            if g < 2:
                                c.tt(rsm[:, 4 + g:5 + g], rsm[:, g:g + 1], gn[:, tt, g:g + 1], ALU.mult, [b_rsm, b_gn[tt]], [b_rsm])
                                c.ts(oacc[:, g, :], pa[:, 0:128], rsm[:, 4 + g:5 + g], None, ALU.mult, None, [b_pacc[g], b_rsm], [b_oacc])
                        c.op("dve", lambda e: e.max(out=mx[:, 0:8], in_=imp[:]), [b_imp], [b_mx])
                        c.op("dve", lambda e: e.match_replace(out=imp2[:], in_to_replace=mx[:, 0:8], in_values=imp[:], imm_value=-3.0e4), [b_imp, b_mx], [b_imp2])
                        c.op("dve", lambda e: e.max(out=mx[:, 8:16], in_=imp2[:]), [b_imp2], [b_mx])
                        c.ts(selb[:], imp[:], mx[:, 15:16], NEG, ALU.is_lt, ALU.mult, [b_imp, b_mx], [b_selb])

                    for k, mc in enumerate(mcs):
                        delta = qb - 16 * mc
                        di = min(delta, 16) if mc == 0 else delta
                        for hf in range(2):
                            extra = [((lambda st, gg=gg: st[:, gg * 128:(gg + 1) * 128]), identb[:], cb0[:, di, :], [b_identb, b_cb0]) for gg in range(2)]
                            s1 = mk_s1(kcT[:, mc * 128:(mc + 1) * 128], [b_kcT], 256, qT[:, 2 * hf:2 * hf + 2, tsl], extra)

                            def s2(slot, k=k, mc=mc, hf=hf):
                                e = slot["e"]
                                lastk = k == len(mcs) - 1
                                for gg in range(2):
                                    g = 2 * hf + gg
                                    c.mm(pacc[g][:, 0:193], ET[e][:, gg * 128:(gg + 1) * 128], vcx[:, mc, 0:193], [b_ET[e], b_vcx], [b_pacc[g]],
                                         start=(k == 0), stop=lastk)
                                if lastk and hf == 1:
                                    fin_cmp()
                            pairs.append((s1, s2))
                    i_cmp_last = len(pairs) - 1

                    def b4_pe():
                        st, b_st = pst.get()
                        stb = st.bitcast(BF16)
                        c.tr(stb[0:64, 0:128], selb[:], identb[:], [b_selb, b_identb], [b_st])
                        c.cp(selbT[:, 0, :], stb[0:64, 0:128], [b_st], [b_selbT])
                        c.cp(selbT[:, 1, :], stb[0:64, 0:128], [b_st], [b_selbT])

                    for br in (1, 0):
                        if br == 0:
                            kcs = list(range(0, qb + 1))
                            KT_, b_KT, V_, b_V = kslc, b_kslc, vslc, b_vslc
                            pg0 = 0
                        else:
                            kcs = list(range(max(0, qb - 4), qb + 1))
                            KT_, b_KT, V_, b_V = kwin, b_kwin, vwin, b_vwin
                            pg0 = 2

                        def fin_br(br=br, pg0=pg0):
                            for g in range(2):
                                pa, bpa = pacc[pg0 + g], b_pacc[pg0 + g]
                                c.op("dve", lambda e: e.reciprocal(out=rsm[:, g:g + 1], in_=pa[:, 128:129]), [bpa], [b_rsm])
                                gi = (1 + br) * 2 + g
                                c.tt(rsm[:, 4 + g:5 + g], rsm[:, g:g + 1], gn[:, tt, gi:gi + 1], ALU.mult, [b_rsm, b_gn[tt]], [b_rsm])
                                c.stt(oacc[:, g, :], pa[:, 0:128], rsm[:, 4 + g:5 + g], oacc[:, g, :], ALU.mult, ALU.add,
                                      [bpa, b_rsm, b_oacc], [b_oacc])

                        if br == 0:
                            while len(pairs) < i_cmp_last + SK + 1:
                                pairs.append((lambda slot: None, lambda slot: None))
                        for k, kc in enumerate(kcs):
                            extra = []
                            full = (lambda st: st[:, 0:256])
                            if br == 0:
                                extra.append((full, Ec[:, kc, :], selbT[:].rearrange("p a b -> p (a b)"), [b_E, b_selbT]))
                            if kc == qb:
                                extra.append((full, identb[:], triU[:], [b_identb, b_triU]))
                            if br == 1 and kc == qb - 4:
                                extra.append((full, identb[:], triL[:], [b_identb, b_triL]))
                            s1 = mk_s1(KT_[:, kc * 128:(kc + 1) * 128], [b_KT], 256, qT[:, 0:2, tsl], extra,
                                       pre=(b4_pe if (br == 0 and k == 0) else None))

                            def s2(slot, k=k, kc=kc, n=len(kcs), V_=V_, b_V=b_V, pg0=pg0, fin=fin_br):
                                e = slot["e"]
                                for g in range(2):
                                    c.mm(pacc[pg0 + g][:, 0:129], ET[e][:, g * 128:(g + 1) * 128], V_[:, kc, 0:129], [b_ET[e], b_V], [b_pacc[pg0 + g]],
                                         start=(k == 0), stop=(k == n - 1))
                                if k == n - 1:
                                    fin()
                            pairs.append((s1, s2))

                    slots = [dict() for _ in pairs]
                    for i in range(len(pairs) + SK):
                        if i < len(pairs):
                            pairs[i][0](slots[i])
                        if i >= SK:
                            pairs[i - SK][1](slots[i - SK])
                    c.cp(ynb[:], oacc[:], [b_oacc], [b_ynb], eng="act")
                    for g in range(2):
                        st, b_st = pst.get()
                        stb = st.bitcast(BF16)
                        c.tr(stb[:, 0:128], ynb[:, g, :], identb[:], [b_ynb, b_identb], [b_st])
                        c.cp(yTn[:, g, tsl], stb[:, 0:128], [b_st], [b_yTn])
                if debug:
                    bd = Buf(); dbg_bufs.append(bd)
                    c.dma("sp", yTn_d[:, :, blk * 512:(blk + 1) * 512], yTn[:], R=[b_yTn], W=[bd])
                for tt in range(4):
                    ti = blk * 4 + tt
                    for cbk in range(4):
                        po, b_po = pout[cbk % 2], b_pout[cbk % 2]
                        pp, b_pp = pproj.get()
                        for fc in range(4):
                            lhs = yTg[:, fc, ti * 128:(ti + 1) * 128] if fc < 2 else yTn[:, fc - 2, tt * 128:(tt + 1) * 128]
                            rb = [b_yTg[blk]] if fc < 2 else [b_yTn]
                            c.mm(pp, lhs, wo[:, fc, cbk * 512:(cbk + 1) * 512], rb + [b_wo], [b_pp], start=(fc == 0), stop=(fc == 3), inc=(fc == 3))
                        c.cp(po[:], pp, [b_pp], [b_po], eng=("act" if cbk % 2 else "dve"))
                        b_part = Buf()
                        b_parts.append(b_part)
                        c.dma("sp", partial.ap()[ti * 128:(ti + 1) * 128, cbk * 512:(cbk + 1) * 512], po[:], R=[b_po], W=[b_part])
        c.barrier()
        c.es = es0
        if stop == "B":
            c.drain()
            return nc
        if debug:
            bd = Buf(); dbg_bufs.append(bd)
            c.dma("sp", part_d, partial.ap(), R=b_parts, W=[bd])
        b_rs = Buf()
        b_rs0 = Buf()
        c.op("pool", lambda e: e.collective_compute("ReduceScatter", ALU.add, replica_groups=[[0, 1, 2, 3], [4, 5, 6, 7]],
                                                    ins=[partial.ap().opt()], outs=[rs_out.ap().opt()]), b_parts, [b_rs0])
        c.op("pool", lambda e: e.collective_compute("AllReduce", ALU.add, replica_groups=[[0, 1, 2, 3], [4, 5, 6, 7]],
                                                    ins=[fence_in.ap().opt()], outs=[fence_out.ap().opt()]), [b_rs0], [b_rs])

        ada_part(32, 96, True)
        if stop == "rs":
            c.drain()
            return nc
        with ExitStack() as es:
            c.es = es
            h2T = c.sb("h2T", [128, 16, 1024], BF16)
            b_h2T = Buf()
            acc = c.sb("acc", [128, 8, D], F32)
            b_acc = [Buf() for _ in range(8)]
            comb = c.sb("comb", [128, 8, 65], F32)
            b_comb = Buf()
            ptr = PSlots(c, "ptr2", 1, 1)
            pgu = PSlots(c, "pgu", 4, 1)
            pd = PSlots(c, "pd", 3, 1)
            b_x1d = [Buf() for _ in range(8)]
            with ExitStack() as es2:
                c.es = es2
                ga = c.sb("ga", [128, D], F32)
                b_ga = Buf()
                c.dma("sp", ga[:], modD[32:48, :].rearrange("a b -> (a b)").partition_broadcast(128), R=[b_modD], W=[b_ga])
                rwt = c.sb("rwt", [128, 16, 64], BF16)
                b_rwt = Buf()
                c.dma("pool", rwt[:], rw.rearrange("(kc p) n -> p kc n", p=128), W=[b_rwt])
                rbr, b_rbr = load_const("rbr", rbias, [128, 64], F32)
                xts = [c.sb("xt2", [128, D], F32) for _ in range(2)]
                b_xts = [Buf(), Buf()]
                ats = [c.sb("at2", [128, D], F32) for _ in range(2)]
                b_ats = [Buf(), Buf()]
                ntmp = norm_tmp()
                sc = c.sb("sc", [128, 64], F32)
                sv = c.sb("sv", [128, 64], F32)
                mx8 = c.sb("mx8", [128, 8], F32)
                den = c.sb("den", [128, 1], F32)
                b_sc, b_sv, b_mx8, b_den = Buf(), Buf(), Buf(), Buf()
                c.op("pool", lambda e: e.memset(comb[:, :, 64:65], 1.0), [], [b_comb])
                for tt in range(8):
                    xt, b_xt = xts[tt % 2], b_xts[tt % 2]
                    at, b_at = ats[tt % 2], b_ats[tt % 2]
                    c.dma("sp", xt[:], xq[tt * 128:(tt + 1) * 128, :], W=[b_xt])
                    c.dma("sp", at[:], rs_out.ap()[tt * 128:(tt + 1) * 128, :], R=[b_rs], W=[b_at])
                    c.tt(at[:], at[:], ga[:], ALU.mult, [b_at, b_ga], [b_at])
                    c.tt(xt[:], xt[:], at[:], ALU.add, [b_xt, b_at], [b_xt])
                    c.dma("sp", x1_d[tt * 128:(tt + 1) * 128, :], xt[:], R=[b_xt], W=[b_x1d[tt]])
                    norm_tile(xt, b_xt, A2, B2, h2T, b_h2T, tt * 128, ntmp, ptr)
                    pp, b_pp = pd.get()
                    for kc in range(16):
                        c.mm(pp[:, 0:64], h2T[:, kc, tt * 128:(tt + 1) * 128], rwt[:, kc, :], [b_h2T, b_rwt], [b_pp],
                             start=(kc == 0), stop=(kc == 15), inc=(kc == 15))
                    c.act(sc[:], pp[:, 0:64], AF.Sigmoid, [b_pp], [b_sc])
                    c.tt(sv[:], sc[:], rbr[:], ALU.add, [b_sc, b_rbr], [b_sv])
                    c.op("dve", lambda e: e.max(out=mx8[:], in_=sv[:]), [b_sv], [b_mx8])
                    c.ts(sv[:], sv[:], mx8[:, 7:8], None, ALU.is_ge, None, [b_sv, b_mx8], [b_sv])
                    c.tt(sv[:], sv[:], sc[:], ALU.mult, [b_sv, b_sc], [b_sv])
                    c.op("dve", lambda e: e.reduce_sum(out=den[:], in_=sv[:], axis=mybir.AxisListType.X), [b_sv], [b_den])
                    c.op("dve", lambda e: e.reciprocal(out=den[:], in_=den[:]), [b_den], [b_den])
                    c.ts(comb[:, tt, 0:64], sv[:], den[:, 0:1], 2.5, ALU.mult, ALU.mult, [b_sv, b_den, b_comb], [b_comb])
            c.barrier()
            pd.slots.append(ptr.slots[0])
            es3 = ExitStack()
            es3.__enter__()
            c.es = es3
            wgu = [[c.sb("wg", [128, 16, 128], BF16), c.sb("wu", [128, 16, 128], BF16)] for _ in range(4)]
            b_wgu = [[Buf(), Buf()] for _ in range(4)]
            wdt = [c.sb("wd", [128, 4, D], BF16) for _ in range(2)]
            b_wdt = [Buf(), Buf()]
            actT = [c.sb("actT", [128, 4, 1024], BF16) for _ in range(2)]
            b_actT = [Buf(), Buf()]
            sgt = [c.sb("sgt", [128, 512], BF16) for _ in range(2)]
            b_sgt = [Buf(), Buf()]
            ui = 0
            for e_ in list(range(n_exp)) + [64]:
                if e_ < 64:
                    sg_, su_, sd_ = ewg[e_], ewu[e_], ewd[e_]
                else:
                    sg_, su_, sd_ = swg, swu, swd
                wd_t, b_wd = wdt[e_ % 2], b_wdt[e_ % 2]
                for half in range(2):
                    c.dma("pool", wd_t[:, half * 2:(half + 1) * 2, :], sd_[half * 256:(half + 1) * 256, :].rearrange("(a p) n -> p a n", p=128), W=[b_wd])
                aT, b_aT = actT[e_ % 2], b_actT[e_ % 2]
                for fc in range(4):
                    (wg_t, wu_t), (b_wg, b_wu) = wgu[ui % 4], b_wgu[ui % 4]
                    ui += 1
                    c.dma("pool", wg_t[:], sg_[:, fc * 128:(fc + 1) * 128].rearrange("(kc p) n -> p kc n", p=128), W=[b_wg])
                    c.dma("pool", wu_t[:], su_[:, fc * 128:(fc + 1) * 128].rearrange("(kc p) n -> p kc n", p=128), W=[b_wu])
                    for tb in range(2):
                        pG, b_pG = pgu.get()
                        pU, b_pU = pgu.get()
                        for kc in range(16):
                            c.mm(pG, wg_t[:, kc, :], h2T[:, kc, tb * 512:(tb + 1) * 512], [b_wg, b_h2T], [b_pG], start=(kc == 0), stop=(kc == 15), inc=(kc == 15))
                        for kc in range(16):
                            c.mm(pU, wu_t[:, kc, :], h2T[:, kc, tb * 512:(tb + 1) * 512], [b_wu, b_h2T], [b_pU], start=(kc == 0), stop=(kc == 15), inc=(kc == 15))
                        k2 = (fc * 2 + tb) % 2
                        c.act(sgt[k2][:], pG, AF.Silu, [b_pG], [b_sgt[k2]])
                        c.tt(aT[:, fc, tb * 512:(tb + 1) * 512], sgt[k2][:], pU, ALU.mult, [b_sgt[k2], b_pU], [b_aT])
                for tt in range(8):
                    for cbk in range(4):
                        pD, b_pD = pd.get()
                        for fc in range(4):
                            c.mm(pD, aT[:, fc, tt * 128:(tt + 1) * 128], wd_t[:, fc, cbk * 512:(cbk + 1) * 512], [b_aT, b_wd], [b_pD],
                                 start=(fc == 0), stop=(fc == 3), inc=(fc == 3))
                        dst = acc[:, tt, cbk * 512:(cbk + 1) * 512]
                        if e_ == (0 if n_exp else 64):
                            c.ts(dst, pD, comb[:, tt, e_:e_ + 1], None, ALU.mult, None, [b_pD, b_comb], [b_acc[tt]])
                        else:
                            c.stt(dst, pD, comb[:, tt, e_:e_ + 1], dst, ALU.mult, ALU.add, [b_pD, b_comb, b_acc[tt]], [b_acc[tt]])
            es3.__exit__(None, None, None)
            c.barrier()
            c.es = es
            gm = c.sb("gm", [128, D], F32)
            b_gm = Buf()
            c.dma("sp", gm[:], modD[80:96, :].rearrange("a b -> (a b)").partition_broadcast(128), R=[b_modD], W=[b_gm])
            nf = c.sb("nf", [128, D], F32)
            b_nf = Buf()
            c.dma("sp", nf[:], nrm_f.partition_broadcast(128), W=[b_nf])
            xts = [c.sb("xt3", [128, D], F32) for _ in range(2)]
            b_xts = [Buf(), Buf()]
            sq3 = c.sb("sq3", [128, D], BF16)
            ss3 = c.sb("ss3", [128, 1], F32)
            b_sq3, b_ss3 = Buf(), Buf()
            b_out = []
            for tt in range(8):
                xt, b_xt = xts[tt % 2], b_xts[tt % 2]
                c.dma("sp", xt[:], x1_d[tt * 128:(tt + 1) * 128, :], R=[b_x1d[tt]], W=[b_xt])
                z = acc[:, tt, :]
                c.tt(z, z, gm[:], ALU.mult, [b_acc[tt], b_gm], [b_acc[tt]])
                c.tt(z, z, xt[:], ALU.add, [b_acc[tt], b_xt], [b_acc[tt]])
                c.act(sq3[:], z, AF.Square, [b_acc[tt]], [b_sq3, b_ss3], accum_out=ss3[:])
                c.ts(ss3[:], ss3[:], 1.0 / D, EPS, ALU.mult, ALU.add, [b_ss3], [b_ss3])
                c.act(ss3[:], ss3[:], AF.Sqrt, [b_ss3], [b_ss3])
                c.op("dve", lambda e: e.reciprocal(out=ss3[:], in_=ss3[:]), [b_ss3], [b_ss3])
                c.stt(xt[:], z, ss3[:, 0:1], nf[:], ALU.mult, ALU.mult, [b_acc[tt], b_ss3, b_nf, b_xt], [b_xt])
                bo = Buf()
                b_out.append(bo)
                c.dma("sp", y[tt * 128:(tt + 1) * 128, :], xt[:], R=[b_xt], W=[bo])
            c.finish(b_out + dbg_bufs)
        c.barrier()
        c.es = es0
        return nc


_NC = None


def _consts():
    bf = ml_dtypes.bfloat16
    p = np.arange(128)
    k = {}
    k["k_identf"] = np.eye(128, dtype=np.float32)
    k["k_identb"] = np.eye(128).astype(bf)
    k["k_U"] = (p[:, None] <= p[None, :]).astype(np.float32)
    k["k_SL"] = (p[:, None] > p[None, :]).astype(np.float32)
    k["k_ILT"] = (p[None, :] >= p[:, None]).astype(np.float32)
    k["k_ones"] = np.ones((128, 128), np.float32)
    triU = np.where(p[:, None] <= p[None, :], 0.0, NEG).astype(np.float32)
    triL = np.where(p[:, None] > p[None, :], 0.0, NEG).astype(np.float32)
    k["k_triU"] = np.concatenate([triU, triU], axis=1).astype(bf)
    k["k_triL"] = np.concatenate([triL, triL], axis=1).astype(bf)
    E = np.zeros((64, 32, 128), np.float32)
    for kc in range(32):
        for pp in range(128):
            E[2 * kc + pp // 64, kc, pp] = 1.0
    k["k_E"] = E.astype(bf)
    ov = np.zeros((256, 65), np.float32)
    for m in range(1, 256):
        n = m - 1
        cs = n * 16
        for j in range(64):
            if cs < j * 64 + 64 and cs + 32 > j * 64:
                ov[m, j] = 1.0
        ov[m, 64] = 1.0
    k["k_ovl"] = np.ascontiguousarray(ov.reshape(2, 128, 65).transpose(1, 0, 2))
    cb0 = np.zeros((128, 17, 128), np.float32)
    col = np.arange(128)
    for d in range(16):
        valid = (16 * p[:, None] + 15) <= (128 * d + col[None, :])
        cb0[:, d, :] = np.where(valid, 0.0, NEG)
    k["k_cb0"] = cb0.astype(bf)
    fb = np.zeros((128, 32, 64), np.float32)
    for qb in range(32):
        for t in range(128):
            cur = 2 * qb + t // 64
            for j in range(64):
                if j == 0 or j == cur or j == cur - 1:
                    fb[t, qb, j] = 1.0e4
                elif j > cur:
                    fb[t, qb, j] = -1.0e4
    k["k_fb"] = fb.astype(bf)
    return k


def _prep(inp):
    f = lambda a: np.ascontiguousarray(np.asarray(a, dtype=np.float32))
    x = f(inp["x"]); cc = f(inp["c"])
    w_in = f(inp["w_in"])[0]
    conv = f(inp["gdn_conv_w"])[0]
    wo_full = f(inp["w_out"])[0]
    consts = _consts()
    shared = {
        "w_ada": f(inp["w_ada"])[0], "b_ada": f(inp["b_ada"])[0],
        "nrm_a": np.ascontiguousarray(f(inp["norm_attn_w"])[0].reshape(16, 128).T),
        "nrm_m": np.ascontiguousarray(f(inp["norm_ffn_w"])[0].reshape(16, 128).T),
        "nrm_f": f(inp["norm_final_w"]),
        "gnw": np.ascontiguousarray(np.broadcast_to(f(inp["gdn_norm_w"])[0][None, :], (128, 128))),
        "w1k": f(inp["cmp_w1_k"])[0], "w1v": f(inp["cmp_w1_v"])[0],
        "w2k": f(inp["cmp_w2_k"])[0], "w2v": f(inp["cmp_w2_v"])[0],
        "pek": np.ascontiguousarray(f(inp["cmp_pe_k"])[0].T), "pev": np.ascontiguousarray(f(inp["cmp_pe_v"])[0].T),
        "rw": f(inp["router_w"])[0],
        "rbias": np.ascontiguousarray(np.broadcast_to(f(inp["router_bias"])[0][None, :], (128, 64))),
        "ewg": f(inp["expert_w_gate"])[0], "ewu": f(inp["expert_w_up"])[0], "ewd": f(inp["expert_w_down"])[0],
        "swg": f(inp["shared_w_gate"])[0], "swu": f(inp["shared_w_up"])[0], "swd": f(inp["shared_w_down"])[0],
    }
    shared.update(consts)
    maps = []
    for core in range(8):
        b, hg = core // 4, core % 4
        kvh = hg // 2
        g = [2 * hg, 2 * hg + 1]
        m = dict(shared)
        m["xb"] = x[b]
        m["xq"] = np.ascontiguousarray(x[b, hg * 1024:(hg + 1) * 1024])
        m["cvec"] = np.ascontiguousarray(cc[b].reshape(16, 128).T)
        cols = []
        for part in range(3):
            for h in g:
                cols += list(range(part * 1024 + h * 128, part * 1024 + h * 128 + 128))
        m["wA_fm"] = np.ascontiguousarray(w_in[:, cols])
        m["convw"] = np.ascontiguousarray(conv[:, cols].reshape(4, 6, 128).transpose(2, 1, 0))
        tcols = []
        for h in g:
            tcols += list(range(3088 + h * 128, 3088 + h * 128 + 128))
        tcols += [3072 + g[0], 3072 + g[1], 3080 + g[0], 3080 + g[1]]
        m["wA_tm"] = np.ascontiguousarray(w_in[:, tcols])
        al = f(inp["gdn_a_log"])[0][g]; db = f(inp["gdn_dt_bias"])[0][g]
        m["alog"] = np.ascontiguousarray(np.broadcast_to(al[None, :], (128, 2)))
        m["dtb"] = np.ascontiguousarray(np.broadcast_to(db[None, :], (128, 2)))
        grp = [4 * kvh + i for i in range(4)]
        qh = g + [h for h in grp if h not in g]
        bcols = []
        for h in qh:
            bcols += list(range(4112 + h * 128, 4112 + h * 128 + 128))
        for part in (0, 1, 2, 4):
            bcols += list(range(5136 + part * 256 + kvh * 128, 5136 + part * 256 + kvh * 128 + 128))
        m["wB_fm"] = np.ascontiguousarray(w_in[:, bcols])
        btc = []
        for part in (3, 5):
            btc += list(range(5136 + part * 256 + kvh * 128, 5136 + part * 256 + kvh * 128 + 128))
        for br in range(3):
            for h in g:
                btc.append(6672 + br * 8 + h)
        m["wB_tm"] = np.ascontiguousarray(w_in[:, btc])
        rows = []
        for h in g:
            rows += list(range(h * 128, h * 128 + 128))
        for h in g:
            rows += list(range(1024 + h * 128, 1024 + h * 128 + 128))
        m["wout"] = np.ascontiguousarray(wo_full[rows, :])
        maps.append(m)
    return maps


def kernel(**inputs):
    global _NC
    maps = _prep(inputs)
    if _NC is None:
        _NC = build()
    res = run_bass_kernel_spmd(_NC, maps, core_ids=list(range(8)))
    out = np.zeros((2, T, D), np.float32)
    for core in range(8):
        b, q = core // 4, core % 4
        out[b, q * 1024:(q + 1) * 1024] = res.results[core]["y"]
    return out
```

```python
import numpy as np
from contextlib import ExitStack
import ml_dtypes
import concourse.bass as bass
import concourse.mybir as mybir
from concourse.bass_utils import run_bass_kernel_spmd

F32 = mybir.dt.float32
F32R = mybir.dt.float32r
BF16 = mybir.dt.bfloat16
AF = mybir.ActivationFunctionType
ALU = mybir.AluOpType

D = 2048
T = 4096
NEG = -10000.0
EPS = 1e-6
SCALE = 128 ** -0.5


class Buf:
    __slots__ = ("w", "r", "name", "excl")

    def __init__(self, name="", excl=False):
        self.w = None
        self.r = {}
        self.name = name
        self.excl = excl


class Ctx:
    NS = 6

    def __init__(self, nc, es):
        self.nc = nc
        self.es = es
        self.E = {"pe": nc.tensor, "dve": nc.vector, "act": nc.scalar, "pool": nc.gpsimd, "sp": nc.sync}
        self.sem = {}
        self.cnt = {}
        for k in self.E:
            self.sem[k] = es.enter_context(nc.semaphore("s_" + k))
            self.cnt[k] = 0
        self.dq = {}
        for q in ("sp", "pool"):
            sl = []
            for i in range(self.NS):
                key = "d_%s%d" % (q, i)
                self.sem[key] = es.enter_context(nc.semaphore(key))
                self.cnt[key] = 0
                sl.append(key)
            self.dq[q] = [sl, 0]
        self.sem["d_coll"] = es.enter_context(nc.semaphore("d_coll"))
        self.cnt["d_coll"] = 0
        self.seen = {k: {} for k in self.E}
        self.uid = 0

    def sb(self, name, shape, dt):
        self.uid += 1
        return self.es.enter_context(self.nc.sbuf_tensor("%s_%d" % (name, self.uid), list(shape), dt))

    def ps(self, name, shape, dt=F32):
        self.uid += 1
        return self.es.enter_context(self.nc.psum_tensor("%s_%d" % (name, self.uid), list(shape), dt))

    def _wait(self, eng, tok):
        if tok is None:
            return
        key, val = tok
        if key == eng and eng == "pe":
            return
        if self.seen[eng].get(key, 0) >= val:
            return
        self.seen[eng][key] = val
        self.E[eng].wait_ge(self.sem[key], val)

    def _deps(self, eng, reads, writes):
        for b in reads:
            self._wait(eng, b.w)
            if b.excl:
                for t in list(b.r.items()):
                    if t[0] != eng:
                        self._wait(eng, t)
        for b in writes:
            self._wait(eng, b.w)
            for t in list(b.r.items()):
                self._wait(eng, t)

    def _mark(self, tok, reads, writes):
        for b in reads:
            if b.r.get(tok[0], 0) < tok[1]:
                b.r[tok[0]] = tok[1]
        for b in writes:
            b.w = tok
            b.r = {}

    def op(self, eng, fn, R=(), W=(), inc=True):
        self._deps(eng, R, W)
        ins = fn(self.E[eng])
        if inc:
            self.cnt[eng] += 1
            ins.then_inc(self.sem[eng], 1)
            tok = (eng, self.cnt[eng])
        else:
            tok = (eng, self.cnt[eng] + 1)
        self._mark(tok, R, W)
        return ins

    def dma(self, q, out, in_, R=(), W=(), **kw):
        sl, k = self.dq[q]
        key = sl[k % self.NS]
        self.dq[q][1] = k + 1
        if self.cnt[key]:
            self._wait(q, (key, self.cnt[key]))
        self._deps(q, R, W)
        ins = self.E[q].dma_start(out=out, in_=in_, **kw)
        self.cnt[key] += 16
        ins.then_inc(self.sem[key], 16)
        tok = (key, self.cnt[key])
        self._mark(tok, R, W)
        return tok

    def coll(self, fn, R=(), W=()):
        self._deps("pool", R, W)
        ins = fn(self.E["pool"])
        self.cnt["d_coll"] += 16
        ins.then_inc(self.sem["d_coll"], 16)
        tok = ("d_coll", self.cnt["d_coll"])
        self._mark(tok, R, W)
        return ins

    def barrier(self):
        for e in self.E:
            for k, v in self.cnt.items():
                if v and k != e:
                    self._wait(e, (k, v))

    def drain(self):
        for k, v in self.cnt.items():
            if v:
                self._wait("sp", (k, v))

    def finish(self, bufs):
        for b in bufs:
            self._wait("sp", b.w)

    def mm(self, out, lhsT, rhs, R, W, start=True, stop=True, inc=True):
        return self.op("pe", lambda e: e.matmul(out, lhsT, rhs, start=start, stop=stop), R, W, inc=inc)

    def tr(self, out, in_, ident, R, W):
        return self.op("pe", lambda e: e.transpose(out, in_, ident), R, W)

    def act(self, out, in_, func, R, W, **kw):
        return self.op("act", lambda e: e.activation(out=out, in_=in_, func=func, **kw), R, W)

    def tt(self, out, in0, in1, op, R, W, eng="dve"):
        return self.op(eng, lambda e: e.tensor_tensor(out=out, in0=in0, in1=in1, op=op), R, W)

    def ts(self, out, in0, s1, s2, op0, op1, R, W, eng="dve"):
        if op1 is None:
            return self.op(eng, lambda e: e.tensor_scalar(out=out, in0=in0, scalar1=s1, scalar2=None, op0=op0), R, W)
        return self.op(eng, lambda e: e.tensor_scalar(out=out, in0=in0, scalar1=s1, scalar2=s2, op0=op0, op1=op1), R, W)

    def stt(self, out, in0, scalar, in1, op0, op1, R, W):
        return self.op("dve", lambda e: e.scalar_tensor_tensor(out=out, in0=in0, scalar=scalar, in1=in1, op0=op0, op1=op1), R, W)

    def cp(self, out, in_, R, W, eng="dve"):
        if eng == "act":
            return self.op("act", lambda e: e.copy(out=out, in_=in_), R, W)
        return self.op(eng, lambda e: e.tensor_copy(out=out, in_=in_), R, W)


class PSlots:
    def __init__(self, c, name, nbanks, per, width=None):
        self.slots = []
        w = width or 512 // per
        for b in range(nbanks):
            t = c.ps(name, [128, 512])
            bb = Buf(excl=True)
            for s in range(per):
                self.slots.append((t[:, s * w:(s + 1) * w], bb))
        self.i = 0

    def get(self):
        s = self.slots[self.i % len(self.slots)]
        self.i += 1
        return s


def r32(ap):
    return ap


def build(debug=False, n_exp=64, stop=None):
    nc = bass.Bass("TRN2", target_bir_lowering=False)

    def din(name, shape, dt=F32):
        return nc.dram_tensor(name, list(shape), dt, kind="ExternalInput").ap()

    xb = din("xb", [T, D])
    xq = din("xq", [1024, D])
    cvec = din("cvec", [128, 16])
    w_ada = din("w_ada", [D, 6 * D])
    b_ada = din("b_ada", [6 * D])
    nrm_a = din("nrm_a", [128, 16])
    nrm_m = din("nrm_m", [128, 16])
    nrm_f = din("nrm_f", [D])
    wA_fm = din("wA_fm", [D, 768])
    wA_tm = din("wA_tm", [D, 260])
    convw = din("convw", [128, 6, 4])
    alog = din("alog", [128, 2])
    dtb = din("dtb", [128, 2])
    gnw = din("gnw", [128, 128])
    wB_fm = din("wB_fm", [D, 1024])
    wB_tm = din("wB_tm", [D, 262])
    w1k = din("w1k", [4096, 256])
    w1v = din("w1v", [4096, 256])
    w2k = din("w2k", [256, 128])
    w2v = din("w2v", [256, 128])
    pek = din("pek", [128, 32])
    pev = din("pev", [128, 32])
    wout = din("wout", [512, D])
    rw = din("rw", [D, 64])
    rbias = din("rbias", [128, 64])
    if n_exp:
        ewg = din("ewg", [64, D, 512])
        ewu = din("ewu", [64, D, 512])
        ewd = din("ewd", [64, 512, D])
    swg = din("swg", [D, 512])
    swu = din("swu", [D, 512])
    swd = din("swd", [512, D])
    k_identf = din("k_identf", [128, 128])
    k_identb = din("k_identb", [128, 128], BF16)
    k_U = din("k_U", [128, 128])
    k_SL = din("k_SL", [128, 128])
    k_ILT = din("k_ILT", [128, 128])
    k_ones = din("k_ones", [128, 128])
    k_triU = din("k_triU", [128, 256], BF16)
    k_triL = din("k_triL", [128, 256], BF16)
    k_E = din("k_E", [64, 32, 128], BF16)
    k_ovl = din("k_ovl", [128, 2, 65])
    k_cb0 = din("k_cb0", [128, 17, 128], BF16)
    k_fb = din("k_fb", [128, 32, 64], BF16)

    y = nc.dram_tensor("y", [1024, D], F32, kind="ExternalOutput").ap()

    dk = dict(kind="ExternalOutput") if debug else {}
    hT_d = nc.dram_tensor("hT_d", [128, 16, T], BF16, **dk).ap()
    modD = nc.dram_tensor("modD", [96, 128], F32, **dk).ap()
    partial = nc.dram_tensor("partial", [T, D], F32)
    rs_out = nc.dram_tensor("rs_out", [1024, D], F32)
    fence_in = nc.dram_tensor("fence_in", [128, 128], F32)
    fence_out = nc.dram_tensor("fence_out", [128, 128], F32)
    x1_d = nc.dram_tensor("x1_d", [1024, D], F32, **dk).ap()
    if debug:
        yTg_d = nc.dram_tensor("yTg_d", [128, 2, T], BF16, kind="ExternalOutput").ap()
        yTn_d = nc.dram_tensor("yTn_d", [128, 2, T], BF16, kind="ExternalOutput").ap()
        part_d = nc.dram_tensor("part_d", [T, D], F32, kind="ExternalOutput").ap()

    with ExitStack() as es0:
        c = Ctx(nc, es0)

        def load_const(name, src, shape, dt, q="sp"):
            t = c.sb(name, shape, dt)
            b = Buf(name)
            c.dma(q, t[:], src, W=[b])
            return t, b

        identf, b_identf = load_const("identf", k_identf, [128, 128], F32)
        identb, b_identb = load_const("identb", k_identb, [128, 128], BF16)
        nrmA, b_nrmA = load_const("nrmA", nrm_a, [128, 16], F32)
        nrmM, b_nrmM = load_const("nrmM", nrm_m, [128, 16], F32)
        modT = c.sb("modT", [128, 96], F32)
        b_modT = Buf()
        A1 = c.sb("A1", [128, 16], F32)
        A2 = c.sb("A2", [128, 16], F32)
        b_A = Buf()

        b_modD = Buf()

        def ada_part(j0, j1, last):
            with ExitStack() as es:
                c.es = es
                cv = c.sb("cv", [128, 16], F32)
                b_cv = Buf()
                c.dma("sp", cv[:], cvec, W=[b_cv])
                cond = c.sb("cond", [128, 16, 2], F32)
                b_cond = Buf()
                for j in range(2):
                    c.act(cond[:, :, j], cv[:], AF.Silu, [b_cv], [b_cond])
                wt = [c.sb("wada", [128, 16, 512], F32) for _ in range(2)]
                bw = [Buf(), Buf()]
                prow = [c.ps("prow", [128, 512]) for _ in range(2)]
                b_prow = [Buf(excl=True), Buf(excl=True)]
                pmT = c.ps("pmT", [128, 512])
                b_pmT = Buf(excl=True)
                rowsb = [c.sb("rowsb", [2, 512], F32) for _ in range(2)]
                b_rowsb = [Buf(), Buf()]
                bad = c.sb("bad", [128, 96], F32)
                b_bad = Buf()
                c.dma("sp", bad[:], b_ada.rearrange("(j p) -> p j", p=128), W=[b_bad], allow_slow_non_contiguous=True)
                for blk in range(j0 // 4, j1 // 4):
                    k2 = blk % 2
                    c.dma("sp", wt[k2][:], w_ada[:, blk * 512:(blk + 1) * 512].rearrange("(kc p) n -> p kc n", p=128), W=[bw[k2]])
                    for kc in range(16):
                        c.mm(prow[k2][0:2, :], cond[:, kc, :], wt[k2][:, kc, :], [bw[k2], b_cond], [b_prow[k2]],
                             start=(kc == 0), stop=(kc == 15), inc=(kc == 15))
                    c.cp(rowsb[k2][:], prow[k2][0:2, :], [b_prow[k2]], [b_rowsb[k2]], eng="act")
                    for nn in range(4):
                        jj = blk * 4 + nn - j0
                        c.tr(pmT[:, 2 * jj:2 * jj + 2], rowsb[k2][0:2, nn * 128:(nn + 1) * 128], identf[0:2, 0:2],
                             [b_rowsb[k2], b_identf], [b_pmT])
                nj = j1 - j0
                c.tt(modT[:, j0:j1], pmT[:, 0:2 * nj].rearrange("p (j two) -> p j two", two=2)[:, :, 0], bad[:, j0:j1], ALU.add,
                     [b_pmT, b_bad], [b_modT])
                if not last:
                    c.stt(A1[:], modT[:, 16:32], 1.0, nrmA[:], ALU.add, ALU.mult, [b_modT, b_nrmA], [b_A])
                else:
                    c.stt(A2[:], modT[:, 64:80], 1.0, nrmM[:], ALU.add, ALU.mult, [b_modT, b_nrmM], [b_A])
                    ptm = c.ps("ptm", [128, 512])
                    b_ptm = Buf(excl=True)
                    c.tr(ptm[0:96, 0:128], modT[:], identf[:], [b_modT, b_identf], [b_ptm])
                    mrow = c.sb("mrow", [96, 128], F32)
                    b_mrow = Buf()
                    c.cp(mrow[:], ptm[0:96, 0:128], [b_ptm], [b_mrow])
                    c.dma("sp", modD, mrow[:], R=[b_mrow], W=[b_modD])
            c.barrier()
            c.es = es0

        ada_part(0, 32, False)
        if stop == "p0all":
            ada_part(32, 96, True)
            c.drain()
            return nc
        if stop == "p0":
            c.drain()
            return nc
        B1 = modT[:, 0:16]
        B2 = modT[:, 48:64]

        def norm_tile(xt, b_xt, Acol, Bcol, hT, b_hT, col0, tmp, ptr):
            sq, ssq, rstd, xn = tmp["sq"], tmp["ss"], tmp["rstd"], tmp["xn"]
            b = tmp["b"]
            c.act(sq[:], xt[:], AF.Square, [b_xt], [b["sq"], b["ss"]], accum_out=ssq[:])
            c.ts(rstd[:], ssq[:], 1.0 / D, EPS, ALU.mult, ALU.add, [b["ss"]], [b["rstd"]])
            c.act(rstd[:], rstd[:], AF.Sqrt, [b["rstd"]], [b["rstd"]])
            c.op("dve", lambda e: e.reciprocal(out=rstd[:], in_=rstd[:]), [b["rstd"]], [b["rstd"]])
            c.ts(xn[:], xt[:], rstd[:, 0:1], None, ALU.mult, None, [b_xt, b["rstd"]], [b["xn"]])
            for k4 in range(4):
                pt, b_pt = ptr.get()
                ptb = pt.bitcast(BF16)
                for i in range(4):
                    kc = k4 * 4 + i
                    c.tr(ptb[:, i * 128:(i + 1) * 128], xn[:, kc * 128:(kc + 1) * 128], identb[:], [b["xn"], b_identb], [b_pt])
                for i in range(4):
                    kc = k4 * 4 + i
                    c.act(hT[:, kc, col0:col0 + 128], ptb[:, i * 128:(i + 1) * 128], AF.Identity, [b_pt, b_A, b_modT], [b_hT],
                          scale=Acol[:, kc:kc + 1], bias=Bcol[:, kc:kc + 1])

        def norm_tmp():
            return {"sq": c.sb("sq", [128, D], BF16), "ss": c.sb("ss", [128, 1], F32), "rstd": c.sb("rstd", [128, 1], F32),
                    "xn": c.sb("xn", [128, D], BF16), "b": {k: Buf() for k in ("sq", "ss", "rstd", "xn")}}

        yTg = c.sb("yTg", [128, 2, T], BF16)
        b_yTg = [Buf() for _ in range(8)]
        b_hTd = [Buf() for _ in range(8)]
        b_parts = []
        dbg_bufs = []

        with ExitStack() as es:
            c.es = es
            wfm = c.sb("wAfm", [128, 16, 768], BF16)
            wtm = c.sb("wAtm", [128, 16, 260], BF16)
            b_wfm, b_wtm = Buf(), Buf()
            c.dma("pool", wfm[:], wA_fm.rearrange("(kc p) n -> p kc n", p=128), W=[b_wfm])
            c.dma("pool", wtm[:], wA_tm.rearrange("(kc p) n -> p kc n", p=128), W=[b_wtm])
            cw, b_cw = load_const("cw", convw, [128, 6, 4], F32)
            Uc, b_U = load_const("Uc", k_U, [128, 128], F32)
            SLc, b_SL = load_const("SLc", k_SL, [128, 128], F32)
            ILTc, b_ILT = load_const("ILTc", k_ILT, [128, 128], F32)
            onesc, b_ones = load_const("onesc", k_ones, [128, 128], F32)
            al, b_al = load_const("al", alog, [128, 2], F32)
            dtbc, b_dtb = load_const("dtbc", dtb, [128, 2], F32)
            gnwr = c.sb("gnwr", [128, 128], F32)
            b_gnw = Buf()
            c.dma("sp", gnwr[:], gnw, W=[b_gnw])
            nega = c.sb("nega", [128, 2], F32)
            b_nega = Buf()
            c.act(nega[:], al[:], AF.Exp, [b_al], [b_nega])
            c.ts(nega[:], nega[:], -1.0, None, ALU.mult, None, [b_nega], [b_nega])

            hT = c.sb("hT", [128, 16, 512], BF16)
            b_hT = Buf()
            xts = [c.sb("xt", [128, D], F32) for _ in range(2)]
            b_xts = [Buf(), Buf()]
            ntmp = norm_tmp()
            ptr = PSlots(c, "ptr", 1, 1)
            pproj = PSlots(c, "pproj", 2, 1)
            pg = PSlots(c, "pg", 5, 1, 128)
            for ap_, bb_ in ptr.slots + pproj.slots:
                pg.slots.append((ap_[:, 0:128], bb_))

            raw = c.sb("raw", [128, 6, 515], F32)
            b_raw = [Buf() for _ in range(6)]
            for u in range(6):
                c.op("pool", lambda e: e.memset(raw[:, u, 0:3], 0.0), [], [b_raw[u]])
            qkv = c.sb("qkvs", [128, 6, 512], F32)
            b_qkv = [Buf() for _ in range(6)]
            sqt = c.sb("sqt", [128, 512], F32)
            b_sqt = Buf()
            rn = c.sb("rn", [128, 512], F32)
            b_rn = Buf()
            gsil = c.sb("gsil", [128, 4, 256], BF16)
            b_gsil = [Buf() for _ in range(4)]
            gb = c.sb("gb", [128, 4, 4], F32)
            b_gb = [Buf() for _ in range(4)]
            sp1 = c.sb("sp1", [128, 2], F32)
            b_sp1 = Buf()
            S = [c.sb("S", [128, 128], F32) for _ in range(2)]
            b_S = [Buf(), Buf()]
            for h in range(2):
                c.op("pool", lambda e: e.memset(S[h][:], 0.0), [], [b_S[h]])

            def unit_tmp():
                t = {}
                for n in ("Gs", "Dm", "DTm", "A", "AT", "Pa", "PaT", "Pb", "PbT", "FT", "QK", "WT", "kdec", "o1s", "o", "vnew"):
                    t[n] = c.sb("u_" + n, [128, 128], F32)
                for n in ("Xa", "Xb"):
                    t[n] = c.sb("u_" + n, [128, 256], F32)
                for n in ("bcol", "eb", "ebl", "edec", "bek", "ss2", "rs2"):
                    t[n] = c.sb("u_" + n, [128, 1], F32)
                t["y"] = c.sb("u_y", [128, 128], BF16)
                t["b"] = {}
                return t
            units = [[unit_tmp() for _ in range(2)] for _ in range(2)]

            def B(u, n):
                if n not in u["b"]:
                    u["b"][n] = Buf(n)
                return u["b"][n]

            for blk in range(8):
                for tt in range(4):
                    ti = blk * 4 + tt
                    xt, b_xt = xts[ti % 2], b_xts[ti % 2]
                    c.dma("sp", xt[:], xb[ti * 128:(ti + 1) * 128, :], W=[b_xt])
                    norm_tile(xt, b_xt, A1, B1, hT, b_hT, tt * 128, ntmp, ptr)
                c.dma("sp", hT_d[:, :, blk * 512:(blk + 1) * 512], hT[:], R=[b_hT], W=[b_hTd[blk]])
                if stop == "A1" and blk == 0:
                    c.drain()
                    return nc
                for u in range(6):
                    pp, b_pp = pproj.get()
                    for kc in range(16):
                        c.mm(pp, wfm[:, kc, u * 128:(u + 1) * 128], hT[:, kc, :], [b_wfm, b_hT], [b_pp],
                             start=(kc == 0), stop=(kc == 15), inc=(kc == 15))
                    c.cp(raw[:, u, 3:515], pp, [b_pp], [b_raw[u]], eng="act")
                    c.ts(qkv[:, u, :], raw[:, u, 0:512], cw[:, u, 0:1], None, ALU.mult, None, [b_raw[u], b_cw], [b_qkv[u]])
                    for i in range(1, 4):
                        c.stt(qkv[:, u, :], raw[:, u, i:i + 512], cw[:, u, i:i + 1], qkv[:, u, :], ALU.mult, ALU.add,
                              [b_raw[u], b_cw, b_qkv[u]], [b_qkv[u]])
                    c.cp(raw[:, u, 0:3], raw[:, u, 512:515], [b_raw[u]], [b_raw[u]], eng="pool")
                    c.act(qkv[:, u, :], qkv[:, u, :], AF.Silu, [b_qkv[u]], [b_qkv[u]])
                if stop == "A2" and blk == 0:
                    c.drain()
                    return nc
                for u in range(4):
                    c.tt(sqt[:], qkv[:, u, :], qkv[:, u, :], ALU.mult, [b_qkv[u]], [b_sqt])
                    pp, b_pp = pproj.get()
                    c.mm(pp, r32(onesc[:]), r32(sqt[:]), [b_ones, b_sqt], [b_pp])
                    c.ts(rn[:], pp, EPS, None, ALU.add, None, [b_pp], [b_rn])
                    c.act(rn[:], rn[:], AF.Sqrt, [b_rn], [b_rn])
                    c.op("dve", lambda e: e.reciprocal(out=rn[:], in_=rn[:]), [b_rn], [b_rn])
                    c.stt(qkv[:, u, :], qkv[:, u, :], (SCALE if u < 2 else 1.0), rn[:], ALU.mult, ALU.mult,
                          [b_qkv[u], b_rn], [b_qkv[u]])
                for tt in range(4):
                    pp, b_pp = pproj.get()
                    for kc in range(16):
                        c.mm(pp[:, 0:260], hT[:, kc, tt * 128:(tt + 1) * 128], wtm[:, kc, :], [b_wtm, b_hT], [b_pp],
                             start=(kc == 0), stop=(kc == 15), inc=(kc == 15))
                    c.act(gsil[:, tt, :], pp[:, 0:256], AF.Silu, [b_pp], [b_gsil[tt]])
                    c.act(gb[:, tt, 0:2], pp[:, 256:258], AF.Sigmoid, [b_pp], [b_gb[tt]])
                    for h in range(2):
                        c.act(sp1[:, h:h + 1], pp[:, 258 + h:259 + h], AF.Exp, [b_pp, b_dtb], [b_sp1], bias=dtbc[:, h:h + 1], scale=1.0)
                    c.act(sp1[:], sp1[:], AF.Ln, [b_sp1], [b_sp1], bias=1.0, scale=1.0)
                    c.tt(gb[:, tt, 2:4], sp1[:], nega[:], ALU.mult, [b_sp1, b_nega], [b_gb[tt]])

                if stop == "A2b" and blk == 0:
                    c.drain()
                    return nc
                def tile_ctx(tt):
                    ti = blk * 4 + tt
                    return ti, slice(tt * 128, (tt + 1) * 128), [units[ti % 2][h] for h in range(2)], b_gb[tt]

                def g_pre(tt, h):
                    ti, tsl, us, bg = tile_ctx(tt)
                    u = us[h]
                    beta = gb[:, tt, h:h + 1]
                    g = gb[:, tt, 2 + h:3 + h]
                    yield
                    p1, bp1 = pg.get()
                    c.mm(p1[:, 0:2], Uc[:], gb[:, tt, 2:4], [b_U, bg], [bp1])
                    c.cp(u["bcol"][:], p1[:, h:h + 1], [bp1], [B(u, "bcol")])
                    yield
                    p2, bp2 = pg.get()
                    c.mm(p2[:, 0:2], onesc[:], gb[:, tt, 2:4], [b_ones, bg], [bp2])
                    c.act(u["ebl"][:], p2[:, h:h + 1], AF.Exp, [bp2], [B(u, "ebl")])
                    c.cp(u["ss2"][:], p2[:, h:h + 1], [bp2], [B(u, "ss2")])
                    c.act(u["edec"][:], u["bcol"][:], AF.Exp, [B(u, "bcol"), B(u, "ss2")], [B(u, "edec")], scale=-1.0, bias=u["ss2"][:, 0:1])
                    c.act(u["eb"][:], u["bcol"][:], AF.Exp, [B(u, "bcol")], [B(u, "eb")])
                    c.tt(u["bek"][:], u["eb"][:], beta, ALU.mult, [B(u, "eb"), bg], [B(u, "bek")])
                    c.ts(u["Gs"][:], SLc[:], g, None, ALU.mult, None, [b_SL, bg], [B(u, "Gs")])
                    yield
                    p3, bp3 = pg.get()
                    c.mm(p3, Uc[:], u["Gs"][:], [b_U, B(u, "Gs")], [bp3])
                    c.act(u["Dm"][:], p3, AF.Exp, [bp3], [B(u, "Dm")])
                    c.stt(u["Dm"][:], u["Dm"][:], beta, SLc[:], ALU.mult, ALU.mult, [B(u, "Dm"), bg, b_SL], [B(u, "Dm")])
                    yield
                    p4, bp4 = pg.get()
                    c.mm(p4, u["Gs"][:], Uc[:], [b_U, B(u, "Gs")], [bp4])
                    c.act(u["DTm"][:], p4, AF.Exp, [bp4], [B(u, "DTm")])
                    c.tt(u["DTm"][:], u["DTm"][:], ILTc[:], ALU.mult, [B(u, "DTm"), b_ILT], [B(u, "DTm")])
                    u = us[h]
                    beta = gb[:, tt, h:h + 1]
                    KT = qkv[:, 2 + h, tsl]
                    QT = qkv[:, h, tsl]
                    VT = qkv[:, 4 + h, tsl]
                    yield
                    p1, bp1 = pg.get()
                    c.mm(p1, r32(KT), r32(KT), [b_qkv[2 + h]], [bp1])
                    c.tt(u["A"][:], p1, u["Dm"][:], ALU.mult, [bp1, B(u, "Dm")], [B(u, "A")])
                    yield
                    p2, bp2 = pg.get()
                    c.mm(p2, r32(KT), r32(QT), [b_qkv[2 + h], b_qkv[h]], [bp2])
                    c.tt(u["QK"][:], p2, u["DTm"][:], ALU.mult, [bp2, B(u, "DTm")], [B(u, "QK")])
                    yield
                    p3, bp3 = pg.get()
                    c.tr(p3, u["A"][:], identf[:], [B(u, "A"), b_identf], [bp3])
                    c.cp(u["AT"][:], p3, [bp3], [B(u, "AT")], eng="act")
                    yield
                    p4, bp4 = pg.get()
                    c.tr(p4, VT, identf[:], [b_qkv[4 + h], b_identf], [bp4])
                    c.ts(u["Xa"][:, 0:128], p4, beta, None, ALU.mult, None, [bp4, bg], [B(u, "Xa")])
                    yield
                    p5, bp5 = pg.get()
                    c.tr(p5, KT, identf[:], [b_qkv[2 + h], b_identf], [bp5])
                    c.ts(u["Xa"][:, 128:256], p5, u["bek"][:, 0:1], None, ALU.mult, None, [bp5, B(u, "bek")], [B(u, "Xa")])
                    c.act(u["kdec"][:], p5, AF.Identity, [bp5, B(u, "edec")], [B(u, "kdec")], scale=u["edec"][:, 0:1])
                    u = us[h]
                    X = [(u["Xa"], B(u, "Xa")), (u["Xb"], B(u, "Xb"))]
                    c.stt(u["FT"][:], u["AT"][:], -1.0, identf[:], ALU.mult, ALU.add, [B(u, "AT"), b_identf], [B(u, "FT")])
                    xi = 0
                    Pc, PcT, bPc, bPcT = u["A"], u["AT"], B(u, "A"), B(u, "AT")
                    pp_ = [(u["Pa"], u["PaT"], "Pa", "PaT"), (u["Pb"], u["PbT"], "Pb", "PbT")]
                    for lvl in range(7):
                        Xc, bXc = X[xi]
                        Xn, bXn = X[1 - xi]
                        yield
                        pa, bpa = pg.get()
                        pa2, bpa2 = pg.get()
                        c.mm(pa, r32(u["FT"][:]), r32(Xc[:, 0:128]), [B(u, "FT"), bXc], [bpa])
                        c.mm(pa2, r32(u["FT"][:]), r32(Xc[:, 128:256]), [B(u, "FT"), bXc], [bpa2])
                        c.cp(Xn[:, 0:128], pa, [bpa], [bXn], eng="act")
                        c.cp(Xn[:, 128:256], pa2, [bpa2], [bXn])
                        xi = 1 - xi
                        if lvl == 6:
                            break
                        Pn, PnT, nPn, nPnT = pp_[lvl % 2]
                        yield
                        pq, bpq = pg.get()
                        c.mm(pq, r32(Pc[:]), r32(PcT[:]), [bPc, bPcT], [bpq])
                        c.cp(PnT[:], pq, [bpq], [B(u, nPnT)], eng="act")
                        c.tt(u["FT"][:], pq, identf[:], ALU.add, [bpq, b_identf], [B(u, "FT")])
                        if lvl < 5:
                            yield
                            pq2, bpq2 = pg.get()
                            c.mm(pq2, r32(PcT[:]), r32(Pc[:]), [bPc, bPcT], [bpq2])
                            c.cp(Pn[:], pq2, [bpq2], [B(u, nPn)])
                        Pc, PcT, bPc, bPcT = Pn, PnT, B(u, nPn), B(u, nPnT)
                    u["Xf"], u["bXf"] = X[xi]
                    yield
                    pw, bpw = pg.get()
                    c.tr(pw, u["Xf"][:, 128:256], identf[:], [u["bXf"], b_identf], [bpw])
                    c.cp(u["WT"][:], pw, [bpw], [B(u, "WT")], eng="act")

                def g_seq(tt, h):
                    ti, tsl, us, bg = tile_ctx(tt)
                    u = us[h]
                    QT = qkv[:, h, tsl]
                    Xf, bXf = u["Xf"], u["bXf"]
                    yield
                    p1, bp1 = pg.get()
                    c.mm(p1, r32(u["WT"][:]), r32(S[h][:]), [B(u, "WT"), b_S[h]], [bp1])
                    c.tt(u["vnew"][:], Xf[:, 0:128], p1, ALU.subtract, [bXf, bp1], [B(u, "vnew")])
                    yield
                    p2, bp2 = pg.get()
                    c.mm(p2, r32(QT), r32(S[h][:]), [b_qkv[h], b_S[h]], [bp2])
                    c.act(u["o1s"][:], p2, AF.Identity, [bp2, B(u, "eb")], [B(u, "o1s")], scale=u["eb"][:, 0:1])
                    yield
                    p3, bp3 = pg.get()
                    c.mm(p3, r32(u["QK"][:]), r32(u["vnew"][:]), [B(u, "QK"), B(u, "vnew")], [bp3])
                    c.tt(u["o"][:], u["o1s"][:], p3, ALU.add, [B(u, "o1s"), bp3], [B(u, "o")])
                    yield
                    p4, bp4 = pg.get()
                    c.mm(p4, r32(u["kdec"][:]), r32(u["vnew"][:]), [B(u, "kdec"), B(u, "vnew")], [bp4])
                    c.stt(S[h][:], S[h][:], u["ebl"][:, 0:1], p4, ALU.mult, ALU.add, [b_S[h], B(u, "ebl"), bp4], [b_S[h]])
                    c.act(u["o1s"][:], u["o"][:], AF.Square, [B(u, "o")], [B(u, "o1s"), B(u, "ss2")], accum_out=u["ss2"][:])
                    c.ts(u["rs2"][:], u["ss2"][:], 1.0 / 128, EPS, ALU.mult, ALU.add, [B(u, "ss2")], [B(u, "rs2")])
                    c.act(u["rs2"][:], u["rs2"][:], AF.Sqrt, [B(u, "rs2")], [B(u, "rs2")])
                    c.op("dve", lambda e: e.reciprocal(out=u["rs2"][:], in_=u["rs2"][:]), [B(u, "rs2")], [B(u, "rs2")])
                    c.stt(u["o"][:], u["o"][:], u["rs2"][:, 0:1], gnwr[:], ALU.mult, ALU.mult, [B(u, "o"), B(u, "rs2"), b_gnw], [B(u, "o")])
                    c.tt(u["y"][:], u["o"][:], gsil[:, tt, h * 128:(h + 1) * 128], ALU.mult, [B(u, "o"), b_gsil[tt]], [B(u, "y")])
                    yield
                    py, bpy = pg.get()
                    pyb = py.bitcast(BF16)
                    c.tr(pyb[:, 0:128], u["y"][:], identb[:], [B(u, "y"), b_identb], [bpy])
                    c.cp(yTg[:, h, ti * 128:(ti + 1) * 128], pyb[:, 0:128], [bpy], [b_yTg[blk]], eng="act")

                def run_rr(gens):
                    gens = list(gens)
                    while gens:
                        for g_ in list(gens):
                            try:
                                next(g_)
                            except StopIteration:
                                gens.remove(g_)

                for step in range(5):
                    gens = []
                    if step < 4:
                        gens += [g_pre(step, 0), g_pre(step, 1)]
                    if step > 0:
                        gens += [g_seq(step - 1, 0), g_seq(step - 1, 1)]
                    run_rr(gens)
            if debug:
                bd = Buf(); dbg_bufs.append(bd)
                c.dma("sp", yTg_d, yTg[:], R=b_yTg, W=[bd])
        c.barrier()
        c.es = es0
        if stop == "A":
            c.drain()
            return nc

        with ExitStack() as es:
            c.es = es
            wfm = c.sb("wBfm", [128, 16, 1024], BF16)
            wtm = c.sb("wBtm", [128, 16, 262], BF16)
            b_wfm, b_wtm = Buf(), Buf()
            c.dma("pool", wfm[:], wB_fm.rearrange("(kc p) n -> p kc n", p=128), W=[b_wfm])
            c.dma("pool", wtm[:], wB_tm.rearrange("(kc p) n -> p kc n", p=128), W=[b_wtm])
            w1 = [c.sb("w1", [128, 32, 256], BF16) for _ in range(2)]
            b_w1 = [Buf(), Buf()]
            for i, src in enumerate((w1k, w1v)):
                for half in range(2):
                    c.dma("pool", w1[i][:, half * 16:(half + 1) * 16, :],
                          src[half * 2048:(half + 1) * 2048, :].rearrange("(l p) n -> p l n", p=128), W=[b_w1[i]])
            w2 = [c.sb("w2", [128, 2, 128], BF16) for _ in range(2)]
            b_w2 = [Buf(), Buf()]
            for i, src in enumerate((w2k, w2v)):
                c.dma("pool", w2[i][:], src.rearrange("(a p) n -> p a n", p=128), W=[b_w2[i]])
            pe_ = [c.sb("pe", [128, 32], BF16) for _ in range(2)]
            b_pe = [Buf(), Buf()]
            for i, src in enumerate((pek, pev)):
                c.dma("pool", pe_[i][:], src, W=[b_pe[i]])
            wo = c.sb("wo", [128, 4, D], BF16)
            b_wo = Buf()
            c.dma("pool", wo[:], wout.rearrange("(a p) n -> p a n", p=128), W=[b_wo])
            triU, b_triU = load_const("triU", k_triU, [128, 256], BF16)
            triL, b_triL = load_const("triL", k_triL, [128, 256], BF16)
            Ec, b_E = load_const("Ec", k_E, [64, 32, 128], BF16)
            cb0, b_cb0 = load_const("cb0", k_cb0, [128, 17, 128], BF16)
            fb, b_fb = load_const("fb", k_fb, [128, 32, 64], BF16)
            ovl, b_ovl = load_const("ovl", k_ovl, [128, 2, 65], F32)

            kcmp = [c.sb("kcmp", [128, 16 + T], BF16) for _ in range(2)]
            b_kcmp = [Buf(), Buf()]
            for i in range(2):
                c.op("pool", lambda e: e.memset(kcmp[i][:], 0.0), [], [b_kcmp[i]])
            kslc = c.sb("kslc", [128, T], BF16)
            kwin = c.sb("kwin", [128, T], BF16)
            b_kslc, b_kwin = Buf(), Buf()
            vslc = c.sb("vslc", [128, 32, 130], BF16)
            vwin = c.sb("vwin", [128, 32, 130], BF16)
            b_vslc, b_vwin = Buf(), Buf()
            c.op("pool", lambda e: e.memset(vslc[:, :, 128:130], 1.0), [], [b_vslc])
            c.op("pool", lambda e: e.memset(vwin[:, :, 128:130], 1.0), [], [b_vwin])
            kcT = c.sb("kcT", [128, 256], BF16)
            b_kcT = Buf()
            c.op("pool", lambda e: e.memset(kcT[:], 0.0), [], [b_kcT])
            vcx = c.sb("vcx", [128, 2, 194], BF16)
            b_vcx = Buf()
            c.op("pool", lambda e: e.memset(vcx[:], 0.0), [], [b_vcx])
            c.cp(vcx[:, :, 128:193], ovl[:], [b_ovl, b_vcx], [b_vcx])

            hT = c.sb("hTb", [128, 16, 512], BF16)
            b_hT = Buf()
            qT = c.sb("qT", [128, 4, 512], BF16)
            b_qT = Buf()
            gn = c.sb("gn", [128, 4, 6], F32)
            b_gn = [Buf() for _ in range(4)]
            hs = [c.sb("hs", [128, 2, 32], BF16) for _ in range(2)]
            b_hs = [Buf(), Buf()]
            peb = [c.sb("peb", [128, 2], F32) for _ in range(2)]
            b_peb = [Buf(), Buf()]
            NET = 5
            ET = [c.sb("ET", [128, 256], BF16) for _ in range(NET)]
            b_ET = [Buf() for _ in range(NET)]
            eti = [0]
            imp = c.sb("imp", [128, 64], F32)
            imp2 = c.sb("imp2", [128, 64], F32)
            mx = c.sb("mx", [128, 16], F32)
            b_imp, b_imp2, b_mx = Buf(), Buf(), Buf()
            selb = c.sb("selb", [128, 64], BF16)
            b_selb = Buf()
            selbT = c.sb("selbT", [64, 2, 128], BF16)
            b_selbT = Buf()
            oacc = c.sb("oacc", [128, 2, 128], F32)
            b_oacc = Buf()
            rsm = c.sb("rsm", [128, 8], F32)
            b_rsm = Buf()
            ynb = c.sb("ynb", [128, 2, 128], BF16)
            b_ynb = Buf()
            yTn = c.sb("yTn", [128, 2, 512], BF16)
            b_yTn = Buf()
            pout = [c.sb("pout", [128, 512], F32) for _ in range(2)]
            b_pout = [Buf(), Buf()]

            pproj = PSlots(c, "pprojB", 2, 1)
            pst = PSlots(c, "pst", 2, 1)
            pst_attn = PSlots(c, "pst_attn", 0, 1)
            pst_attn.slots = [pst.slots[0], pproj.slots[0], pst.slots[1], pproj.slots[1]]
            pacc = [c.ps("pacc", [128, 512]) for _ in range(4)]
            b_pacc = [Buf(excl=True) for _ in range(4)]

            for i in range(2):
                for half in range(2):
                    pp, b_pp = pproj.get()
                    for l in range(32):
                        c.mm(pp[:, 0:1], w1[i][:, l, half * 128:(half + 1) * 128], pe_[i][:, l:l + 1],
                             [b_w1[i], b_pe[i]], [b_pp], start=(l == 0), stop=(l == 31), inc=(l == 31))
                    c.cp(peb[i][:, half:half + 1], pp[:, 0:1], [b_pp], [b_peb[i]])

            for blk in range(8):
                if blk == 0:
                    c.dma("sp", hT[:], hT_d[:, :, 0:512], R=[b_hTd[0]], W=[b_hT])
                for u in range(8):
                    pp, b_pp = pproj.get()
                    for kc in range(16):
                        c.mm(pp, wfm[:, kc, u * 128:(u + 1) * 128], hT[:, kc, :], [b_wfm, b_hT], [b_pp],
                             start=(kc == 0), stop=(kc == 15), inc=(kc == 15))
                    eng = "act" if u % 2 else "dve"
                    if u < 4:
                        c.cp(qT[:, u, :], pp, [b_pp], [b_qT], eng=eng)
                    elif u < 6:
                        c.cp(kcmp[u - 4][:, 16 + blk * 512:16 + (blk + 1) * 512], pp, [b_pp], [b_kcmp[u - 4]], eng=eng)
                    elif u == 6:
                        c.cp(kslc[:, blk * 512:(blk + 1) * 512], pp, [b_pp], [b_kslc], eng=eng)
                    else:
                        c.cp(kwin[:, blk * 512:(blk + 1) * 512], pp, [b_pp], [b_kwin], eng=eng)
                for tt in range(4):
                    ti = blk * 4 + tt
                    pp, b_pp = pproj.get()
                    for kc in range(16):
                        c.mm(pp[:, 0:262], hT[:, kc, tt * 128:(tt + 1) * 128], wtm[:, kc, :], [b_wtm, b_hT], [b_pp],
                             start=(kc == 0), stop=(kc == 15), inc=(kc == 15))
                    c.cp(vslc[:, ti, 0:128], pp[:, 0:128], [b_pp], [b_vslc], eng="act")
                    c.cp(vwin[:, ti, 0:128], pp[:, 128:256], [b_pp], [b_vwin])
                    c.act(gn[:, tt, :], pp[:, 256:262], AF.Sigmoid, [b_pp], [b_gn[tt]])
                if blk < 7:
                    c.dma("sp", hT[:], hT_d[:, :, (blk + 1) * 512:(blk + 2) * 512], R=[b_hTd[blk + 1]], W=[b_hT])
                m0 = 32 * blk
                for i in range(2):
                    for half in range(2):
                        pp, b_pp = pproj.get()
                        for l in range(32):
                            rhs = kcmp[i][:, 16 * m0 + l: 16 * m0 + l + 16 * 31 + 1: 16]
                            c.mm(pp[:, 0:32], w1[i][:, l, half * 128:(half + 1) * 128], rhs, [b_w1[i], b_kcmp[i]], [b_pp],
                                 start=(l == 0), stop=(l == 31), inc=(l == 31))
                        c.act(hs[i][:, half, :], pp[:, 0:32], AF.Silu, [b_pp, b_peb[i]], [b_hs[i]], bias=peb[i][:, half:half + 1], scale=1.0)
                pp, b_pp = pproj.get()
                for half in range(2):
                    c.mm(pp[:, 0:32], w2[0][:, half, :], hs[0][:, half, :], [b_w2[0], b_hs[0]], [b_pp], start=(half == 0), stop=(half == 1), inc=(half == 1))
                c.cp(kcT[:, m0:m0 + 32], pp[:, 0:32], [b_pp], [b_kcT])
                pp, b_pp = pproj.get()
                for half in range(2):
                    c.mm(pp[0:32, 0:128], hs[1][:, half, :], w2[1][:, half, :], [b_w2[1], b_hs[1]], [b_pp], start=(half == 0), stop=(half == 1), inc=(half == 1))
                mc_, mp = m0 // 128, m0 % 128
                c.cp(vcx[mp:mp + 32, mc_, 0:128], pp[0:32, 0:128], [b_pp], [b_vcx], eng="act")
                if blk == 0:
                    c.op("pool", lambda e: e.memset(vcx[0:1, 0, :], 0.0), [], [b_vcx])

                for tt in range(4):
                    qb = blk * 4 + tt
                    tsl = slice(tt * 128, (tt + 1) * 128)
                    pairs = []
                    SK = 3

                    def mk_s1(lhs_k, rb_k, ncol, rhs_q, extra, pre=None):
                        def s1(slot):
                            if pre is not None:
                                pre()
                            st, b_st = pst_attn.get()
                            c.mm(st[:, 0:ncol], lhs_k, rhs_q, rb_k + [b_qT], [b_st],
                                 start=True, stop=(len(extra) == 0), inc=(len(extra) == 0))
                            for xi, (o_, l_, r_, rb) in enumerate(extra):
                                last = xi == len(extra) - 1
                                c.mm(o_(st), l_, r_, rb, [b_st], start=False, stop=last, inc=last)
                            e = eti[0] % NET
                            eti[0] += 1
                            slot["e"] = e
                            c.act(ET[e][:, 0:ncol], st[:, 0:ncol], AF.Exp, [b_st], [b_ET[e]], scale=SCALE)
                        return s1

                    mcs = [0] + ([1] if qb >= 16 else [])

                    def fin_cmp():
                        for g in range(4):
                            pa = pacc[g]
                            c.ts(rsm[:, g:g + 1], pa[:, 192:193], 1e-30, None, ALU.max, None, [b_pacc[g]], [b_rsm])
                            c.op("dve", lambda e: e.reciprocal(out=rsm[:, g:g + 1], in_=rsm[:, g:g + 1]), [b_rsm], [b_rsm])
                            c.stt(imp[:], pa[:, 128:192], rsm[:, g:g + 1], (fb[:, qb, :] if g == 0 else imp[:]), ALU.mult, ALU.add,
                                  [b_pacc[g], b_rsm, b_fb, b_imp], [b_imp])
                            if g < 2:
                                c.tt(rsm[:, 4 + g:5 + g], rsm[:, g:g + 1], gn[:, tt, g:g + 1], ALU.mult, [b_rsm, b_gn[tt]], [b_rsm])
                                c.ts(oacc[:, g, :], pa[:, 0:128], rsm[:, 4 + g:5 + g], None, ALU.mult, None, [b_pacc[g], b_rsm], [b_oacc])
                        c.op("dve", lambda e: e.max(out=mx[:, 0:8], in_=imp[:]), [b_imp], [b_mx])
                        c.op("dve", lambda e: e.match_replace(out=imp2[:], in_to_replace=mx[:, 0:8], in_values=imp[:], imm_value=-3.0e4), [b_imp, b_mx], [b_imp2])
                        c.op("dve", lambda e: e.max(out=mx[:, 8:16], in_=imp2[:]), [b_imp2], [b_mx])
                        c.ts(selb[:], imp[:], mx[:, 15:16], NEG, ALU.is_lt, ALU.mult, [b_imp, b_mx], [b_selb])

                    for k, mc in enumerate(mcs):
                        delta = qb - 16 * mc
                        di = min(delta, 16) if mc == 0 else delta
                        for hf in range(2):
                            extra = [((lambda st, gg=gg: st[:, gg * 128:(gg + 1) * 128]), identb[:], cb0[:, di, :], [b_identb, b_cb0]) for gg in range(2)]
                            s1 = mk_s1(kcT[:, mc * 128:(mc + 1) * 128], [b_kcT], 256, qT[:, 2 * hf:2 * hf + 2, tsl], extra)

                            def s2(slot, k=k, mc=mc, hf=hf):
                                e = slot["e"]
                                lastk = k == len(mcs) - 1
                                for gg in range(2):
                                    g = 2 * hf + gg
                                    c.mm(pacc[g][:, 0:193], ET[e][:, gg * 128:(gg + 1) * 128], vcx[:, mc, 0:193], [b_ET[e], b_vcx], [b_pacc[g]],
                                         start=(k == 0), stop=lastk)
                                if lastk and hf == 1:
                                    fin_cmp()
                            pairs.append((s1, s2))
                    i_cmp_last = len(pairs) - 1

                    def b4_pe():
                        st, b_st = pst.get()
                        stb = st.bitcast(BF16)
                        c.tr(stb[0:64, 0:128], selb[:], identb[:], [b_selb, b_identb], [b_st])
                        c.cp(selbT[:, 0, :], stb[0:64, 0:128], [b_st], [b_selbT])
                        c.cp(selbT[:, 1, :], stb[0:64, 0:128], [b_st], [b_selbT])

                    for br in (1, 0):
                        if br == 0:
                            kcs = list(range(0, qb + 1))
                            KT_, b_KT, V_, b_V = kslc, b_kslc, vslc, b_vslc
                            pg0 = 0
                        else:
                            kcs = list(range(max(0, qb - 4), qb + 1))
                            KT_, b_KT, V_, b_V = kwin, b_kwin, vwin, b_vwin
                            pg0 = 2

                        def fin_br(br=br, pg0=pg0):
                            for g in range(2):
                                pa, bpa = pacc[pg0 + g], b_pacc[pg0 + g]
                                c.op("dve", lambda e: e.reciprocal(out=rsm[:, g:g + 1], in_=pa[:, 128:129]), [bpa], [b_rsm])
                                gi = (1 + br) * 2 + g
                                c.tt(rsm[:, 4 + g:5 + g], rsm[:, g:g + 1], gn[:, tt, gi:gi + 1], ALU.mult, [b_rsm, b_gn[tt]], [b_rsm])
                                c.stt(oacc[:, g, :], pa[:, 0:128], rsm[:, 4 + g:5 + g], oacc[:, g, :], ALU.mult, ALU.add,
                                      [bpa, b_rsm, b_oacc], [b_oacc])

                        if br == 0:
                            while len(pairs) < i_cmp_last + SK + 1:
                                pairs.append((lambda slot: None, lambda slot: None))
                        for k, kc in enumerate(kcs):
                            extra = []
                            full = (lambda st: st[:, 0:256])
                            if br == 0:
                                extra.append((full, Ec[:, kc, :], selbT[:].rearrange("p a b -> p (a b)"), [b_E, b_selbT]))
                            if kc == qb:
                                extra.append((full, identb[:], triU[:], [b_identb, b_triU]))
                            if br == 1 and kc == qb - 4:
                                extra.append((full, identb[:], triL[:], [b_identb, b_triL]))
                            s1 = mk_s1(KT_[:, kc * 128:(kc + 1) * 128], [b_KT], 256, qT[:, 0:2, tsl], extra,
                                       pre=(b4_pe if (br == 0 and k == 0) else None))

                            def s2(slot, k=k, kc=kc, n=len(kcs), V_=V_, b_V=b_V, pg0=pg0, fin=fin_br):
                                e = slot["e"]
                                for g in range(2):
                                    c.mm(pacc[pg0 + g][:, 0:129], ET[e][:, g * 128:(g + 1) * 128], V_[:, kc, 0:129], [b_ET[e], b_V], [b_pacc[pg0 + g]],
                                         start=(k == 0), stop=(k == n - 1))
                                if k == n - 1:
                                    fin()
                            pairs.append((s1, s2))

                    slots = [dict() for _ in pairs]
                    for i in range(len(pairs) + SK):
                        if i < len(pairs):
                            pairs[i][0](slots[i])
                        if i >= SK:
                            pairs[i - SK][1](slots[i - SK])
                    c.cp(ynb[:], oacc[:], [b_oacc], [b_ynb], eng="act")
                    for g in range(2):
                        st, b_st = pst.get()
                        stb = st.bitcast(BF16)
                        c.tr(stb[:, 0:128], ynb[:, g, :], identb[:], [b_ynb, b_identb], [b_st])
                        c.cp(yTn[:, g, tsl], stb[:, 0:128], [b_st], [b_yTn])
                if debug:
                    bd = Buf(); dbg_bufs.append(bd)
                    c.dma("sp", yTn_d[:, :, blk * 512:(blk + 1) * 512], yTn[:], R=[b_yTn], W=[bd])
                for tt in range(4):
                    ti = blk * 4 + tt
                    for cbk in range(4):
                        po, b_po = pout[cbk % 2], b_pout[cbk % 2]
                        pp, b_pp = pproj.get()
                        for fc in range(4):
                            lhs = yTg[:, fc, ti * 128:(ti + 1) * 128] if fc < 2 else yTn[:, fc - 2, tt * 128:(tt + 1) * 128]
                            rb = [b_yTg[blk]] if fc < 2 else [b_yTn]
                            c.mm(pp, lhs, wo[:, fc, cbk * 512:(cbk + 1) * 512], rb + [b_wo], [b_pp], start=(fc == 0), stop=(fc == 3), inc=(fc == 3))
                        c.cp(po[:], pp, [b_pp], [b_po], eng=("act" if cbk % 2 else "dve"))
                        b_part = Buf()
                        b_parts.append(b_part)
                        c.dma("sp", partial.ap()[ti * 128:(ti + 1) * 128, cbk * 512:(cbk + 1) * 512], po[:], R=[b_po], W=[b_part])
        c.barrier()
        c.es = es0
        if stop == "B":
            c.drain()
            return nc
        if debug:
            bd = Buf(); dbg_bufs.append(bd)
            c.dma("sp", part_d, partial.ap(), R=b_parts, W=[bd])
        b_rs = Buf()
        b_rs0 = Buf()
        c.op("pool", lambda e: e.collective_compute("ReduceScatter", ALU.add, replica_groups=[[0, 1, 2, 3], [4, 5, 6, 7]],
                                                    ins=[partial.ap().opt()], outs=[rs_out.ap().opt()]), b_parts, [b_rs0])
        c.op("pool", lambda e: e.collective_compute("AllReduce", ALU.add, replica_groups=[[0, 1, 2, 3], [4, 5, 6, 7]],
                                                    ins=[fence_in.ap().opt()], outs=[fence_out.ap().opt()]), [b_rs0], [b_rs])

        ada_part(32, 96, True)
        if stop == "rs":
            c.drain()
            return nc
        with ExitStack() as es:
            c.es = es
            h2T = c.sb("h2T", [128, 16, 1024], BF16)
            b_h2T = Buf()
            acc = c.sb("acc", [128, 8, D], F32)
            b_acc = [Buf() for _ in range(8)]
            comb = c.sb("comb", [128, 8, 65], F32)
            b_comb = Buf()
            ptr = PSlots(c, "ptr2", 1, 1)
            pgu = PSlots(c, "pgu", 4, 1)
            pd = PSlots(c, "pd", 3, 1)
            b_x1d = [Buf() for _ in range(8)]
            with ExitStack() as es2:
                c.es = es2
                ga = c.sb("ga", [128, D], F32)
                b_ga = Buf()
                c.dma("sp", ga[:], modD[32:48, :].rearrange("a b -> (a b)").partition_broadcast(128), R=[b_modD], W=[b_ga])
                rwt = c.sb("rwt", [128, 16, 64], BF16)
                b_rwt = Buf()
                c.dma("pool", rwt[:], rw.rearrange("(kc p) n -> p kc n", p=128), W=[b_rwt])
                rbr, b_rbr = load_const("rbr", rbias, [128, 64], F32)
                xts = [c.sb("xt2", [128, D], F32) for _ in range(2)]
                b_xts = [Buf(), Buf()]
                ats = [c.sb("at2", [128, D], F32) for _ in range(2)]
                b_ats = [Buf(), Buf()]
                ntmp = norm_tmp()
                sc = c.sb("sc", [128, 64], F32)
                sv = c.sb("sv", [128, 64], F32)
                mx8 = c.sb("mx8", [128, 8], F32)
                den = c.sb("den", [128, 1], F32)
                b_sc, b_sv, b_mx8, b_den = Buf(), Buf(), Buf(), Buf()
                c.op("pool", lambda e: e.memset(comb[:, :, 64:65], 1.0), [], [b_comb])
                for tt in range(8):
                    xt, b_xt = xts[tt % 2], b_xts[tt % 2]
                    at, b_at = ats[tt % 2], b_ats[tt % 2]
                    c.dma("sp", xt[:], xq[tt * 128:(tt + 1) * 128, :], W=[b_xt])
                    c.dma("sp", at[:], rs_out.ap()[tt * 128:(tt + 1) * 128, :], R=[b_rs], W=[b_at])
                    c.tt(at[:], at[:], ga[:], ALU.mult, [b_at, b_ga], [b_at])
                    c.tt(xt[:], xt[:], at[:], ALU.add, [b_xt, b_at], [b_xt])
                    c.dma("sp", x1_d[tt * 128:(tt + 1) * 128, :], xt[:], R=[b_xt], W=[b_x1d[tt]])
                    norm_tile(xt, b_xt, A2, B2, h2T, b_h2T, tt * 128, ntmp, ptr)
                    pp, b_pp = pd.get()
                    for kc in range(16):
                        c.mm(pp[:, 0:64], h2T[:, kc, tt * 128:(tt + 1) * 128], rwt[:, kc, :], [b_h2T, b_rwt], [b_pp],
                             start=(kc == 0), stop=(kc == 15), inc=(kc == 15))
                    c.act(sc[:], pp[:, 0:64], AF.Sigmoid, [b_pp], [b_sc])
                    c.tt(sv[:], sc[:], rbr[:], ALU.add, [b_sc, b_rbr], [b_sv])
                    c.op("dve", lambda e: e.max(out=mx8[:], in_=sv[:]), [b_sv], [b_mx8])
                    c.ts(sv[:], sv[:], mx8[:, 7:8], None, ALU.is_ge, None, [b_sv, b_mx8], [b_sv])
                    c.tt(sv[:], sv[:], sc[:], ALU.mult, [b_sv, b_sc], [b_sv])
                    c.op("dve", lambda e: e.reduce_sum(out=den[:], in_=sv[:], axis=mybir.AxisListType.X), [b_sv], [b_den])
                    c.op("dve", lambda e: e.reciprocal(out=den[:], in_=den[:]), [b_den], [b_den])
                    c.ts(comb[:, tt, 0:64], sv[:], den[:, 0:1], 2.5, ALU.mult, ALU.mult, [b_sv, b_den, b_comb], [b_comb])
            c.barrier()
            pd.slots.append(ptr.slots[0])
            es3 = ExitStack()
            es3.__enter__()
            c.es = es3
            wgu = [[c.sb("wg", [128, 16, 128], BF16), c.sb("wu", [128, 16, 128], BF16)] for _ in range(4)]
            b_wgu = [[Buf(), Buf()] for _ in range(4)]
            wdt = [c.sb("wd", [128, 4, D], BF16) for _ in range(2)]
            b_wdt = [Buf(), Buf()]
            actT = [c.sb("actT", [128, 4, 1024], BF16) for _ in range(2)]
            b_actT = [Buf(), Buf()]
            sgt = [c.sb("sgt", [128, 512], BF16) for _ in range(2)]
            b_sgt = [Buf(), Buf()]
            ui = 0
            for e_ in list(range(n_exp)) + [64]:
                if e_ < 64:
                    sg_, su_, sd_ = ewg[e_], ewu[e_], ewd[e_]
                else:
                    sg_, su_, sd_ = swg, swu, swd
                wd_t, b_wd = wdt[e_ % 2], b_wdt[e_ % 2]
                for half in range(2):
                    c.dma("pool", wd_t[:, half * 2:(half + 1) * 2, :], sd_[half * 256:(half + 1) * 256, :].rearrange("(a p) n -> p a n", p=128), W=[b_wd])
                aT, b_aT = actT[e_ % 2], b_actT[e_ % 2]
                for fc in range(4):
                    (wg_t, wu_t), (b_wg, b_wu) = wgu[ui % 4], b_wgu[ui % 4]
                    ui += 1
                    c.dma("pool", wg_t[:], sg_[:, fc * 128:(fc + 1) * 128].rearrange("(kc p) n -> p kc n", p=128), W=[b_wg])
                    c.dma("pool", wu_t[:], su_[:, fc * 128:(fc + 1) * 128].rearrange("(kc p) n -> p kc n", p=128), W=[b_wu])
                    for tb in range(2):
                        pG, b_pG = pgu.get()
                        pU, b_pU = pgu.get()
                        for kc in range(16):
                            c.mm(pG, wg_t[:, kc, :], h2T[:, kc, tb * 512:(tb + 1) * 512], [b_wg, b_h2T], [b_pG], start=(kc == 0), stop=(kc == 15), inc=(kc == 15))
                        for kc in range(16):
                            c.mm(pU, wu_t[:, kc, :], h2T[:, kc, tb * 512:(tb + 1) * 512], [b_wu, b_h2T], [b_pU], start=(kc == 0), stop=(kc == 15), inc=(kc == 15))
                        k2 = (fc * 2 + tb) % 2
                        c.act(sgt[k2][:], pG, AF.Silu, [b_pG], [b_sgt[k2]])
                        c.tt(aT[:, fc, tb * 512:(tb + 1) * 512], sgt[k2][:], pU, ALU.mult, [b_sgt[k2], b_pU], [b_aT])
                for tt in range(8):
                    for cbk in range(4):
                        pD, b_pD = pd.get()
                        for fc in range(4):
                            c.mm(pD, aT[:, fc, tt * 128:(tt + 1) * 128], wd_t[:, fc, cbk * 512:(cbk + 1) * 512], [b_aT, b_wd], [b_pD],
                                 start=(fc == 0), stop=(fc == 3), inc=(fc == 3))
                        dst = acc[:, tt, cbk * 512:(cbk + 1) * 512]
                        if e_ == (0 if n_exp else 64):
                            c.ts(dst, pD, comb[:, tt, e_:e_ + 1], None, ALU.mult, None, [b_pD, b_comb], [b_acc[tt]])
                        else:
                            c.stt(dst, pD, comb[:, tt, e_:e_ + 1], dst, ALU.mult, ALU.add, [b_pD, b_comb, b_acc[tt]], [b_acc[tt]])
            es3.__exit__(None, None, None)
            c.barrier()
            c.es = es
            gm = c.sb("gm", [128, D], F32)
            b_gm = Buf()
            c.dma("sp", gm[:], modD[80:96, :].rearrange("a b -> (a b)").partition_broadcast(128), R=[b_modD], W=[b_gm])
            nf = c.sb("nf", [128, D], F32)
            b_nf = Buf()
            c.dma("sp", nf[:], nrm_f.partition_broadcast(128), W=[b_nf])
            xts = [c.sb("xt3", [128, D], F32) for _ in range(2)]
            b_xts = [Buf(), Buf()]
            sq3 = c.sb("sq3", [128, D], BF16)
            ss3 = c.sb("ss3", [128, 1], F32)
            b_sq3, b_ss3 = Buf(), Buf()
            b_out = []
            for tt in range(8):
                xt, b_xt = xts[tt % 2], b_xts[tt % 2]
                c.dma("sp", xt[:], x1_d[tt * 128:(tt + 1) * 128, :], R=[b_x1d[tt]], W=[b_xt])
                z = acc[:, tt, :]
                c.tt(z, z, gm[:], ALU.mult, [b_acc[tt], b_gm], [b_acc[tt]])
                c.tt(z, z, xt[:], ALU.add, [b_acc[tt], b_xt], [b_acc[tt]])
                c.act(sq3[:], z, AF.Square, [b_acc[tt]], [b_sq3, b_ss3], accum_out=ss3[:])
                c.ts(ss3[:], ss3[:], 1.0 / D, EPS, ALU.mult, ALU.add, [b_ss3], [b_ss3])
                c.act(ss3[:], ss3[:], AF.Sqrt, [b_ss3], [b_ss3])
                c.op("dve", lambda e: e.reciprocal(out=ss3[:], in_=ss3[:]), [b_ss3], [b_ss3])
                c.stt(xt[:], z, ss3[:, 0:1], nf[:], ALU.mult, ALU.mult, [b_acc[tt], b_ss3, b_nf, b_xt], [b_xt])
                bo = Buf()
                b_out.append(bo)
                c.dma("sp", y[tt * 128:(tt + 1) * 128, :], xt[:], R=[b_xt], W=[bo])
            c.finish(b_out + dbg_bufs)
        c.barrier()
        c.es = es0
        return nc


_NC = None


def _consts():
    bf = ml_dtypes.bfloat16
    p = np.arange(128)
    k = {}
    k["k_identf"] = np.eye(128, dtype=np.float32)
    k["k_identb"] = np.eye(128).astype(bf)
    k["k_U"] = (p[:, None] <= p[None, :]).astype(np.float32)
    k["k_SL"] = (p[:, None] > p[None, :]).astype(np.float32)
    k["k_ILT"] = (p[None, :] >= p[:, None]).astype(np.float32)
    k["k_ones"] = np.ones((128, 128), np.float32)
    triU = np.where(p[:, None] <= p[None, :], 0.0, NEG).astype(np.float32)
    triL = np.where(p[:, None] > p[None, :], 0.0, NEG).astype(np.float32)
    k["k_triU"] = np.concatenate([triU, triU], axis=1).astype(bf)
    k["k_triL"] = np.concatenate([triL, triL], axis=1).astype(bf)
    E = np.zeros((64, 32, 128), np.float32)
    for kc in range(32):
        for pp in range(128):
            E[2 * kc + pp // 64, kc, pp] = 1.0
    k["k_E"] = E.astype(bf)
    ov = np.zeros((256, 65), np.float32)
    for m in range(1, 256):
        n = m - 1
        cs = n * 16
        for j in range(64):
            if cs < j * 64 + 64 and cs + 32 > j * 64:
                ov[m, j] = 1.0
        ov[m, 64] = 1.0
    k["k_ovl"] = np.ascontiguousarray(ov.reshape(2, 128, 65).transpose(1, 0, 2))
    cb0 = np.zeros((128, 17, 128), np.float32)
    col = np.arange(128)
    for d in range(16):
        valid = (16 * p[:, None] + 15) <= (128 * d + col[None, :])
        cb0[:, d, :] = np.where(valid, 0.0, NEG)
    k["k_cb0"] = cb0.astype(bf)
    fb = np.zeros((128, 32, 64), np.float32)
    for qb in range(32):
        for t in range(128):
            cur = 2 * qb + t // 64
            for j in range(64):
                if j == 0 or j == cur or j == cur - 1:
                    fb[t, qb, j] = 1.0e4
                elif j > cur:
                    fb[t, qb, j] = -1.0e4
    k["k_fb"] = fb.astype(bf)
    return k


def _prep(inp):
    f = lambda a: np.ascontiguousarray(np.asarray(a, dtype=np.float32))
    x = f(inp["x"]); cc = f(inp["c"])
    w_in = f(inp["w_in"])[0]
    conv = f(inp["gdn_conv_w"])[0]
    wo_full = f(inp["w_out"])[0]
    consts = _consts()
    shared = {
        "w_ada": f(inp["w_ada"])[0], "b_ada": f(inp["b_ada"])[0],
        "nrm_a": np.ascontiguousarray(f(inp["norm_attn_w"])[0].reshape(16, 128).T),
        "nrm_m": np.ascontiguousarray(f(inp["norm_ffn_w"])[0].reshape(16, 128).T),
        "nrm_f": f(inp["norm_final_w"]),
        "gnw": np.ascontiguousarray(np.broadcast_to(f(inp["gdn_norm_w"])[0][None, :], (128, 128))),
        "w1k": f(inp["cmp_w1_k"])[0], "w1v": f(inp["cmp_w1_v"])[0],
        "w2k": f(inp["cmp_w2_k"])[0], "w2v": f(inp["cmp_w2_v"])[0],
        "pek": np.ascontiguousarray(f(inp["cmp_pe_k"])[0].T), "pev": np.ascontiguousarray(f(inp["cmp_pe_v"])[0].T),
        "rw": f(inp["router_w"])[0],
        "rbias": np.ascontiguousarray(np.broadcast_to(f(inp["router_bias"])[0][None, :], (128, 64))),
        "ewg": f(inp["expert_w_gate"])[0], "ewu": f(inp["expert_w_up"])[0], "ewd": f(inp["expert_w_down"])[0],
        "swg": f(inp["shared_w_gate"])[0], "swu": f(inp["shared_w_up"])[0], "swd": f(inp["shared_w_down"])[0],
    }
    shared.update(consts)
    maps = []
    for core in range(8):
        b, hg = core // 4, core % 4
        kvh = hg // 2
        g = [2 * hg, 2 * hg + 1]
        m = dict(shared)
        m["xb"] = x[b]
        m["xq"] = np.ascontiguousarray(x[b, hg * 1024:(hg + 1) * 1024])
        m["cvec"] = np.ascontiguousarray(cc[b].reshape(16, 128).T)
        cols = []
        for part in range(3):
            for h in g:
                cols += list(range(part * 1024 + h * 128, part * 1024 + h * 128 + 128))
        m["wA_fm"] = np.ascontiguousarray(w_in[:, cols])
        m["convw"] = np.ascontiguousarray(conv[:, cols].reshape(4, 6, 128).transpose(2, 1, 0))
        tcols = []
        for h in g:
            tcols += list(range(3088 + h * 128, 3088 + h * 128 + 128))
        tcols += [3072 + g[0], 3072 + g[1], 3080 + g[0], 3080 + g[1]]
        m["wA_tm"] = np.ascontiguousarray(w_in[:, tcols])
        al = f(inp["gdn_a_log"])[0][g]; db = f(inp["gdn_dt_bias"])[0][g]
        m["alog"] = np.ascontiguousarray(np.broadcast_to(al[None, :], (128, 2)))
        m["dtb"] = np.ascontiguousarray(np.broadcast_to(db[None, :], (128, 2)))
        grp = [4 * kvh + i for i in range(4)]
        qh = g + [h for h in grp if h not in g]
        bcols = []
        for h in qh:
            bcols += list(range(4112 + h * 128, 4112 + h * 128 + 128))
        for part in (0, 1, 2, 4):
            bcols += list(range(5136 + part * 256 + kvh * 128, 5136 + part * 256 + kvh * 128 + 128))
        m["wB_fm"] = np.ascontiguousarray(w_in[:, bcols])
        btc = []
        for part in (3, 5):
            btc += list(range(5136 + part * 256 + kvh * 128, 5136 + part * 256 + kvh * 128 + 128))
        for br in range(3):
            for h in g:
                btc.append(6672 + br * 8 + h)
        m["wB_tm"] = np.ascontiguousarray(w_in[:, btc])
        rows = []
        for h in g:
            rows += list(range(h * 128, h * 128 + 128))
        for h in g:
            rows += list(range(1024 + h * 128, 1024 + h * 128 + 128))
        m["wout"] = np.ascontiguousarray(wo_full[rows, :])
        maps.append(m)
    return maps


def kernel(**inputs):
    global _NC
    maps = _prep(inputs)
    if _NC is None:
        _NC = build()
    res = run_bass_kernel_spmd(_NC, maps, core_ids=list(range(8)))
    out = np.zeros((2, T, D), np.float32)
    for core in range(8):
        b, q = core // 4, core % 4
        out[b, q * 1024:(q + 1) * 1024] = res.results[core]["y"]
    return out
```

```python
import numpy as np
from contextlib import ExitStack
import ml_dtypes
import concourse.bass as bass
import concourse.mybir as mybir
from concourse.bass_utils import run_bass_kernel_spmd

F32 = mybir.dt.float32
F32R = mybir.dt.float32r
BF16 = mybir.dt.bfloat16
AF = mybir.ActivationFunctionType
ALU = mybir.AluOpType

D = 2048
T = 4096
NEG = -10000.0
EPS = 1e-6
SCALE = 128 ** -0.5


class Buf:
    __slots__ = ("w", "r", "name", "excl")

    def __init__(self, name="", excl=False):
        self.w = None
        self.r = {}
        self.name = name
        self.excl = excl


class Ctx:
    NS = 6

    def __init__(self, nc, es):
        self.nc = nc
        self.es = es
        self.E = {"pe": nc.tensor, "dve": nc.vector, "act": nc.scalar, "pool": nc.gpsimd, "sp": nc.sync}
        self.sem = {}
        self.cnt = {}
        for k in self.E:
            self.sem[k] = es.enter_context(nc.semaphore("s_" + k))
            self.cnt[k] = 0
        self.dq = {}
        for q in ("sp", "pool"):
            sl = []
            for i in range(self.NS):
                key = "d_%s%d" % (q, i)
                self.sem[key] = es.enter_context(nc.semaphore(key))
                self.cnt[key] = 0
                sl.append(key)
            self.dq[q] = [sl, 0]
        self.sem["d_coll"] = es.enter_context(nc.semaphore("d_coll"))
        self.cnt["d_coll"] = 0
        self.seen = {k: {} for k in self.E}
        self.uid = 0

    def sb(self, name, shape, dt):
        self.uid += 1
        return self.es.enter_context(self.nc.sbuf_tensor("%s_%d" % (name, self.uid), list(shape), dt))

    def ps(self, name, shape, dt=F32):
        self.uid += 1
        return self.es.enter_context(self.nc.psum_tensor("%s_%d" % (name, self.uid), list(shape), dt))

    def _wait(self, eng, tok):
        if tok is None:
            return
        key, val = tok
        if key == eng and eng == "pe":
            return
        if self.seen[eng].get(key, 0) >= val:
            return
        self.seen[eng][key] = val
        self.E[eng].wait_ge(self.sem[key], val)

    def _deps(self, eng, reads, writes):
        for b in reads:
            self._wait(eng, b.w)
            if b.excl:
                for t in list(b.r.items()):
                    if t[0] != eng:
                        self._wait(eng, t)
        for b in writes:
            self._wait(eng, b.w)
            for t in list(b.r.items()):
                self._wait(eng, t)

    def _mark(self, tok, reads, writes):
        for b in reads:
            if b.r.get(tok[0], 0) < tok[1]:
                b.r[tok[0]] = tok[1]
        for b in writes:
            b.w = tok
            b.r = {}

    def op(self, eng, fn, R=(), W=(), inc=True):
        self._deps(eng, R, W)
        ins = fn(self.E[eng])
        if inc:
            self.cnt[eng] += 1
            ins.then_inc(self.sem[eng], 1)
            tok = (eng, self.cnt[eng])
        else:
            tok = (eng, self.cnt[eng] + 1)
        self._mark(tok, R, W)
        return ins

    def dma(self, q, out, in_, R=(), W=(), **kw):
        sl, k = self.dq[q]
        key = sl[k % self.NS]
        self.dq[q][1] = k + 1
        if self.cnt[key]:
            self._wait(q, (key, self.cnt[key]))
        self._deps(q, R, W)
        ins = self.E[q].dma_start(out=out, in_=in_, **kw)
        self.cnt[key] += 16
        ins.then_inc(self.sem[key], 16)
        tok = (key, self.cnt[key])
        self._mark(tok, R, W)
        return tok

    def coll(self, fn, R=(), W=()):
        self._deps("pool", R, W)
        ins = fn(self.E["pool"])
        self.cnt["d_coll"] += 16
        ins.then_inc(self.sem["d_coll"], 16)
        tok = ("d_coll", self.cnt["d_coll"])
        self._mark(tok, R, W)
        return ins

    def barrier(self):
        for e in self.E:
            for k, v in self.cnt.items():
                if v and k != e:
                    self._wait(e, (k, v))

    def drain(self):
        for k, v in self.cnt.items():
            if v:
                self._wait("sp", (k, v))

    def finish(self, bufs):
        for b in bufs:
            self._wait("sp", b.w)

    def mm(self, out, lhsT, rhs, R, W, start=True, stop=True, inc=True):
        return self.op("pe", lambda e: e.matmul(out, lhsT, rhs, start=start, stop=stop), R, W, inc=inc)

    def tr(self, out, in_, ident, R, W):
        return self.op("pe", lambda e: e.transpose(out, in_, ident), R, W)

    def act(self, out, in_, func, R, W, **kw):
        return self.op("act", lambda e: e.activation(out=out, in_=in_, func=func, **kw), R, W)

    def tt(self, out, in0, in1, op, R, W, eng="dve"):
        return self.op(eng, lambda e: e.tensor_tensor(out=out, in0=in0, in1=in1, op=op), R, W)

    def ts(self, out, in0, s1, s2, op0, op1, R, W, eng="dve"):
        if op1 is None:
            return self.op(eng, lambda e: e.tensor_scalar(out=out, in0=in0, scalar1=s1, scalar2=None, op0=op0), R, W)
        return self.op(eng, lambda e: e.tensor_scalar(out=out, in0=in0, scalar1=s1, scalar2=s2, op0=op0, op1=op1), R, W)

    def stt(self, out, in0, scalar, in1, op0, op1, R, W):
        return self.op("dve", lambda e: e.scalar_tensor_tensor(out=out, in0=in0, scalar=scalar, in1=in1, op0=op0, op1=op1), R, W)

    def cp(self, out, in_, R, W, eng="dve"):
        if eng == "act":
            return self.op("act", lambda e: e.copy(out=out, in_=in_), R, W)
        return self.op(eng, lambda e: e.tensor_copy(out=out, in_=in_), R, W)


class PSlots:
    def __init__(self, c, name, nbanks, per, width=None):
        self.slots = []
        self.full = []
        w = width or 512 // per
        for b in range(nbanks):
            t = c.ps(name, [128, 512])
            bb = Buf(excl=True)
            self.full.append((t[:, :], bb))
            for s in range(per):
                self.slots.append((t[:, s * w:(s + 1) * w], bb))
        self.i = 0

    def get(self):
        s = self.slots[self.i % len(self.slots)]
        self.i += 1
        return s


def r32(ap):
    return ap


def build(debug=False, n_exp=64, stop=None):
    nc = bass.Bass("TRN2", target_bir_lowering=False)

    def din(name, shape, dt=F32):
        return nc.dram_tensor(name, list(shape), dt, kind="ExternalInput").ap()

    xb = din("xb", [T, D])
    xq = din("xq", [1024, D])
    cvec = din("cvec", [128, 16])
    w_ada = din("w_ada", [D, 6 * D])
    b_ada = din("b_ada", [6 * D])
    nrm_a = din("nrm_a", [128, 16])
    nrm_m = din("nrm_m", [128, 16])
    nrm_f = din("nrm_f", [D])
    wA_fm = din("wA_fm", [D, 768])
    wA_tm = din("wA_tm", [D, 260])
    convw = din("convw", [128, 6, 4])
    alog = din("alog", [128, 2])
    dtb = din("dtb", [128, 2])
    gnw = din("gnw", [128, 128])
    wB_fm = din("wB_fm", [D, 1024])
    wB_tm = din("wB_tm", [D, 262])
    w1k = din("w1k", [4096, 256])
    w1v = din("w1v", [4096, 256])
    w2k = din("w2k", [256, 128])
    w2v = din("w2v", [256, 128])
    pek = din("pek", [128, 32])
    pev = din("pev", [128, 32])
    wout = din("wout", [512, D])
    rw = din("rw", [D, 64])
    rbias = din("rbias", [128, 64])
    if n_exp:
        ewg = din("ewg", [64, D, 512])
        ewu = din("ewu", [64, D, 512])
        ewd = din("ewd", [64, 512, D])
    swg = din("swg", [D, 512])
    swu = din("swu", [D, 512])
    swd = din("swd", [512, D])
    k_identf = din("k_identf", [128, 128])
    k_identb = din("k_identb", [128, 128], BF16)
    k_U = din("k_U", [128, 128])
    k_SL = din("k_SL", [128, 128])
    k_ILT = din("k_ILT", [128, 128])
    k_ones = din("k_ones", [128, 128])
    k_triU = din("k_triU", [128, 256], BF16)
    k_triL = din("k_triL", [128, 256], BF16)
    k_E = din("k_E", [64, 32, 128], BF16)
    k_ovl = din("k_ovl", [128, 2, 65])
    k_cb0 = din("k_cb0", [128, 17, 128], BF16)
    k_fb = din("k_fb", [128, 32, 64], BF16)

    y = nc.dram_tensor("y", [1024, D], F32, kind="ExternalOutput").ap()

    dk = dict(kind="ExternalOutput") if debug else {}
    hT_d = nc.dram_tensor("hT_d", [128, 16, T], BF16, **dk).ap()
    modD = nc.dram_tensor("modD", [96, 128], F32, **dk).ap()
    partial = nc.dram_tensor("partial", [T, D], F32)
    rs_out = nc.dram_tensor("rs_out", [1024, D], F32)
    fence_in = nc.dram_tensor("fence_in", [128, 128], F32)
    fence_out = nc.dram_tensor("fence_out", [128, 128], F32)
    x1_d = nc.dram_tensor("x1_d", [1024, D], F32, **dk).ap()
    if debug:
        yTg_d = nc.dram_tensor("yTg_d", [128, 2, T], BF16, kind="ExternalOutput").ap()
        yTn_d = nc.dram_tensor("yTn_d", [128, 2, T], BF16, kind="ExternalOutput").ap()
        part_d = nc.dram_tensor("part_d", [T, D], F32, kind="ExternalOutput").ap()

    with ExitStack() as es0:
        c = Ctx(nc, es0)

        def load_const(name, src, shape, dt, q="sp"):
            t = c.sb(name, shape, dt)
            b = Buf(name)
            c.dma(q, t[:], src, W=[b])
            return t, b

        identf, b_identf = load_const("identf", k_identf, [128, 128], F32)
        identb, b_identb = load_const("identb", k_identb, [128, 128], BF16)
        nrmA, b_nrmA = load_const("nrmA", nrm_a, [128, 16], F32)
        nrmM, b_nrmM = load_const("nrmM", nrm_m, [128, 16], F32)
        modT = c.sb("modT", [128, 96], F32)
        b_modT = Buf()
        A1 = c.sb("A1", [128, 16], F32)
        A2 = c.sb("A2", [128, 16], F32)
        b_A = Buf()

        b_modD = Buf()

        def ada_part(j0, j1, last):
            with ExitStack() as es:
                c.es = es
                cv = c.sb("cv", [128, 16], F32)
                b_cv = Buf()
                c.dma("sp", cv[:], cvec, W=[b_cv])
                cond = c.sb("cond", [128, 16, 2], F32)
                b_cond = Buf()
                for j in range(2):
                    c.act(cond[:, :, j], cv[:], AF.Silu, [b_cv], [b_cond])
                wt = [c.sb("wada", [128, 16, 512], F32) for _ in range(2)]
                bw = [Buf(), Buf()]
                prow = [c.ps("prow", [128, 512]) for _ in range(2)]
                b_prow = [Buf(excl=True), Buf(excl=True)]
                pmT = c.ps("pmT", [128, 512])
                b_pmT = Buf(excl=True)
                rowsb = [c.sb("rowsb", [2, 512], F32) for _ in range(2)]
                b_rowsb = [Buf(), Buf()]
                bad = c.sb("bad", [128, 96], F32)
                b_bad = Buf()
                c.dma("sp", bad[:], b_ada.rearrange("(j p) -> p j", p=128), W=[b_bad], allow_slow_non_contiguous=True)
                for blk in range(j0 // 4, j1 // 4):
                    k2 = blk % 2
                    c.dma("sp", wt[k2][:], w_ada[:, blk * 512:(blk + 1) * 512].rearrange("(kc p) n -> p kc n", p=128), W=[bw[k2]])
                    for kc in range(16):
                        c.mm(prow[k2][0:2, :], cond[:, kc, :], wt[k2][:, kc, :], [bw[k2], b_cond], [b_prow[k2]],
                             start=(kc == 0), stop=(kc == 15), inc=(kc == 15))
                    c.cp(rowsb[k2][:], prow[k2][0:2, :], [b_prow[k2]], [b_rowsb[k2]], eng="act")
                    for nn in range(4):
                        jj = blk * 4 + nn - j0
                        c.tr(pmT[:, 2 * jj:2 * jj + 2], rowsb[k2][0:2, nn * 128:(nn + 1) * 128], identf[0:2, 0:2],
                             [b_rowsb[k2], b_identf], [b_pmT])
                nj = j1 - j0
                c.tt(modT[:, j0:j1], pmT[:, 0:2 * nj].rearrange("p (j two) -> p j two", two=2)[:, :, 0], bad[:, j0:j1], ALU.add,
                     [b_pmT, b_bad], [b_modT])
                if not last:
                    c.stt(A1[:], modT[:, 16:32], 1.0, nrmA[:], ALU.add, ALU.mult, [b_modT, b_nrmA], [b_A])
                else:
                    c.stt(A2[:], modT[:, 64:80], 1.0, nrmM[:], ALU.add, ALU.mult, [b_modT, b_nrmM], [b_A])
                    ptm = c.ps("ptm", [128, 512])
                    b_ptm = Buf(excl=True)
                    c.tr(ptm[0:96, 0:128], modT[:], identf[:], [b_modT, b_identf], [b_ptm])
                    mrow = c.sb("mrow", [96, 128], F32)
                    b_mrow = Buf()
                    c.cp(mrow[:], ptm[0:96, 0:128], [b_ptm], [b_mrow])
                    c.dma("sp", modD, mrow[:], R=[b_mrow], W=[b_modD])
            c.barrier()
            c.es = es0

        ada_part(0, 32, False)
        if stop == "p0all":
            ada_part(32, 96, True)
            c.drain()
            return nc
        if stop == "p0":
            c.drain()
            return nc
        B1 = modT[:, 0:16]
        B2 = modT[:, 48:64]

        def norm_tile(xt, b_xt, Acol, Bcol, hT, b_hT, col0, tmp, ptr):
            sq, ssq, rstd, xn = tmp["sq"], tmp["ss"], tmp["rstd"], tmp["xn"]
            b = tmp["b"]
            c.act(sq[:], xt[:], AF.Square, [b_xt], [b["sq"], b["ss"]], accum_out=ssq[:])
            c.ts(rstd[:], ssq[:], 1.0 / D, EPS, ALU.mult, ALU.add, [b["ss"]], [b["rstd"]])
            c.act(rstd[:], rstd[:], AF.Sqrt, [b["rstd"]], [b["rstd"]])
            c.op("dve", lambda e: e.reciprocal(out=rstd[:], in_=rstd[:]), [b["rstd"]], [b["rstd"]])
            c.ts(xn[:], xt[:], rstd[:, 0:1], None, ALU.mult, None, [b_xt, b["rstd"]], [b["xn"]])
            for k4 in range(4):
                pt, b_pt = ptr.get()
                ptb = pt.bitcast(BF16)
                for i in range(4):
                    kc = k4 * 4 + i
                    c.tr(ptb[:, i * 128:(i + 1) * 128], xn[:, kc * 128:(kc + 1) * 128], identb[:], [b["xn"], b_identb], [b_pt])
                for i in range(4):
                    kc = k4 * 4 + i
                    if k4 % 2 == 0:
                        c.act(hT[:, kc, col0:col0 + 128], ptb[:, i * 128:(i + 1) * 128], AF.Identity, [b_pt, b_A, b_modT], [b_hT],
                              scale=Acol[:, kc:kc + 1], bias=Bcol[:, kc:kc + 1])
                    else:
                        c.ts(hT[:, kc, col0:col0 + 128], ptb[:, i * 128:(i + 1) * 128], Acol[:, kc:kc + 1], Bcol[:, kc:kc + 1],
                             ALU.mult, ALU.add, [b_pt, b_A, b_modT], [b_hT])

        def norm_tmp():
            return {"sq": c.sb("sq", [128, D], BF16), "ss": c.sb("ss", [128, 1], F32), "rstd": c.sb("rstd", [128, 1], F32),
                    "xn": c.sb("xn", [128, D], BF16), "b": {k: Buf() for k in ("sq", "ss", "rstd", "xn")}}

        yTg = c.sb("yTg", [128, 2, T], BF16)
        b_yTg = [Buf() for _ in range(8)]
        b_hTd = [Buf() for _ in range(8)]
        b_parts = []
        dbg_bufs = []

        with ExitStack() as es:
            c.es = es
            wfm = c.sb("wAfm", [128, 16, 768], BF16)
            wtm = c.sb("wAtm", [128, 16, 260], BF16)
            b_wfm, b_wtm = Buf(), Buf()
            c.dma("pool", wfm[:], wA_fm.rearrange("(kc p) n -> p kc n", p=128), W=[b_wfm])
            c.dma("pool", wtm[:], wA_tm.rearrange("(kc p) n -> p kc n", p=128), W=[b_wtm])
            cw, b_cw = load_const("cw", convw, [128, 6, 4], F32)
            Uc, b_U = load_const("Uc", k_U, [128, 128], F32)
            SLc, b_SL = load_const("SLc", k_SL, [128, 128], F32)
            ILTc, b_ILT = load_const("ILTc", k_ILT, [128, 128], F32)
            onesc, b_ones = load_const("onesc", k_ones, [128, 128], F32)
            al, b_al = load_const("al", alog, [128, 2], F32)
            dtbc, b_dtb = load_const("dtbc", dtb, [128, 2], F32)
            gnwr = c.sb("gnwr", [128, 128], F32)
            b_gnw = Buf()
            c.dma("sp", gnwr[:], gnw, W=[b_gnw])
            nega = c.sb("nega", [128, 2], F32)
            b_nega = Buf()
            c.act(nega[:], al[:], AF.Exp, [b_al], [b_nega])
            c.ts(nega[:], nega[:], -1.0, None, ALU.mult, None, [b_nega], [b_nega])

            hT = c.sb("hT", [128, 16, 512], BF16)
            b_hT = Buf()
            xts = [c.sb("xt", [128, D], F32) for _ in range(2)]
            b_xts = [Buf(), Buf()]
            ntmp = norm_tmp()
            ptr = PSlots(c, "ptr", 1, 1)
            pproj = PSlots(c, "pproj", 2, 1)
            pg = PSlots(c, "pg", 5, 1, 128)
            for ap_, bb_ in ptr.slots + pproj.slots:
                pg.slots.append((ap_[:, 0:128], bb_))
            ptr.slots += pg.full[0:2]

            raw = c.sb("raw", [128, 6, 515], F32)
            b_raw = [Buf() for _ in range(6)]
            for u in range(6):
                c.op("pool", lambda e: e.memset(raw[:, u, 0:3], 0.0), [], [b_raw[u]])
            qkv = c.sb("qkvs", [128, 6, 512], F32)
            b_qkv = [Buf() for _ in range(6)]
            sqt = c.sb("sqt", [128, 512], F32)
            b_sqt = Buf()
            rn = c.sb("rn", [128, 512], F32)
            b_rn = Buf()
            gsil = c.sb("gsil", [128, 4, 256], BF16)
            b_gsil = [Buf() for _ in range(4)]
            gb = c.sb("gb", [128, 4, 4], F32)
            b_gb = [Buf() for _ in range(4)]
            sp1 = c.sb("sp1", [128, 2], F32)
            b_sp1 = Buf()
            S = [c.sb("S", [128, 128], F32) for _ in range(2)]
            b_S = [Buf(), Buf()]
            for h in range(2):
                c.op("pool", lambda e: e.memset(S[h][:], 0.0), [], [b_S[h]])

            def unit_tmp():
                t = {}
                for n in ("Gs", "Dm", "DTm", "A", "AT", "Pa", "PaT", "Pb", "PbT", "FT", "QK", "WT", "kdec", "o1s", "o", "vnew"):
                    t[n] = c.sb("u_" + n, [128, 128], F32)
                for n in ("Xa", "Xb"):
                    t[n] = c.sb("u_" + n, [128, 256], F32)
                for n in ("bcol", "eb", "ebl", "edec", "bek", "ss2", "rs2"):
                    t[n] = c.sb("u_" + n, [128, 1], F32)
                t["y"] = c.sb("u_y", [128, 128], BF16)
                t["b"] = {}
                return t
            units = [[unit_tmp() for _ in range(2)] for _ in range(2)]

            def B(u, n):
                if n not in u["b"]:
                    u["b"][n] = Buf(n)
                return u["b"][n]

            for blk in range(8):
                for tt in range(4):
                    ti = blk * 4 + tt
                    xt, b_xt = xts[ti % 2], b_xts[ti % 2]
                    c.dma("sp", xt[:], xb[ti * 128:(ti + 1) * 128, :], W=[b_xt])
                    norm_tile(xt, b_xt, A1, B1, hT, b_hT, tt * 128, ntmp, ptr)
                c.dma("sp", hT_d[:, :, blk * 512:(blk + 1) * 512], hT[:], R=[b_hT], W=[b_hTd[blk]])
                if stop == "A1" and blk == 0:
                    c.drain()
                    return nc
                for u in range(6):
                    pp, b_pp = pproj.get()
                    for kc in range(16):
                        c.mm(pp, wfm[:, kc, u * 128:(u + 1) * 128], hT[:, kc, :], [b_wfm, b_hT], [b_pp],
                             start=(kc == 0), stop=(kc == 15), inc=(kc == 15))
                    c.cp(raw[:, u, 3:515], pp, [b_pp], [b_raw[u]], eng="act")
                    c.ts(qkv[:, u, :], raw[:, u, 0:512], cw[:, u, 0:1], None, ALU.mult, None, [b_raw[u], b_cw], [b_qkv[u]])
                    for i in range(1, 4):
                        c.stt(qkv[:, u, :], raw[:, u, i:i + 512], cw[:, u, i:i + 1], qkv[:, u, :], ALU.mult, ALU.add,
                              [b_raw[u], b_cw, b_qkv[u]], [b_qkv[u]])
                    c.cp(raw[:, u, 0:3], raw[:, u, 512:515], [b_raw[u]], [b_raw[u]], eng="pool")
                    c.act(qkv[:, u, :], qkv[:, u, :], AF.Silu, [b_qkv[u]], [b_qkv[u]])
                if stop == "A2" and blk == 0:
                    c.drain()
                    return nc
                for u in range(4):
                    c.tt(sqt[:], qkv[:, u, :], qkv[:, u, :], ALU.mult, [b_qkv[u]], [b_sqt])
                    pp, b_pp = pproj.get()
                    c.mm(pp, r32(onesc[:]), r32(sqt[:]), [b_ones, b_sqt], [b_pp])
                    c.ts(rn[:], pp, EPS, None, ALU.add, None, [b_pp], [b_rn])
                    c.act(rn[:], rn[:], AF.Sqrt, [b_rn], [b_rn])
                    c.op("dve", lambda e: e.reciprocal(out=rn[:], in_=rn[:]), [b_rn], [b_rn])
                    c.stt(qkv[:, u, :], qkv[:, u, :], (SCALE if u < 2 else 1.0), rn[:], ALU.mult, ALU.mult,
                          [b_qkv[u], b_rn], [b_qkv[u]])
                for tt in range(4):
                    pp, b_pp = pproj.get()
                    for kc in range(16):
                        c.mm(pp[:, 0:260], hT[:, kc, tt * 128:(tt + 1) * 128], wtm[:, kc, :], [b_wtm, b_hT], [b_pp],
                             start=(kc == 0), stop=(kc == 15), inc=(kc == 15))
                    c.act(gsil[:, tt, :], pp[:, 0:256], AF.Silu, [b_pp], [b_gsil[tt]])
                    c.act(gb[:, tt, 0:2], pp[:, 256:258], AF.Sigmoid, [b_pp], [b_gb[tt]])
                    for h in range(2):
                        c.act(sp1[:, h:h + 1], pp[:, 258 + h:259 + h], AF.Exp, [b_pp, b_dtb], [b_sp1], bias=dtbc[:, h:h + 1], scale=1.0)
                    c.act(sp1[:], sp1[:], AF.Ln, [b_sp1], [b_sp1], bias=1.0, scale=1.0)
                    c.tt(gb[:, tt, 2:4], sp1[:], nega[:], ALU.mult, [b_sp1, b_nega], [b_gb[tt]])

                if stop == "A2b" and blk == 0:
                    c.drain()
                    return nc
                def tile_ctx(tt):
                    ti = blk * 4 + tt
                    return ti, slice(tt * 128, (tt + 1) * 128), [units[ti % 2][h] for h in range(2)], b_gb[tt]

                def g_pre(tt, h):
                    ti, tsl, us, bg = tile_ctx(tt)
                    u = us[h]
                    beta = gb[:, tt, h:h + 1]
                    g = gb[:, tt, 2 + h:3 + h]
                    yield
                    p1, bp1 = pg.get()
                    c.mm(p1[:, 0:2], Uc[:], gb[:, tt, 2:4], [b_U, bg], [bp1])
                    c.cp(u["bcol"][:], p1[:, h:h + 1], [bp1], [B(u, "bcol")])
                    yield
                    p2, bp2 = pg.get()
                    c.mm(p2[:, 0:2], onesc[:], gb[:, tt, 2:4], [b_ones, bg], [bp2])
                    c.act(u["ebl"][:], p2[:, h:h + 1], AF.Exp, [bp2], [B(u, "ebl")])
                    c.cp(u["ss2"][:], p2[:, h:h + 1], [bp2], [B(u, "ss2")])
                    c.act(u["edec"][:], u["bcol"][:], AF.Exp, [B(u, "bcol"), B(u, "ss2")], [B(u, "edec")], scale=-1.0, bias=u["ss2"][:, 0:1])
                    c.act(u["eb"][:], u["bcol"][:], AF.Exp, [B(u, "bcol")], [B(u, "eb")])
                    c.tt(u["bek"][:], u["eb"][:], beta, ALU.mult, [B(u, "eb"), bg], [B(u, "bek")])
                    c.ts(u["Gs"][:], SLc[:], g, None, ALU.mult, None, [b_SL, bg], [B(u, "Gs")])
                    yield
                    p3, bp3 = pg.get()
                    c.mm(p3, Uc[:], u["Gs"][:], [b_U, B(u, "Gs")], [bp3])
                    c.act(u["Dm"][:], p3, AF.Exp, [bp3], [B(u, "Dm")])
                    c.stt(u["Dm"][:], u["Dm"][:], beta, SLc[:], ALU.mult, ALU.mult, [B(u, "Dm"), bg, b_SL], [B(u, "Dm")])
                    yield
                    p4, bp4 = pg.get()
                    c.mm(p4, u["Gs"][:], Uc[:], [b_U, B(u, "Gs")], [bp4])
                    c.act(u["DTm"][:], p4, AF.Exp, [bp4], [B(u, "DTm")])
                    c.tt(u["DTm"][:], u["DTm"][:], ILTc[:], ALU.mult, [B(u, "DTm"), b_ILT], [B(u, "DTm")])
                    u = us[h]
                    beta = gb[:, tt, h:h + 1]
                    KT = qkv[:, 2 + h, tsl]
                    QT = qkv[:, h, tsl]
                    VT = qkv[:, 4 + h, tsl]
                    yield
                    p1, bp1 = pg.get()
                    c.mm(p1, r32(KT), r32(KT), [b_qkv[2 + h]], [bp1])
                    c.tt(u["A"][:], p1, u["Dm"][:], ALU.mult, [bp1, B(u, "Dm")], [B(u, "A")])
                    yield
                    p2, bp2 = pg.get()
                    c.mm(p2, r32(KT), r32(QT), [b_qkv[2 + h], b_qkv[h]], [bp2])
                    c.tt(u["QK"][:], p2, u["DTm"][:], ALU.mult, [bp2, B(u, "DTm")], [B(u, "QK")])
                    yield
                    p3, bp3 = pg.get()
                    c.tr(p3, u["A"][:], identf[:], [B(u, "A"), b_identf], [bp3])
                    c.cp(u["AT"][:], p3, [bp3], [B(u, "AT")], eng="act")
                    yield
                    p4, bp4 = pg.get()
                    c.tr(p4, VT, identf[:], [b_qkv[4 + h], b_identf], [bp4])
                    c.ts(u["Xa"][:, 0:128], p4, beta, None, ALU.mult, None, [bp4, bg], [B(u, "Xa")])
                    yield
                    p5, bp5 = pg.get()
                    c.tr(p5, KT, identf[:], [b_qkv[2 + h], b_identf], [bp5])
                    c.ts(u["Xa"][:, 128:256], p5, u["bek"][:, 0:1], None, ALU.mult, None, [bp5, B(u, "bek")], [B(u, "Xa")])
                    c.act(u["kdec"][:], p5, AF.Identity, [bp5, B(u, "edec")], [B(u, "kdec")], scale=u["edec"][:, 0:1])
                    u = us[h]
                    X = [(u["Xa"], B(u, "Xa")), (u["Xb"], B(u, "Xb"))]
                    c.stt(u["FT"][:], u["AT"][:], -1.0, identf[:], ALU.mult, ALU.add, [B(u, "AT"), b_identf], [B(u, "FT")])
                    xi = 0
                    Pc, PcT, bPc, bPcT = u["A"], u["AT"], B(u, "A"), B(u, "AT")
                    pp_ = [(u["Pa"], u["PaT"], "Pa", "PaT"), (u["Pb"], u["PbT"], "Pb", "PbT")]
                    for lvl in range(7):
                        Xc, bXc = X[xi]
                        Xn, bXn = X[1 - xi]
                        yield
                        pa, bpa = pg.get()
                        pa2, bpa2 = pg.get()
                        c.mm(pa, r32(u["FT"][:]), r32(Xc[:, 0:128]), [B(u, "FT"), bXc], [bpa])
                        c.mm(pa2, r32(u["FT"][:]), r32(Xc[:, 128:256]), [B(u, "FT"), bXc], [bpa2])
                        c.cp(Xn[:, 0:128], pa, [bpa], [bXn], eng="act")
                        c.cp(Xn[:, 128:256], pa2, [bpa2], [bXn])
                        xi = 1 - xi
                        if lvl == 6:
                            break
                        Pn, PnT, nPn, nPnT = pp_[lvl % 2]
                        yield
                        pq, bpq = pg.get()
                        c.mm(pq, r32(Pc[:]), r32(PcT[:]), [bPc, bPcT], [bpq])
                        c.cp(PnT[:], pq, [bpq], [B(u, nPnT)], eng="act")
                        c.tt(u["FT"][:], pq, identf[:], ALU.add, [bpq, b_identf], [B(u, "FT")])
                        if lvl < 5:
                            yield
                            pq2, bpq2 = pg.get()
                            c.mm(pq2, r32(PcT[:]), r32(Pc[:]), [bPc, bPcT], [bpq2])
                            c.cp(Pn[:], pq2, [bpq2], [B(u, nPn)])
                        Pc, PcT, bPc, bPcT = Pn, PnT, B(u, nPn), B(u, nPnT)
                    u["Xf"], u["bXf"] = X[xi]
                    yield
                    pw, bpw = pg.get()
                    c.tr(pw, u["Xf"][:, 128:256], identf[:], [u["bXf"], b_identf], [bpw])
                    c.cp(u["WT"][:], pw, [bpw], [B(u, "WT")], eng="act")

                def g_seq(tt, h):
                    ti, tsl, us, bg = tile_ctx(tt)
                    u = us[h]
                    QT = qkv[:, h, tsl]
                    Xf, bXf = u["Xf"], u["bXf"]
                    yield
                    p1, bp1 = pg.get()
                    c.mm(p1, r32(u["WT"][:]), r32(S[h][:]), [B(u, "WT"), b_S[h]], [bp1])
                    c.tt(u["vnew"][:], Xf[:, 0:128], p1, ALU.subtract, [bXf, bp1], [B(u, "vnew")])
                    yield
                    p2, bp2 = pg.get()
                    c.mm(p2, r32(QT), r32(S[h][:]), [b_qkv[h], b_S[h]], [bp2])
                    c.act(u["o1s"][:], p2, AF.Identity, [bp2, B(u, "eb")], [B(u, "o1s")], scale=u["eb"][:, 0:1])
                    yield
                    p3, bp3 = pg.get()
                    c.mm(p3, r32(u["QK"][:]), r32(u["vnew"][:]), [B(u, "QK"), B(u, "vnew")], [bp3])
                    c.tt(u["o"][:], u["o1s"][:], p3, ALU.add, [B(u, "o1s"), bp3], [B(u, "o")])
                    yield
                    p4, bp4 = pg.get()
                    c.mm(p4, r32(u["kdec"][:]), r32(u["vnew"][:]), [B(u, "kdec"), B(u, "vnew")], [bp4])
                    c.stt(S[h][:], S[h][:], u["ebl"][:, 0:1], p4, ALU.mult, ALU.add, [b_S[h], B(u, "ebl"), bp4], [b_S[h]])
                    c.act(u["o1s"][:], u["o"][:], AF.Square, [B(u, "o")], [B(u, "o1s"), B(u, "ss2")], accum_out=u["ss2"][:])
                    c.ts(u["rs2"][:], u["ss2"][:], 1.0 / 128, EPS, ALU.mult, ALU.add, [B(u, "ss2")], [B(u, "rs2")])
                    c.act(u["rs2"][:], u["rs2"][:], AF.Sqrt, [B(u, "rs2")], [B(u, "rs2")])
                    c.op("dve", lambda e: e.reciprocal(out=u["rs2"][:], in_=u["rs2"][:]), [B(u, "rs2")], [B(u, "rs2")])
                    c.stt(u["o"][:], u["o"][:], u["rs2"][:, 0:1], gnwr[:], ALU.mult, ALU.mult, [B(u, "o"), B(u, "rs2"), b_gnw], [B(u, "o")])
                    c.tt(u["y"][:], u["o"][:], gsil[:, tt, h * 128:(h + 1) * 128], ALU.mult, [B(u, "o"), b_gsil[tt]], [B(u, "y")])
                    yield
                    py, bpy = pg.get()
                    pyb = py.bitcast(BF16)
                    c.tr(pyb[:, 0:128], u["y"][:], identb[:], [B(u, "y"), b_identb], [bpy])
                    c.cp(yTg[:, h, ti * 128:(ti + 1) * 128], pyb[:, 0:128], [bpy], [b_yTg[blk]], eng="act")

                def run_rr(gens):
                    gens = list(gens)
                    while gens:
                        for g_ in list(gens):
                            try:
                                next(g_)
                            except StopIteration:
                                gens.remove(g_)

                for step in range(5):
                    gens = []
                    if step < 4:
                        gens += [g_pre(step, 0), g_pre(step, 1)]
                    if step > 0:
                        gens += [g_seq(step - 1, 0), g_seq(step - 1, 1)]
                    run_rr(gens)
            if debug:
                bd = Buf(); dbg_bufs.append(bd)
                c.dma("sp", yTg_d, yTg[:], R=b_yTg, W=[bd])
        c.barrier()
        c.es = es0
        if stop == "A":
            c.drain()
            return nc

        with ExitStack() as es:
            c.es = es
            wfm = c.sb("wBfm", [128, 16, 1024], BF16)
            wtm = c.sb("wBtm", [128, 16, 262], BF16)
            b_wfm, b_wtm = Buf(), Buf()
            c.dma("pool", wfm[:], wB_fm.rearrange("(kc p) n -> p kc n", p=128), W=[b_wfm])
            c.dma("pool", wtm[:], wB_tm.rearrange("(kc p) n -> p kc n", p=128), W=[b_wtm])
            w1 = [c.sb("w1", [128, 32, 256], BF16) for _ in range(2)]
            b_w1 = [Buf(), Buf()]
            for i, src in enumerate((w1k, w1v)):
                for half in range(2):
                    c.dma("pool", w1[i][:, half * 16:(half + 1) * 16, :],
                          src[half * 2048:(half + 1) * 2048, :].rearrange("(l p) n -> p l n", p=128), W=[b_w1[i]])
            w2 = [c.sb("w2", [128, 2, 128], BF16) for _ in range(2)]
            b_w2 = [Buf(), Buf()]
            for i, src in enumerate((w2k, w2v)):
                c.dma("pool", w2[i][:], src.rearrange("(a p) n -> p a n", p=128), W=[b_w2[i]])
            pe_ = [c.sb("pe", [128, 32], BF16) for _ in range(2)]
            b_pe = [Buf(), Buf()]
            for i, src in enumerate((pek, pev)):
                c.dma("pool", pe_[i][:], src, W=[b_pe[i]])
            wo = c.sb("wo", [128, 4, D], BF16)
            b_wo = Buf()
            c.dma("pool", wo[:], wout.rearrange("(a p) n -> p a n", p=128), W=[b_wo])
            triU, b_triU = load_const("triU", k_triU, [128, 256], BF16)
            triL, b_triL = load_const("triL", k_triL, [128, 256], BF16)
            Ec, b_E = load_const("Ec", k_E, [64, 32, 128], BF16)
            cb0, b_cb0 = load_const("cb0", k_cb0, [128, 17, 128], BF16)
            fb, b_fb = load_const("fb", k_fb, [128, 32, 64], BF16)
            ovl, b_ovl = load_const("ovl", k_ovl, [128, 2, 65], F32)

            kcmp = [c.sb("kcmp", [128, 16 + T], BF16) for _ in range(2)]
            b_kcmp = [Buf(), Buf()]
            for i in range(2):
                c.op("pool", lambda e: e.memset(kcmp[i][:], 0.0), [], [b_kcmp[i]])
            kslc = c.sb("kslc", [128, T], BF16)
            kwin = c.sb("kwin", [128, T], BF16)
            b_kslc, b_kwin = Buf(), Buf()
            vslc = c.sb("vslc", [128, 32, 130], BF16)
            vwin = c.sb("vwin", [128, 32, 130], BF16)
            b_vslc, b_vwin = Buf(), Buf()
            c.op("pool", lambda e: e.memset(vslc[:, :, 128:130], 1.0), [], [b_vslc])
            c.op("pool", lambda e: e.memset(vwin[:, :, 128:130], 1.0), [], [b_vwin])
            kcT = c.sb("kcT", [128, 256], BF16)
            b_kcT = Buf()
            c.op("pool", lambda e: e.memset(kcT[:], 0.0), [], [b_kcT])
            vcx = c.sb("vcx", [128, 2, 194], BF16)
            b_vcx = Buf()
            c.op("pool", lambda e: e.memset(vcx[:], 0.0), [], [b_vcx])
            c.cp(vcx[:, :, 128:193], ovl[:], [b_ovl, b_vcx], [b_vcx])

            hT = c.sb("hTb", [128, 16, 512], BF16)
            b_hT = Buf()
            qT = c.sb("qT", [128, 4, 512], BF16)
            b_qT = Buf()
            gn = c.sb("gn", [128, 4, 6], F32)
            b_gn = [Buf() for _ in range(4)]
            hs = [c.sb("hs", [128, 2, 32], BF16) for _ in range(2)]
            b_hs = [Buf(), Buf()]
            peb = [c.sb("peb", [128, 2], F32) for _ in range(2)]
            b_peb = [Buf(), Buf()]
            NET = 5
            ET = [c.sb("ET", [128, 256], BF16) for _ in range(NET)]
            b_ET = [Buf() for _ in range(NET)]
            eti = [0]
            imp = c.sb("imp", [128, 64], F32)
            imp2 = c.sb("imp2", [128, 64], F32)
            mx = c.sb("mx", [128, 16], F32)
            b_imp, b_imp2, b_mx = Buf(), Buf(), Buf()
            selb = c.sb("selb", [128, 64], BF16)
            b_selb = Buf()
            selbT = c.sb("selbT", [64, 2, 128], BF16)
            b_selbT = Buf()
            oacc = c.sb("oacc", [128, 2, 128], F32)
            b_oacc = Buf()
            rsm = c.sb("rsm", [128, 8], F32)
            b_rsm = Buf()
            ynb = c.sb("ynb", [128, 2, 128], BF16)
            b_ynb = Buf()
            yTn = c.sb("yTn", [128, 2, 512], BF16)
            b_yTn = Buf()
            pout = [c.sb("pout", [128, 512], F32) for _ in range(2)]
            b_pout = [Buf(), Buf()]

            pproj = PSlots(c, "pprojB", 2, 1)
            pst = PSlots(c, "pst", 2, 1)
            pst_attn = PSlots(c, "pst_attn", 0, 1)
            pst_attn.slots = [pst.slots[0], pproj.slots[0], pst.slots[1], pproj.slots[1]]
            pacc = [c.ps("pacc", [128, 512]) for _ in range(4)]
            b_pacc = [Buf(excl=True) for _ in range(4)]

            for i in range(2):
                for half in range(2):
                    pp, b_pp = pproj.get()
                    for l in range(32):
                        c.mm(pp[:, 0:1], w1[i][:, l, half * 128:(half + 1) * 128], pe_[i][:, l:l + 1],
                             [b_w1[i], b_pe[i]], [b_pp], start=(l == 0), stop=(l == 31), inc=(l == 31))
                    c.cp(peb[i][:, half:half + 1], pp[:, 0:1], [b_pp], [b_peb[i]])

            for blk in range(8):
                if blk == 0:
                    c.dma("sp", hT[:], hT_d[:, :, 0:512], R=[b_hTd[0]], W=[b_hT])
                for u in range(8):
                    pp, b_pp = pproj.get()
                    for kc in range(16):
                        c.mm(pp, wfm[:, kc, u * 128:(u + 1) * 128], hT[:, kc, :], [b_wfm, b_hT], [b_pp],
                             start=(kc == 0), stop=(kc == 15), inc=(kc == 15))
                    eng = "act" if u % 2 else "dve"
                    if u < 4:
                        c.cp(qT[:, u, :], pp, [b_pp], [b_qT], eng=eng)
                    elif u < 6:
                        c.cp(kcmp[u - 4][:, 16 + blk * 512:16 + (blk + 1) * 512], pp, [b_pp], [b_kcmp[u - 4]], eng=eng)
                    elif u == 6:
                        c.cp(kslc[:, blk * 512:(blk + 1) * 512], pp, [b_pp], [b_kslc], eng=eng)
                    else:
                        c.cp(kwin[:, blk * 512:(blk + 1) * 512], pp, [b_pp], [b_kwin], eng=eng)
                for tt in range(4):
                    ti = blk * 4 + tt
                    pp, b_pp = pproj.get()
                    for kc in range(16):
                        c.mm(pp[:, 0:262], hT[:, kc, tt * 128:(tt + 1) * 128], wtm[:, kc, :], [b_wtm, b_hT], [b_pp],
                             start=(kc == 0), stop=(kc == 15), inc=(kc == 15))
                    c.cp(vslc[:, ti, 0:128], pp[:, 0:128], [b_pp], [b_vslc], eng="act")
                    c.cp(vwin[:, ti, 0:128], pp[:, 128:256], [b_pp], [b_vwin])
                    c.act(gn[:, tt, :], pp[:, 256:262], AF.Sigmoid, [b_pp], [b_gn[tt]])
                if blk < 7:
                    c.dma("sp", hT[:], hT_d[:, :, (blk + 1) * 512:(blk + 2) * 512], R=[b_hTd[blk + 1]], W=[b_hT])
                m0 = 32 * blk
                for i in range(2):
                    for half in range(2):
                        pp, b_pp = pproj.get()
                        for l in range(32):
                            rhs = kcmp[i][:, 16 * m0 + l: 16 * m0 + l + 16 * 31 + 1: 16]
                            c.mm(pp[:, 0:32], w1[i][:, l, half * 128:(half + 1) * 128], rhs, [b_w1[i], b_kcmp[i]], [b_pp],
                                 start=(l == 0), stop=(l == 31), inc=(l == 31))
                        c.act(hs[i][:, half, :], pp[:, 0:32], AF.Silu, [b_pp, b_peb[i]], [b_hs[i]], bias=peb[i][:, half:half + 1], scale=1.0)
                pp, b_pp = pproj.get()
                for half in range(2):
                    c.mm(pp[:, 0:32], w2[0][:, half, :], hs[0][:, half, :], [b_w2[0], b_hs[0]], [b_pp], start=(half == 0), stop=(half == 1), inc=(half == 1))
                c.cp(kcT[:, m0:m0 + 32], pp[:, 0:32], [b_pp], [b_kcT])
                pp, b_pp = pproj.get()
                for half in range(2):
                    c.mm(pp[0:32, 0:128], hs[1][:, half, :], w2[1][:, half, :], [b_w2[1], b_hs[1]], [b_pp], start=(half == 0), stop=(half == 1), inc=(half == 1))
                mc_, mp = m0 // 128, m0 % 128
                c.cp(vcx[mp:mp + 32, mc_, 0:128], pp[0:32, 0:128], [b_pp], [b_vcx], eng="act")
                if blk == 0:
                    c.op("pool", lambda e: e.memset(vcx[0:1, 0, :], 0.0), [], [b_vcx])

                for tt in range(4):
                    qb = blk * 4 + tt
                    tsl = slice(tt * 128, (tt + 1) * 128)
                    pairs = []
                    SK = 3

                    def mk_s1(lhs_k, rb_k, ncol, rhs_q, extra, pre=None):
                        def s1(slot):
                            if pre is not None:
                                pre()
                            st, b_st = pst_attn.get()
                            c.mm(st[:, 0:ncol], lhs_k, rhs_q, rb_k + [b_qT], [b_st],
                                 start=True, stop=(len(extra) == 0), inc=(len(extra) == 0))
                            for xi, (o_, l_, r_, rb) in enumerate(extra):
                                last = xi == len(extra) - 1
                                c.mm(o_(st), l_, r_, rb, [b_st], start=False, stop=last, inc=last)
                            e = eti[0] % NET
                            eti[0] += 1
                            slot["e"] = e
                            c.act(ET[e][:, 0:ncol], st[:, 0:ncol], AF.Exp, [b_st], [b_ET[e]], scale=SCALE)
                        return s1

                    mcs = [0] + ([1] if qb >= 16 else [])

                    def fin_cmp():
                        for g in range(4):
                            pa = pacc[g]
                            c.ts(rsm[:, g:g + 1], pa[:, 192:193], 1e-30, None, ALU.max, None, [b_pacc[g]], [b_rsm])
                            c.op("dve", lambda e: e.reciprocal(out=rsm[:, g:g + 1], in_=rsm[:, g:g + 1]), [b_rsm], [b_rsm])
                            c.stt(imp[:], pa[:, 128:192], rsm[:, g:g + 1], (fb[:, qb, :] if g == 0 else imp[:]), ALU.mult, ALU.add,
                                  [b_pacc[g], b_rsm, b_fb, b_imp], [b_imp])
                            if g < 2:
                                c.tt(rsm[:, 4 + g:5 + g], rsm[:, g:g + 1], gn[:, tt, g:g + 1], ALU.mult, [b_rsm, b_gn[tt]], [b_rsm])
                                c.ts(oacc[:, g, :], pa[:, 0:128], rsm[:, 4 + g:5 + g], None, ALU.mult, None, [b_pacc[g], b_rsm], [b_oacc])
                        c.op("dve", lambda e: e.max(out=mx[:, 0:8], in_=imp[:]), [b_imp], [b_mx])
                        c.op("dve", lambda e: e.match_replace(out=imp2[:], in_to_replace=mx[:, 0:8], in_values=imp[:], imm_value=-3.0e4), [b_imp, b_mx], [b_imp2])
                        c.op("dve", lambda e: e.max(out=mx[:, 8:16], in_=imp2[:]), [b_imp2], [b_mx])
                        c.ts(selb[:], imp[:], mx[:, 15:16], NEG, ALU.is_lt, ALU.mult, [b_imp, b_mx], [b_selb])

                    for k, mc in enumerate(mcs):
                        delta = qb - 16 * mc
                        di = min(delta, 16) if mc == 0 else delta
                        for hf in range(2):
                            extra = [((lambda st, gg=gg: st[:, gg * 128:(gg + 1) * 128]), identb[:], cb0[:, di, :], [b_identb, b_cb0]) for gg in range(2)]
                            s1 = mk_s1(kcT[:, mc * 128:(mc + 1) * 128], [b_kcT], 256, qT[:, 2 * hf:2 * hf + 2, tsl], extra)

                            def s2(slot, k=k, mc=mc, hf=hf):
                                e = slot["e"]
                                lastk = k == len(mcs) - 1
                                for gg in range(2):
                                    g = 2 * hf + gg
                                    c.mm(pacc[g][:, 0:193], ET[e][:, gg * 128:(gg + 1) * 128], vcx[:, mc, 0:193], [b_ET[e], b_vcx], [b_pacc[g]],
                                         start=(k == 0), stop=lastk)
                                if lastk and hf == 1:
                                    fin_cmp()
                            pairs.append((s1, s2))
                    i_cmp_last = len(pairs) - 1

                    def b4_pe():
                        st, b_st = pst.get()
                        stb = st.bitcast(BF16)
                        c.tr(stb[0:64, 0:128], selb[:], identb[:], [b_selb, b_identb], [b_st])
                        c.cp(selbT[:, 0, :], stb[0:64, 0:128], [b_st], [b_selbT])
                        c.cp(selbT[:, 1, :], stb[0:64, 0:128], [b_st], [b_selbT])

                    for br in (1, 0):
                        if br == 0:
                            kcs = list(range(0, qb + 1))
                            KT_, b_KT, V_, b_V = kslc, b_kslc, vslc, b_vslc
                            pg0 = 0
                        else:
                            kcs = list(range(max(0, qb - 4), qb + 1))
                            KT_, b_KT, V_, b_V = kwin, b_kwin, vwin, b_vwin
                            pg0 = 2

                        def fin_br(br=br, pg0=pg0):
                            for g in range(2):
                                pa, bpa = pacc[pg0 + g], b_pacc[pg0 + g]
                                c.op("dve", lambda e: e.reciprocal(out=rsm[:, g:g + 1], in_=pa[:, 128:129]), [bpa], [b_rsm])
                                gi = (1 + br) * 2 + g
                                c.tt(rsm[:, 4 + g:5 + g], rsm[:, g:g + 1], gn[:, tt, gi:gi + 1], ALU.mult, [b_rsm, b_gn[tt]], [b_rsm])
                                c.stt(oacc[:, g, :], pa[:, 0:128], rsm[:, 4 + g:5 + g], oacc[:, g, :], ALU.mult, ALU.add,
                                      [bpa, b_rsm, b_oacc], [b_oacc])

                        if br == 0:
                            while len(pairs) < i_cmp_last + SK + 1:
                                pairs.append((lambda slot: None, lambda slot: None))
                        for k, kc in enumerate(kcs):
                            extra = []
                            full = (lambda st: st[:, 0:256])
                            if br == 0:
                                extra.append((full, Ec[:, kc, :], selbT[:].rearrange("p a b -> p (a b)"), [b_E, b_selbT]))
                            if kc == qb:
                                extra.append((full, identb[:], triU[:], [b_identb, b_triU]))
                            if br == 1 and kc == qb - 4:
                                extra.append((full, identb[:], triL[:], [b_identb, b_triL]))
                            s1 = mk_s1(KT_[:, kc * 128:(kc + 1) * 128], [b_KT], 256, qT[:, 0:2, tsl], extra,
                                       pre=(b4_pe if (br == 0 and k == 0) else None))

                            def s2(slot, k=k, kc=kc, n=len(kcs), V_=V_, b_V=b_V, pg0=pg0, fin=fin_br):
                                e = slot["e"]
                                for g in range(2):
                                    c.mm(pacc[pg0 + g][:, 0:129], ET[e][:, g * 128:(g + 1) * 128], V_[:, kc, 0:129], [b_ET[e], b_V], [b_pacc[pg0 + g]],
                                         start=(k == 0), stop=(k == n - 1))
                                if k == n - 1:
                                    fin()
                            pairs.append((s1, s2))

                    slots = [dict() for _ in pairs]
                    for i in range(len(pairs) + SK):
                        if i < len(pairs):
                            pairs[i][0](slots[i])
                        if i >= SK:
                            pairs[i - SK][1](slots[i - SK])
                    c.cp(ynb[:], oacc[:], [b_oacc], [b_ynb], eng="act")
                    for g in range(2):
                        st, b_st = pst.get()
                        stb = st.bitcast(BF16)
                        c.tr(stb[:, 0:128], ynb[:, g, :], identb[:], [b_ynb, b_identb], [b_st])
                        c.cp(yTn[:, g, tsl], stb[:, 0:128], [b_st], [b_yTn])
                if debug:
                    bd = Buf(); dbg_bufs.append(bd)
                    c.dma("sp", yTn_d[:, :, blk * 512:(blk + 1) * 512], yTn[:], R=[b_yTn], W=[bd])
                for tt in range(4):
                    ti = blk * 4 + tt
                    for cbk in range(4):
                        po, b_po = pout[cbk % 2], b_pout[cbk % 2]
                        pp, b_pp = pproj.get()
                        for fc in range(4):
                            lhs = yTg[:, fc, ti * 128:(ti + 1) * 128] if fc < 2 else yTn[:, fc - 2, tt * 128:(tt + 1) * 128]
                            rb = [b_yTg[blk]] if fc < 2 else [b_yTn]
                            c.mm(pp, lhs, wo[:, fc, cbk * 512:(cbk + 1) * 512], rb + [b_wo], [b_pp], start=(fc == 0), stop=(fc == 3), inc=(fc == 3))
                        c.cp(po[:], pp, [b_pp], [b_po], eng=("act" if cbk % 2 else "dve"))
                        b_part = Buf()
                        b_parts.append(b_part)
                        c.dma("sp", partial.ap()[ti * 128:(ti + 1) * 128, cbk * 512:(cbk + 1) * 512], po[:], R=[b_po], W=[b_part])
        c.barrier()
        c.es = es0
        if stop == "B":
            c.drain()
            return nc
        if debug:
            bd = Buf(); dbg_bufs.append(bd)
            c.dma("sp", part_d, partial.ap(), R=b_parts, W=[bd])
        b_rs = Buf()
        b_rs0 = Buf()
        c.op("pool", lambda e: e.collective_compute("ReduceScatter", ALU.add, replica_groups=[[0, 1, 2, 3], [4, 5, 6, 7]],
                                                    ins=[partial.ap().opt()], outs=[rs_out.ap().opt()]), b_parts, [b_rs0])
        c.op("pool", lambda e: e.collective_compute("AllReduce", ALU.add, replica_groups=[[0, 1, 2, 3], [4, 5, 6, 7]],
                                                    ins=[fence_in.ap().opt()], outs=[fence_out.ap().opt()]), [b_rs0], [b_rs])

        ada_part(32, 96, True)
        if stop == "rs":
            c.drain()
            return nc
        with ExitStack() as es:
            c.es = es
            h2T = c.sb("h2T", [128, 16, 1024], BF16)
            b_h2T = Buf()
            acc = c.sb("acc", [128, 8, D], F32)
            b_acc = [Buf() for _ in range(8)]
            comb = c.sb("comb", [128, 8, 65], F32)
            b_comb = Buf()
            ptr = PSlots(c, "ptr2", 1, 1)
            pgu = PSlots(c, "pgu", 4, 1)
            pd = PSlots(c, "pd", 3, 1)
            ptr0 = ptr.slots[0]
            ptr.slots += pd.slots[1:3]
            b_x1d = [Buf() for _ in range(8)]
            with ExitStack() as es2:
                c.es = es2
                ga = c.sb("ga", [128, D], F32)
                b_ga = Buf()
                c.dma("sp", ga[:], modD[32:48, :].rearrange("a b -> (a b)").partition_broadcast(128), R=[b_modD], W=[b_ga])
                rwt = c.sb("rwt", [128, 16, 64], BF16)
                b_rwt = Buf()
                c.dma("pool", rwt[:], rw.rearrange("(kc p) n -> p kc n", p=128), W=[b_rwt])
                rbr, b_rbr = load_const("rbr", rbias, [128, 64], F32)
                xts = [c.sb("xt2", [128, D], F32) for _ in range(2)]
                b_xts = [Buf(), Buf()]
                ats = [c.sb("at2", [128, D], F32) for _ in range(2)]
                b_ats = [Buf(), Buf()]
                ntmp = norm_tmp()
                sc = c.sb("sc", [128, 64], F32)
                sv = c.sb("sv", [128, 64], F32)
                mx8 = c.sb("mx8", [128, 8], F32)
                den = c.sb("den", [128, 1], F32)
                b_sc, b_sv, b_mx8, b_den = Buf(), Buf(), Buf(), Buf()
                c.op("pool", lambda e: e.memset(comb[:, :, 64:65], 1.0), [], [b_comb])
                for tt in range(8):
                    xt, b_xt = xts[tt % 2], b_xts[tt % 2]
                    at, b_at = ats[tt % 2], b_ats[tt % 2]
                    c.dma("sp", xt[:], xq[tt * 128:(tt + 1) * 128, :], W=[b_xt])
                    c.dma("sp", at[:], rs_out.ap()[tt * 128:(tt + 1) * 128, :], R=[b_rs], W=[b_at])
                    c.tt(at[:], at[:], ga[:], ALU.mult, [b_at, b_ga], [b_at])
                    c.tt(xt[:], xt[:], at[:], ALU.add, [b_xt, b_at], [b_xt])
                    c.dma("sp", x1_d[tt * 128:(tt + 1) * 128, :], xt[:], R=[b_xt], W=[b_x1d[tt]])
                    norm_tile(xt, b_xt, A2, B2, h2T, b_h2T, tt * 128, ntmp, ptr)
                    pp, b_pp = pd.get()
                    for kc in range(16):
                        c.mm(pp[:, 0:64], h2T[:, kc, tt * 128:(tt + 1) * 128], rwt[:, kc, :], [b_h2T, b_rwt], [b_pp],
                             start=(kc == 0), stop=(kc == 15), inc=(kc == 15))
                    c.act(sc[:], pp[:, 0:64], AF.Sigmoid, [b_pp], [b_sc])
                    c.tt(sv[:], sc[:], rbr[:], ALU.add, [b_sc, b_rbr], [b_sv])
                    c.op("dve", lambda e: e.max(out=mx8[:], in_=sv[:]), [b_sv], [b_mx8])
                    c.ts(sv[:], sv[:], mx8[:, 7:8], None, ALU.is_ge, None, [b_sv, b_mx8], [b_sv])
                    c.tt(sv[:], sv[:], sc[:], ALU.mult, [b_sv, b_sc], [b_sv])
                    c.op("dve", lambda e: e.reduce_sum(out=den[:], in_=sv[:], axis=mybir.AxisListType.X), [b_sv], [b_den])
                    c.op("dve", lambda e: e.reciprocal(out=den[:], in_=den[:]), [b_den], [b_den])
                    c.ts(comb[:, tt, 0:64], sv[:], den[:, 0:1], 2.5, ALU.mult, ALU.mult, [b_sv, b_den, b_comb], [b_comb])
            c.barrier()
            pd.slots.append(ptr0)
            es3 = ExitStack()
            es3.__enter__()
            c.es = es3
            wgu = [[c.sb("wg", [128, 16, 128], BF16), c.sb("wu", [128, 16, 128], BF16)] for _ in range(4)]
            b_wgu = [[Buf(), Buf()] for _ in range(4)]
            wdt = [c.sb("wd", [128, 4, D], BF16) for _ in range(2)]
            b_wdt = [Buf(), Buf()]
            actT = [c.sb("actT", [128, 4, 1024], BF16) for _ in range(2)]
            b_actT = [Buf(), Buf()]
            sgt = [c.sb("sgt", [128, 512], BF16) for _ in range(2)]
            b_sgt = [Buf(), Buf()]
            ui = 0
            for e_ in list(range(n_exp)) + [64]:
                if e_ < 64:
                    sg_, su_, sd_ = ewg[e_], ewu[e_], ewd[e_]
                else:
                    sg_, su_, sd_ = swg, swu, swd
                wd_t, b_wd = wdt[e_ % 2], b_wdt[e_ % 2]
                for half in range(2):
                    c.dma("pool", wd_t[:, half * 2:(half + 1) * 2, :], sd_[half * 256:(half + 1) * 256, :].rearrange("(a p) n -> p a n", p=128), W=[b_wd])
                aT, b_aT = actT[e_ % 2], b_actT[e_ % 2]
                for fc in range(4):
                    (wg_t, wu_t), (b_wg, b_wu) = wgu[ui % 4], b_wgu[ui % 4]
                    ui += 1
                    c.dma("pool", wg_t[:], sg_[:, fc * 128:(fc + 1) * 128].rearrange("(kc p) n -> p kc n", p=128), W=[b_wg])
                    c.dma("pool", wu_t[:], su_[:, fc * 128:(fc + 1) * 128].rearrange("(kc p) n -> p kc n", p=128), W=[b_wu])
                    for tb in range(2):
                        pG, b_pG = pgu.get()
                        pU, b_pU = pgu.get()
                        for kc in range(16):
                            c.mm(pG, wg_t[:, kc, :], h2T[:, kc, tb * 512:(tb + 1) * 512], [b_wg, b_h2T], [b_pG], start=(kc == 0), stop=(kc == 15), inc=(kc == 15))
                        for kc in range(16):
                            c.mm(pU, wu_t[:, kc, :], h2T[:, kc, tb * 512:(tb + 1) * 512], [b_wu, b_h2T], [b_pU], start=(kc == 0), stop=(kc == 15), inc=(kc == 15))
                        k2 = (fc * 2 + tb) % 2
                        c.act(sgt[k2][:], pG, AF.Silu, [b_pG], [b_sgt[k2]])
                        c.tt(aT[:, fc, tb * 512:(tb + 1) * 512], sgt[k2][:], pU, ALU.mult, [b_sgt[k2], b_pU], [b_aT])
                for tt in range(8):
                    for cbk in range(4):
                        pD, b_pD = pd.get()
                        for fc in range(4):
                            c.mm(pD, aT[:, fc, tt * 128:(tt + 1) * 128], wd_t[:, fc, cbk * 512:(cbk + 1) * 512], [b_aT, b_wd], [b_pD],
                                 start=(fc == 0), stop=(fc == 3), inc=(fc == 3))
                        dst = acc[:, tt, cbk * 512:(cbk + 1) * 512]
                        if e_ == (0 if n_exp else 64):
                            c.ts(dst, pD, comb[:, tt, e_:e_ + 1], None, ALU.mult, None, [b_pD, b_comb], [b_acc[tt]])
                        else:
                            c.stt(dst, pD, comb[:, tt, e_:e_ + 1], dst, ALU.mult, ALU.add, [b_pD, b_comb, b_acc[tt]], [b_acc[tt]])
            es3.__exit__(None, None, None)
            c.barrier()
            c.es = es
            gm = c.sb("gm", [128, D], F32)
            b_gm = Buf()
            c.dma("sp", gm[:], modD[80:96, :].rearrange("a b -> (a b)").partition_broadcast(128), R=[b_modD], W=[b_gm])
            nf = c.sb("nf", [128, D], F32)
            b_nf = Buf()
            c.dma("sp", nf[:], nrm_f.partition_broadcast(128), W=[b_nf])
            xts = [c.sb("xt3", [128, D], F32) for _ in range(2)]
            b_xts = [Buf(), Buf()]
            sq3 = c.sb("sq3", [128, D], BF16)
            ss3 = c.sb("ss3", [128, 1], F32)
            b_sq3, b_ss3 = Buf(), Buf()
            b_out = []
            for tt in range(8):
                xt, b_xt = xts[tt % 2], b_xts[tt % 2]
                c.dma("sp", xt[:], x1_d[tt * 128:(tt + 1) * 128, :], R=[b_x1d[tt]], W=[b_xt])
                z = acc[:, tt, :]
                c.tt(z, z, gm[:], ALU.mult, [b_acc[tt], b_gm], [b_acc[tt]])
                c.tt(z, z, xt[:], ALU.add, [b_acc[tt], b_xt], [b_acc[tt]])
                c.act(sq3[:], z, AF.Square, [b_acc[tt]], [b_sq3, b_ss3], accum_out=ss3[:])
                c.ts(ss3[:], ss3[:], 1.0 / D, EPS, ALU.mult, ALU.add, [b_ss3], [b_ss3])
                c.act(ss3[:], ss3[:], AF.Sqrt, [b_ss3], [b_ss3])
                c.op("dve", lambda e: e.reciprocal(out=ss3[:], in_=ss3[:]), [b_ss3], [b_ss3])
                c.stt(xt[:], z, ss3[:, 0:1], nf[:], ALU.mult, ALU.mult, [b_acc[tt], b_ss3, b_nf, b_xt], [b_xt])
                bo = Buf()
                b_out.append(bo)
                c.dma("sp", y[tt * 128:(tt + 1) * 128, :], xt[:], R=[b_xt], W=[bo])
            c.finish(b_out + dbg_bufs)
        c.barrier()
        c.es = es0
        return nc


_NC = None


def _consts():
    bf = ml_dtypes.bfloat16
    p = np.arange(128)
    k = {}
    k["k_identf"] = np.eye(128, dtype=np.float32)
    k["k_identb"] = np.eye(128).astype(bf)
    k["k_U"] = (p[:, None] <= p[None, :]).astype(np.float32)
    k["k_SL"] = (p[:, None] > p[None, :]).astype(np.float32)
    k["k_ILT"] = (p[None, :] >= p[:, None]).astype(np.float32)
    k["k_ones"] = np.ones((128, 128), np.float32)
    triU = np.where(p[:, None] <= p[None, :], 0.0, NEG).astype(np.float32)
    triL = np.where(p[:, None] > p[None, :], 0.0, NEG).astype(np.float32)
    k["k_triU"] = np.concatenate([triU, triU], axis=1).astype(bf)
    k["k_triL"] = np.concatenate([triL, triL], axis=1).astype(bf)
    E = np.zeros((64, 32, 128), np.float32)
    for kc in range(32):
        for pp in range(128):
            E[2 * kc + pp // 64, kc, pp] = 1.0
    k["k_E"] = E.astype(bf)
    ov = np.zeros((256, 65), np.float32)
    for m in range(1, 256):
        n = m - 1
        cs = n * 16
        for j in range(64):
            if cs < j * 64 + 64 and cs + 32 > j * 64:
                ov[m, j] = 1.0
        ov[m, 64] = 1.0
    k["k_ovl"] = np.ascontiguousarray(ov.reshape(2, 128, 65).transpose(1, 0, 2))
    cb0 = np.zeros((128, 17, 128), np.float32)
    col = np.arange(128)
    for d in range(16):
        valid = (16 * p[:, None] + 15) <= (128 * d + col[None, :])
        cb0[:, d, :] = np.where(valid, 0.0, NEG)
    k["k_cb0"] = cb0.astype(bf)
    fb = np.zeros((128, 32, 64), np.float32)
    for qb in range(32):
        for t in range(128):
            cur = 2 * qb + t // 64
            for j in range(64):
                if j == 0 or j == cur or j == cur - 1:
                    fb[t, qb, j] = 1.0e4
                elif j > cur:
                    fb[t, qb, j] = -1.0e4
    k["k_fb"] = fb.astype(bf)
    return k


def _prep(inp):
    f = lambda a: np.ascontiguousarray(np.asarray(a, dtype=np.float32))
    x = f(inp["x"]); cc = f(inp["c"])
    w_in = f(inp["w_in"])[0]
    conv = f(inp["gdn_conv_w"])[0]
    wo_full = f(inp["w_out"])[0]
    consts = _consts()
    shared = {
        "w_ada": f(inp["w_ada"])[0], "b_ada": f(inp["b_ada"])[0],
        "nrm_a": np.ascontiguousarray(f(inp["norm_attn_w"])[0].reshape(16, 128).T),
        "nrm_m": np.ascontiguousarray(f(inp["norm_ffn_w"])[0].reshape(16, 128).T),
        "nrm_f": f(inp["norm_final_w"]),
        "gnw": np.ascontiguousarray(np.broadcast_to(f(inp["gdn_norm_w"])[0][None, :], (128, 128))),
        "w1k": f(inp["cmp_w1_k"])[0], "w1v": f(inp["cmp_w1_v"])[0],
        "w2k": f(inp["cmp_w2_k"])[0], "w2v": f(inp["cmp_w2_v"])[0],
        "pek": np.ascontiguousarray(f(inp["cmp_pe_k"])[0].T), "pev": np.ascontiguousarray(f(inp["cmp_pe_v"])[0].T),
        "rw": f(inp["router_w"])[0],
        "rbias": np.ascontiguousarray(np.broadcast_to(f(inp["router_bias"])[0][None, :], (128, 64))),
        "ewg": f(inp["expert_w_gate"])[0], "ewu": f(inp["expert_w_up"])[0], "ewd": f(inp["expert_w_down"])[0],
        "swg": f(inp["shared_w_gate"])[0], "swu": f(inp["shared_w_up"])[0], "swd": f(inp["shared_w_down"])[0],
    }
    shared.update(consts)
    maps = []
    for core in range(8):
        b, hg = core // 4, core % 4
        kvh = hg // 2
        g = [2 * hg, 2 * hg + 1]
        m = dict(shared)
        m["xb"] = x[b]
        m["xq"] = np.ascontiguousarray(x[b, hg * 1024:(hg + 1) * 1024])
        m["cvec"] = np.ascontiguousarray(cc[b].reshape(16, 128).T)
        cols = []
        for part in range(3):
            for h in g:
                cols += list(range(part * 1024 + h * 128, part * 1024 + h * 128 + 128))
        m["wA_fm"] = np.ascontiguousarray(w_in[:, cols])
        m["convw"] = np.ascontiguousarray(conv[:, cols].reshape(4, 6, 128).transpose(2, 1, 0))
        tcols = []
        for h in g:
            tcols += list(range(3088 + h * 128, 3088 + h * 128 + 128))
        tcols += [3072 + g[0], 3072 + g[1], 3080 + g[0], 3080 + g[1]]
        m["wA_tm"] = np.ascontiguousarray(w_in[:, tcols])
        al = f(inp["gdn_a_log"])[0][g]; db = f(inp["gdn_dt_bias"])[0][g]
        m["alog"] = np.ascontiguousarray(np.broadcast_to(al[None, :], (128, 2)))
        m["dtb"] = np.ascontiguousarray(np.broadcast_to(db[None, :], (128, 2)))
        grp = [4 * kvh + i for i in range(4)]
        qh = g + [h for h in grp if h not in g]
        bcols = []
        for h in qh:
            bcols += list(range(4112 + h * 128, 4112 + h * 128 + 128))
        for part in (0, 1, 2, 4):
            bcols += list(range(5136 + part * 256 + kvh * 128, 5136 + part * 256 + kvh * 128 + 128))
        m["wB_fm"] = np.ascontiguousarray(w_in[:, bcols])
        btc = []
        for part in (3, 5):
            btc += list(range(5136 + part * 256 + kvh * 128, 5136 + part * 256 + kvh * 128 + 128))
        for br in range(3):
            for h in g:
                btc.append(6672 + br * 8 + h)
        m["wB_tm"] = np.ascontiguousarray(w_in[:, btc])
        rows = []
        for h in g:
            rows += list(range(h * 128, h * 128 + 128))
        for h in g:
            rows += list(range(1024 + h * 128, 1024 + h * 128 + 128))
        m["wout"] = np.ascontiguousarray(wo_full[rows, :])
        maps.append(m)
    return maps


def kernel(**inputs):
    global _NC
    maps = _prep(inputs)
    if _NC is None:
        _NC = build()
    res = run_bass_kernel_spmd(_NC, maps, core_ids=list(range(8)))
    out = np.zeros((2, T, D), np.float32)
    for core in range(8):
        b, q = core // 4, core % 4
        out[b, q * 1024:(q + 1) * 1024] = res.results[core]["y"]
    return out
```

```python
import numpy as np
from contextlib import ExitStack
import ml_dtypes
import concourse.bass as bass
import concourse.mybir as mybir
from concourse.bass_utils import run_bass_kernel_spmd

F32 = mybir.dt.float32
F32R = mybir.dt.float32r
BF16 = mybir.dt.bfloat16
AF = mybir.ActivationFunctionType
ALU = mybir.AluOpType

D = 2048
T = 4096
NEG = -10000.0
EPS = 1e-6
SCALE = 128 ** -0.5


class Buf:
    __slots__ = ("w", "r", "name", "excl")

    def __init__(self, name="", excl=False):
        self.w = None
        self.r = {}
        self.name = name
        self.excl = excl


class Ctx:
    NS = 6

    def __init__(self, nc, es):
        self.nc = nc
        self.es = es
        self.E = {"pe": nc.tensor, "dve": nc.vector, "act": nc.scalar, "pool": nc.gpsimd, "sp": nc.sync}
        self.sem = {}
        self.cnt = {}
        for k in self.E:
            self.sem[k] = es.enter_context(nc.semaphore("s_" + k))
            self.cnt[k] = 0
        self.dq = {}
        for q in ("sp", "pool"):
            sl = []
            for i in range(self.NS):
                key = "d_%s%d" % (q, i)
                self.sem[key] = es.enter_context(nc.semaphore(key))
                self.cnt[key] = 0
                sl.append(key)
            self.dq[q] = [sl, 0]
        self.sem["d_coll"] = es.enter_context(nc.semaphore("d_coll"))
        self.cnt["d_coll"] = 0
        self.seen = {k: {} for k in self.E}
        self.uid = 0

    def sb(self, name, shape, dt):
        self.uid += 1
        return self.es.enter_context(self.nc.sbuf_tensor("%s_%d" % (name, self.uid), list(shape), dt))

    def ps(self, name, shape, dt=F32):
        self.uid += 1
        return self.es.enter_context(self.nc.psum_tensor("%s_%d" % (name, self.uid), list(shape), dt))

    def _wait(self, eng, tok):
        if tok is None:
            return
        key, val = tok
        if key == eng and eng == "pe":
            return
        if self.seen[eng].get(key, 0) >= val:
            return
        self.seen[eng][key] = val
        self.E[eng].wait_ge(self.sem[key], val)

    def _deps(self, eng, reads, writes):
        for b in reads:
            self._wait(eng, b.w)
            if b.excl:
                for t in list(b.r.items()):
                    if t[0] != eng:
                        self._wait(eng, t)
        for b in writes:
            self._wait(eng, b.w)
            for t in list(b.r.items()):
                self._wait(eng, t)

    def _mark(self, tok, reads, writes):
        for b in reads:
            if b.r.get(tok[0], 0) < tok[1]:
                b.r[tok[0]] = tok[1]
        for b in writes:
            b.w = tok
            b.r = {}

    def op(self, eng, fn, R=(), W=(), inc=True):
        self._deps(eng, R, W)
        ins = fn(self.E[eng])
        if inc:
            self.cnt[eng] += 1
            ins.then_inc(self.sem[eng], 1)
            tok = (eng, self.cnt[eng])
        else:
            tok = (eng, self.cnt[eng] + 1)
        self._mark(tok, R, W)
        return ins

    def dma(self, q, out, in_, R=(), W=(), **kw):
        sl, k = self.dq[q]
        key = sl[k % self.NS]
        self.dq[q][1] = k + 1
        if self.cnt[key]:
            self._wait(q, (key, self.cnt[key]))
        self._deps(q, R, W)
        ins = self.E[q].dma_start(out=out, in_=in_, **kw)
        self.cnt[key] += 16
        ins.then_inc(self.sem[key], 16)
        tok = (key, self.cnt[key])
        self._mark(tok, R, W)
        return tok

    def coll(self, fn, R=(), W=()):
        self._deps("pool", R, W)
        ins = fn(self.E["pool"])
        self.cnt["d_coll"] += 16
        ins.then_inc(self.sem["d_coll"], 16)
        tok = ("d_coll", self.cnt["d_coll"])
        self._mark(tok, R, W)
        return ins

    def barrier(self):
        for e in self.E:
            for k, v in self.cnt.items():
                if v and k != e:
                    self._wait(e, (k, v))

    def drain(self):
        for k, v in self.cnt.items():
            if v:
                self._wait("sp", (k, v))

    def finish(self, bufs):
        for b in bufs:
            self._wait("sp", b.w)

    def mm(self, out, lhsT, rhs, R, W, start=True, stop=True, inc=True):
        return self.op("pe", lambda e: e.matmul(out, lhsT, rhs, start=start, stop=stop), R, W, inc=inc)

    def tr(self, out, in_, ident, R, W):
        return self.op("pe", lambda e: e.transpose(out, in_, ident), R, W)

    def act(self, out, in_, func, R, W, **kw):
        return self.op("act", lambda e: e.activation(out=out, in_=in_, func=func, **kw), R, W)

    def tt(self, out, in0, in1, op, R, W, eng="dve"):
        return self.op(eng, lambda e: e.tensor_tensor(out=out, in0=in0, in1=in1, op=op), R, W)

    def ts(self, out, in0, s1, s2, op0, op1, R, W, eng="dve"):
        if op1 is None:
            return self.op(eng, lambda e: e.tensor_scalar(out=out, in0=in0, scalar1=s1, scalar2=None, op0=op0), R, W)
        return self.op(eng, lambda e: e.tensor_scalar(out=out, in0=in0, scalar1=s1, scalar2=s2, op0=op0, op1=op1), R, W)

    def stt(self, out, in0, scalar, in1, op0, op1, R, W):
        return self.op("dve", lambda e: e.scalar_tensor_tensor(out=out, in0=in0, scalar=scalar, in1=in1, op0=op0, op1=op1), R, W)

    def cp(self, out, in_, R, W, eng="dve"):
        if eng == "act":
            return self.op("act", lambda e: e.copy(out=out, in_=in_), R, W)
        return self.op(eng, lambda e: e.tensor_copy(out=out, in_=in_), R, W)


class PSlots:
    def __init__(self, c, name, nbanks, per, width=None):
        self.slots = []
        self.full = []
        w = width or 512 // per
        for b in range(nbanks):
            t = c.ps(name, [128, 512])
            bb = Buf(excl=True)
            self.full.append((t[:, :], bb))
            for s in range(per):
                self.slots.append((t[:, s * w:(s + 1) * w], bb))
        self.i = 0

    def get(self):
        s = self.slots[self.i % len(self.slots)]
        self.i += 1
        return s


def r32(ap):
    return ap


def build(debug=False, n_exp=64, stop=None):
    nc = bass.Bass("TRN2", target_bir_lowering=False)

    def din(name, shape, dt=F32):
        return nc.dram_tensor(name, list(shape), dt, kind="ExternalInput").ap()

    xb = din("xb", [T, D])
    xq = din("xq", [1024, D])
    cvec = din("cvec", [128, 16])
    w_ada = din("w_ada", [D, 6 * D])
    b_ada = din("b_ada", [6 * D])
    nrm_a = din("nrm_a", [128, 16])
    nrm_m = din("nrm_m", [128, 16])
    nrm_f = din("nrm_f", [D])
    wA_fm = din("wA_fm", [D, 768])
    wA_tm = din("wA_tm", [D, 260])
    convw = din("convw", [128, 6, 4])
    alog = din("alog", [128, 2])
    dtb = din("dtb", [128, 2])
    gnw = din("gnw", [128, 128])
    wB_fm = din("wB_fm", [D, 1024])
    wB_tm = din("wB_tm", [D, 262])
    w1k = din("w1k", [4096, 256])
    w1v = din("w1v", [4096, 256])
    w2k = din("w2k", [256, 128])
    w2v = din("w2v", [256, 128])
    pek = din("pek", [128, 32])
    pev = din("pev", [128, 32])
    wout = din("wout", [512, D])
    rw = din("rw", [D, 64])
    rbias = din("rbias", [128, 64])
    if n_exp:
        ewg = din("ewg", [64, D, 512])
        ewu = din("ewu", [64, D, 512])
        ewd = din("ewd", [64, 512, D])
    swg = din("swg", [D, 512])
    swu = din("swu", [D, 512])
    swd = din("swd", [512, D])
    k_identf = din("k_identf", [128, 128])
    k_identb = din("k_identb", [128, 128], BF16)
    k_U = din("k_U", [128, 128])
    k_SL = din("k_SL", [128, 128])
    k_ILT = din("k_ILT", [128, 128])
    k_ones = din("k_ones", [128, 128])
    k_triU = din("k_triU", [128, 256], BF16)
    k_triL = din("k_triL", [128, 256], BF16)
    k_E = din("k_E", [64, 32, 128], BF16)
    k_ovl = din("k_ovl", [128, 2, 65])
    k_cb0 = din("k_cb0", [128, 17, 128], BF16)
    k_fb = din("k_fb", [128, 32, 64], BF16)

    y = nc.dram_tensor("y", [1024, D], F32, kind="ExternalOutput").ap()

    dk = dict(kind="ExternalOutput") if debug else {}
    hT_d = nc.dram_tensor("hT_d", [128, 16, T], BF16, **dk).ap()
    modD = nc.dram_tensor("modD", [96, 128], F32, **dk).ap()
    partial = nc.dram_tensor("partial", [T, D], F32)
    rs_out = nc.dram_tensor("rs_out", [1024, D], F32)
    fence_in = nc.dram_tensor("fence_in", [128, 128], F32)
    fence_out = nc.dram_tensor("fence_out", [128, 128], F32)
    x1_d = nc.dram_tensor("x1_d", [1024, D], F32, **dk).ap()
    if debug:
        yTg_d = nc.dram_tensor("yTg_d", [128, 2, T], BF16, kind="ExternalOutput").ap()
        yTn_d = nc.dram_tensor("yTn_d", [128, 2, T], BF16, kind="ExternalOutput").ap()
        part_d = nc.dram_tensor("part_d", [T, D], F32, kind="ExternalOutput").ap()

    with ExitStack() as es0:
        c = Ctx(nc, es0)

        def load_const(name, src, shape, dt, q="sp"):
            t = c.sb(name, shape, dt)
            b = Buf(name)
            c.dma(q, t[:], src, W=[b])
            return t, b

        identf, b_identf = load_const("identf", k_identf, [128, 128], F32)
        identb, b_identb = load_const("identb", k_identb, [128, 128], BF16)
        nrmA, b_nrmA = load_const("nrmA", nrm_a, [128, 16], F32)
        nrmM, b_nrmM = load_const("nrmM", nrm_m, [128, 16], F32)
        modT = c.sb("modT", [128, 96], F32)
        b_modT = Buf()
        A1 = c.sb("A1", [128, 16], F32)
        A2 = c.sb("A2", [128, 16], F32)
        b_A = Buf()

        b_modD = Buf()

        def ada_part(j0, j1, last):
            with ExitStack() as es:
                c.es = es
                cv = c.sb("cv", [128, 16], F32)
                b_cv = Buf()
                c.dma("sp", cv[:], cvec, W=[b_cv])
                cond = c.sb("cond", [128, 16, 2], F32)
                b_cond = Buf()
                for j in range(2):
                    c.act(cond[:, :, j], cv[:], AF.Silu, [b_cv], [b_cond])
                wt = [c.sb("wada", [128, 16, 512], F32) for _ in range(2)]
                bw = [Buf(), Buf()]
                prow = [c.ps("prow", [128, 512]) for _ in range(2)]
                b_prow = [Buf(excl=True), Buf(excl=True)]
                pmT = c.ps("pmT", [128, 512])
                b_pmT = Buf(excl=True)
                rowsb = [c.sb("rowsb", [2, 512], F32) for _ in range(2)]
                b_rowsb = [Buf(), Buf()]
                bad = c.sb("bad", [128, 96], F32)
                b_bad = Buf()
                c.dma("sp", bad[:], b_ada.rearrange("(j p) -> p j", p=128), W=[b_bad], allow_slow_non_contiguous=True)
                for blk in range(j0 // 4, j1 // 4):
                    k2 = blk % 2
                    c.dma("sp", wt[k2][:], w_ada[:, blk * 512:(blk + 1) * 512].rearrange("(kc p) n -> p kc n", p=128), W=[bw[k2]])
                    for kc in range(16):
                        c.mm(prow[k2][0:2, :], cond[:, kc, :], wt[k2][:, kc, :], [bw[k2], b_cond], [b_prow[k2]],
                             start=(kc == 0), stop=(kc == 15), inc=(kc == 15))
                    c.cp(rowsb[k2][:], prow[k2][0:2, :], [b_prow[k2]], [b_rowsb[k2]], eng="act")
                    for nn in range(4):
                        jj = blk * 4 + nn - j0
                        c.tr(pmT[:, 2 * jj:2 * jj + 2], rowsb[k2][0:2, nn * 128:(nn + 1) * 128], identf[0:2, 0:2],
                             [b_rowsb[k2], b_identf], [b_pmT])
                nj = j1 - j0
                c.tt(modT[:, j0:j1], pmT[:, 0:2 * nj].rearrange("p (j two) -> p j two", two=2)[:, :, 0], bad[:, j0:j1], ALU.add,
                     [b_pmT, b_bad], [b_modT])
                if not last:
                    c.stt(A1[:], modT[:, 16:32], 1.0, nrmA[:], ALU.add, ALU.mult, [b_modT, b_nrmA], [b_A])
                else:
                    c.stt(A2[:], modT[:, 64:80], 1.0, nrmM[:], ALU.add, ALU.mult, [b_modT, b_nrmM], [b_A])
                    ptm = c.ps("ptm", [128, 512])
                    b_ptm = Buf(excl=True)
                    c.tr(ptm[0:96, 0:128], modT[:], identf[:], [b_modT, b_identf], [b_ptm])
                    mrow = c.sb("mrow", [96, 128], F32)
                    b_mrow = Buf()
                    c.cp(mrow[:], ptm[0:96, 0:128], [b_ptm], [b_mrow])
                    c.dma("sp", modD, mrow[:], R=[b_mrow], W=[b_modD])
            c.barrier()
            c.es = es0

        ada_part(0, 32, False)
        if stop == "p0all":
            ada_part(32, 96, True)
            c.drain()
            return nc
        if stop == "p0":
            c.drain()
            return nc
        B1 = modT[:, 0:16]
        B2 = modT[:, 48:64]

        def norm_tile(xt, b_xt, Acol, Bcol, hT, b_hT, col0, tmp, ptr):
            sq, ssq, rstd, xn = tmp["sq"], tmp["ss"], tmp["rstd"], tmp["xn"]
            b = tmp["b"]
            c.act(sq[:], xt[:], AF.Square, [b_xt], [b["sq"], b["ss"]], accum_out=ssq[:])
            c.ts(rstd[:], ssq[:], 1.0 / D, EPS, ALU.mult, ALU.add, [b["ss"]], [b["rstd"]])
            c.act(rstd[:], rstd[:], AF.Sqrt, [b["rstd"]], [b["rstd"]])
            c.op("dve", lambda e: e.reciprocal(out=rstd[:], in_=rstd[:]), [b["rstd"]], [b["rstd"]])
            c.ts(xn[:], xt[:], rstd[:, 0:1], None, ALU.mult, None, [b_xt, b["rstd"]], [b["xn"]])
            for k4 in range(4):
                pt, b_pt = ptr.get()
                ptb = pt.bitcast(BF16)
                for i in range(4):
                    kc = k4 * 4 + i
                    c.tr(ptb[:, i * 128:(i + 1) * 128], xn[:, kc * 128:(kc + 1) * 128], identb[:], [b["xn"], b_identb], [b_pt])
                for i in range(4):
                    kc = k4 * 4 + i
                    if k4 % 2 == 0:
                        c.act(hT[:, kc, col0:col0 + 128], ptb[:, i * 128:(i + 1) * 128], AF.Identity, [b_pt, b_A, b_modT], [b_hT],
                              scale=Acol[:, kc:kc + 1], bias=Bcol[:, kc:kc + 1])
                    else:
                        c.ts(hT[:, kc, col0:col0 + 128], ptb[:, i * 128:(i + 1) * 128], Acol[:, kc:kc + 1], Bcol[:, kc:kc + 1],
                             ALU.mult, ALU.add, [b_pt, b_A, b_modT], [b_hT])

        def norm_tmp():
            return {"sq": c.sb("sq", [128, D], BF16), "ss": c.sb("ss", [128, 1], F32), "rstd": c.sb("rstd", [128, 1], F32),
                    "xn": c.sb("xn", [128, D], BF16), "b": {k: Buf() for k in ("sq", "ss", "rstd", "xn")}}

        yTg = c.sb("yTg", [128, 2, T], BF16)
        b_yTg = [Buf() for _ in range(8)]
        b_hTd = [Buf() for _ in range(8)]
        b_parts = []
        dbg_bufs = []

        with ExitStack() as es:
            c.es = es
            wfm = c.sb("wAfm", [128, 16, 768], BF16)
            wtm = c.sb("wAtm", [128, 16, 260], BF16)
            b_wfm, b_wtm = Buf(), Buf()
            c.dma("pool", wfm[:], wA_fm.rearrange("(kc p) n -> p kc n", p=128), W=[b_wfm])
            c.dma("pool", wtm[:], wA_tm.rearrange("(kc p) n -> p kc n", p=128), W=[b_wtm])
            cw, b_cw = load_const("cw", convw, [128, 6, 4], F32)
            Uc, b_U = load_const("Uc", k_U, [128, 128], F32)
            SLc, b_SL = load_const("SLc", k_SL, [128, 128], F32)
            ILTc, b_ILT = load_const("ILTc", k_ILT, [128, 128], F32)
            onesc, b_ones = load_const("onesc", k_ones, [128, 128], F32)
            al, b_al = load_const("al", alog, [128, 2], F32)
            dtbc, b_dtb = load_const("dtbc", dtb, [128, 2], F32)
            gnwr = c.sb("gnwr", [128, 128], F32)
            b_gnw = Buf()
            c.dma("sp", gnwr[:], gnw, W=[b_gnw])
            nega = c.sb("nega", [128, 2], F32)
            b_nega = Buf()
            c.act(nega[:], al[:], AF.Exp, [b_al], [b_nega])
            c.ts(nega[:], nega[:], -1.0, None, ALU.mult, None, [b_nega], [b_nega])

            hT = c.sb("hT", [128, 16, 512], BF16)
            b_hT = Buf()
            xts = [c.sb("xt", [128, D], F32) for _ in range(2)]
            b_xts = [Buf(), Buf()]
            ntmp = norm_tmp()
            ptr = PSlots(c, "ptr", 1, 1)
            pproj = PSlots(c, "pproj", 2, 1)
            pg = PSlots(c, "pg", 5, 1, 128)
            for ap_, bb_ in ptr.slots + pproj.slots:
                pg.slots.append((ap_[:, 0:128], bb_))
            ptr.slots += pg.full[0:2]

            raw = c.sb("raw", [128, 6, 515], F32)
            b_raw = [Buf() for _ in range(6)]
            for u in range(6):
                c.op("pool", lambda e: e.memset(raw[:, u, 0:3], 0.0), [], [b_raw[u]])
            qkv = c.sb("qkvs", [128, 6, 512], F32)
            b_qkv = [Buf() for _ in range(6)]
            sqt = c.sb("sqt", [128, 512], F32)
            b_sqt = Buf()
            rn = c.sb("rn", [128, 512], F32)
            b_rn = Buf()
            gsil = c.sb("gsil", [128, 4, 256], BF16)
            b_gsil = [Buf() for _ in range(4)]
            gb = c.sb("gb", [128, 4, 4], F32)
            b_gb = [Buf() for _ in range(4)]
            sp1 = c.sb("sp1", [128, 2], F32)
            b_sp1 = Buf()
            S = [c.sb("S", [128, 128], F32) for _ in range(2)]
            b_S = [Buf(), Buf()]
            for h in range(2):
                c.op("pool", lambda e: e.memset(S[h][:], 0.0), [], [b_S[h]])

            def unit_tmp():
                t = {}
                for n in ("Gs", "Dm", "DTm", "A", "AT", "Pa", "PaT", "Pb", "PbT", "FT", "QK", "WT", "kdec", "o1s", "o", "vnew"):
                    t[n] = c.sb("u_" + n, [128, 128], F32)
                for n in ("Xa", "Xb"):
                    t[n] = c.sb("u_" + n, [128, 256], F32)
                for n in ("bcol", "eb", "ebl", "edec", "bek", "ss2", "rs2"):
                    t[n] = c.sb("u_" + n, [128, 1], F32)
                t["y"] = c.sb("u_y", [128, 128], BF16)
                t["b"] = {}
                return t
            units = [[unit_tmp() for _ in range(2)] for _ in range(2)]

            def B(u, n):
                if n not in u["b"]:
                    u["b"][n] = Buf(n)
                return u["b"][n]

            for blk in range(8):
                for tt in range(4):
                    ti = blk * 4 + tt
                    xt, b_xt = xts[ti % 2], b_xts[ti % 2]
                    c.dma("sp", xt[:], xb[ti * 128:(ti + 1) * 128, :], W=[b_xt])
                    norm_tile(xt, b_xt, A1, B1, hT, b_hT, tt * 128, ntmp, ptr)
                c.dma("sp", hT_d[:, :, blk * 512:(blk + 1) * 512], hT[:], R=[b_hT], W=[b_hTd[blk]])
                if stop == "A1" and blk == 0:
                    c.drain()
                    return nc
                for u in range(6):
                    pp, b_pp = pproj.get()
                    for kc in range(16):
                        c.mm(pp, wfm[:, kc, u * 128:(u + 1) * 128], hT[:, kc, :], [b_wfm, b_hT], [b_pp],
                             start=(kc == 0), stop=(kc == 15), inc=(kc == 15))
                    c.cp(raw[:, u, 3:515], pp, [b_pp], [b_raw[u]], eng="act")
                    c.ts(qkv[:, u, :], raw[:, u, 0:512], cw[:, u, 0:1], None, ALU.mult, None, [b_raw[u], b_cw], [b_qkv[u]])
                    for i in range(1, 4):
                        c.stt(qkv[:, u, :], raw[:, u, i:i + 512], cw[:, u, i:i + 1], qkv[:, u, :], ALU.mult, ALU.add,
                              [b_raw[u], b_cw, b_qkv[u]], [b_qkv[u]])
                    c.cp(raw[:, u, 0:3], raw[:, u, 512:515], [b_raw[u]], [b_raw[u]], eng="pool")
                    c.act(qkv[:, u, :], qkv[:, u, :], AF.Silu, [b_qkv[u]], [b_qkv[u]])
                if stop == "A2" and blk == 0:
                    c.drain()
                    return nc
                for u in range(4):
                    c.tt(sqt[:], qkv[:, u, :], qkv[:, u, :], ALU.mult, [b_qkv[u]], [b_sqt])
                    pp, b_pp = pproj.get()
                    c.mm(pp, r32(onesc[:]), r32(sqt[:]), [b_ones, b_sqt], [b_pp])
                    c.ts(rn[:], pp, EPS, None, ALU.add, None, [b_pp], [b_rn])
                    c.act(rn[:], rn[:], AF.Sqrt, [b_rn], [b_rn])
                    c.op("dve", lambda e: e.reciprocal(out=rn[:], in_=rn[:]), [b_rn], [b_rn])
                    c.stt(qkv[:, u, :], qkv[:, u, :], (SCALE if u < 2 else 1.0), rn[:], ALU.mult, ALU.mult,
                          [b_qkv[u], b_rn], [b_qkv[u]])
                for tt in range(4):
                    pp, b_pp = pproj.get()
                    for kc in range(16):
                        c.mm(pp[:, 0:260], hT[:, kc, tt * 128:(tt + 1) * 128], wtm[:, kc, :], [b_wtm, b_hT], [b_pp],
                             start=(kc == 0), stop=(kc == 15), inc=(kc == 15))
                    c.act(gsil[:, tt, :], pp[:, 0:256], AF.Silu, [b_pp], [b_gsil[tt]])
                    c.act(gb[:, tt, 0:2], pp[:, 256:258], AF.Sigmoid, [b_pp], [b_gb[tt]])
                    for h in range(2):
                        c.act(sp1[:, h:h + 1], pp[:, 258 + h:259 + h], AF.Exp, [b_pp, b_dtb], [b_sp1], bias=dtbc[:, h:h + 1], scale=1.0)
                    c.act(sp1[:], sp1[:], AF.Ln, [b_sp1], [b_sp1], bias=1.0, scale=1.0)
                    c.tt(gb[:, tt, 2:4], sp1[:], nega[:], ALU.mult, [b_sp1, b_nega], [b_gb[tt]])

                if stop == "A2b" and blk == 0:
                    c.drain()
                    return nc
                def tile_ctx(tt):
                    ti = blk * 4 + tt
                    return ti, slice(tt * 128, (tt + 1) * 128), [units[ti % 2][h] for h in range(2)], b_gb[tt]

                def g_pre(tt, h):
                    ti, tsl, us, bg = tile_ctx(tt)
                    u = us[h]
                    beta = gb[:, tt, h:h + 1]
                    g = gb[:, tt, 2 + h:3 + h]
                    yield
                    p1, bp1 = pg.get()
                    c.mm(p1[:, 0:2], Uc[:], gb[:, tt, 2:4], [b_U, bg], [bp1])
                    c.cp(u["bcol"][:], p1[:, h:h + 1], [bp1], [B(u, "bcol")])
                    yield
                    p2, bp2 = pg.get()
                    c.mm(p2[:, 0:2], onesc[:], gb[:, tt, 2:4], [b_ones, bg], [bp2])
                    c.act(u["ebl"][:], p2[:, h:h + 1], AF.Exp, [bp2], [B(u, "ebl")])
                    c.cp(u["ss2"][:], p2[:, h:h + 1], [bp2], [B(u, "ss2")])
                    c.act(u["edec"][:], u["bcol"][:], AF.Exp, [B(u, "bcol"), B(u, "ss2")], [B(u, "edec")], scale=-1.0, bias=u["ss2"][:, 0:1])
                    c.act(u["eb"][:], u["bcol"][:], AF.Exp, [B(u, "bcol")], [B(u, "eb")])
                    c.tt(u["bek"][:], u["eb"][:], beta, ALU.mult, [B(u, "eb"), bg], [B(u, "bek")])
                    c.ts(u["Gs"][:], SLc[:], g, None, ALU.mult, None, [b_SL, bg], [B(u, "Gs")])
                    yield
                    p3, bp3 = pg.get()
                    c.mm(p3, Uc[:], u["Gs"][:], [b_U, B(u, "Gs")], [bp3])
                    c.act(u["Dm"][:], p3, AF.Exp, [bp3], [B(u, "Dm")])
                    c.stt(u["Dm"][:], u["Dm"][:], beta, SLc[:], ALU.mult, ALU.mult, [B(u, "Dm"), bg, b_SL], [B(u, "Dm")])
                    yield
                    p4, bp4 = pg.get()
                    c.mm(p4, u["Gs"][:], Uc[:], [b_U, B(u, "Gs")], [bp4])
                    c.act(u["DTm"][:], p4, AF.Exp, [bp4], [B(u, "DTm")])
                    c.tt(u["DTm"][:], u["DTm"][:], ILTc[:], ALU.mult, [B(u, "DTm"), b_ILT], [B(u, "DTm")])
                    u = us[h]
                    beta = gb[:, tt, h:h + 1]
                    KT = qkv[:, 2 + h, tsl]
                    QT = qkv[:, h, tsl]
                    VT = qkv[:, 4 + h, tsl]
                    yield
                    p1, bp1 = pg.get()
                    c.mm(p1, r32(KT), r32(KT), [b_qkv[2 + h]], [bp1])
                    c.tt(u["A"][:], p1, u["Dm"][:], ALU.mult, [bp1, B(u, "Dm")], [B(u, "A")])
                    yield
                    p2, bp2 = pg.get()
                    c.mm(p2, r32(KT), r32(QT), [b_qkv[2 + h], b_qkv[h]], [bp2])
                    c.tt(u["QK"][:], p2, u["DTm"][:], ALU.mult, [bp2, B(u, "DTm")], [B(u, "QK")])
                    yield
                    p3, bp3 = pg.get()
                    c.tr(p3, u["A"][:], identf[:], [B(u, "A"), b_identf], [bp3])
                    c.cp(u["AT"][:], p3, [bp3], [B(u, "AT")], eng="act")
                    yield
                    p4, bp4 = pg.get()
                    c.tr(p4, VT, identf[:], [b_qkv[4 + h], b_identf], [bp4])
                    c.ts(u["Xa"][:, 0:128], p4, beta, None, ALU.mult, None, [bp4, bg], [B(u, "Xa")])
                    yield
                    p5, bp5 = pg.get()
                    c.tr(p5, KT, identf[:], [b_qkv[2 + h], b_identf], [bp5])
                    c.ts(u["Xa"][:, 128:256], p5, u["bek"][:, 0:1], None, ALU.mult, None, [bp5, B(u, "bek")], [B(u, "Xa")])
                    c.act(u["kdec"][:], p5, AF.Identity, [bp5, B(u, "edec")], [B(u, "kdec")], scale=u["edec"][:, 0:1])
                    u = us[h]
                    X = [(u["Xa"], B(u, "Xa")), (u["Xb"], B(u, "Xb"))]
                    c.stt(u["FT"][:], u["AT"][:], -1.0, identf[:], ALU.mult, ALU.add, [B(u, "AT"), b_identf], [B(u, "FT")])
                    xi = 0
                    Pc, PcT, bPc, bPcT = u["A"], u["AT"], B(u, "A"), B(u, "AT")
                    pp_ = [(u["Pa"], u["PaT"], "Pa", "PaT"), (u["Pb"], u["PbT"], "Pb", "PbT")]
                    for lvl in range(7):
                        Xc, bXc = X[xi]
                        Xn, bXn = X[1 - xi]
                        yield
                        pa, bpa = pg.get()
                        pa2, bpa2 = pg.get()
                        c.mm(pa, r32(u["FT"][:]), r32(Xc[:, 0:128]), [B(u, "FT"), bXc], [bpa])
                        c.mm(pa2, r32(u["FT"][:]), r32(Xc[:, 128:256]), [B(u, "FT"), bXc], [bpa2])
                        c.cp(Xn[:, 0:128], pa, [bpa], [bXn], eng="act")
                        c.cp(Xn[:, 128:256], pa2, [bpa2], [bXn])
                        xi = 1 - xi
                        if lvl == 6:
                            break
                        Pn, PnT, nPn, nPnT = pp_[lvl % 2]
                        yield
                        pq, bpq = pg.get()
                        c.mm(pq, r32(Pc[:]), r32(PcT[:]), [bPc, bPcT], [bpq])
                        c.cp(PnT[:], pq, [bpq], [B(u, nPnT)], eng="act")
                        c.tt(u["FT"][:], pq, identf[:], ALU.add, [bpq, b_identf], [B(u, "FT")])
                        if lvl < 5:
                            yield
                            pq2, bpq2 = pg.get()
                            c.mm(pq2, r32(PcT[:]), r32(Pc[:]), [bPc, bPcT], [bpq2])
                            c.cp(Pn[:], pq2, [bpq2], [B(u, nPn)])
                        Pc, PcT, bPc, bPcT = Pn, PnT, B(u, nPn), B(u, nPnT)
                    u["Xf"], u["bXf"] = X[xi]
                    yield
                    pw, bpw = pg.get()
                    c.tr(pw, u["Xf"][:, 128:256], identf[:], [u["bXf"], b_identf], [bpw])
                    c.cp(u["WT"][:], pw, [bpw], [B(u, "WT")], eng="act")

                def g_seq(tt, h):
                    ti, tsl, us, bg = tile_ctx(tt)
                    u = us[h]
                    QT = qkv[:, h, tsl]
                    Xf, bXf = u["Xf"], u["bXf"]
                    yield
                    p1, bp1 = pg.get()
                    c.mm(p1, r32(u["WT"][:]), r32(S[h][:]), [B(u, "WT"), b_S[h]], [bp1])
                    c.tt(u["vnew"][:], Xf[:, 0:128], p1, ALU.subtract, [bXf, bp1], [B(u, "vnew")])
                    yield
                    p2, bp2 = pg.get()
                    c.mm(p2, r32(QT), r32(S[h][:]), [b_qkv[h], b_S[h]], [bp2])
                    c.act(u["o1s"][:], p2, AF.Identity, [bp2, B(u, "eb")], [B(u, "o1s")], scale=u["eb"][:, 0:1])
                    yield
                    p3, bp3 = pg.get()
                    c.mm(p3, r32(u["QK"][:]), r32(u["vnew"][:]), [B(u, "QK"), B(u, "vnew")], [bp3])
                    c.tt(u["o"][:], u["o1s"][:], p3, ALU.add, [B(u, "o1s"), bp3], [B(u, "o")])
                    yield
                    p4, bp4 = pg.get()
                    c.mm(p4, r32(u["kdec"][:]), r32(u["vnew"][:]), [B(u, "kdec"), B(u, "vnew")], [bp4])
                    c.stt(S[h][:], S[h][:], u["ebl"][:, 0:1], p4, ALU.mult, ALU.add, [b_S[h], B(u, "ebl"), bp4], [b_S[h]])
                    c.act(u["o1s"][:], u["o"][:], AF.Square, [B(u, "o")], [B(u, "o1s"), B(u, "ss2")], accum_out=u["ss2"][:])
                    c.ts(u["rs2"][:], u["ss2"][:], 1.0 / 128, EPS, ALU.mult, ALU.add, [B(u, "ss2")], [B(u, "rs2")])
                    c.act(u["rs2"][:], u["rs2"][:], AF.Sqrt, [B(u, "rs2")], [B(u, "rs2")])
                    c.op("dve", lambda e: e.reciprocal(out=u["rs2"][:], in_=u["rs2"][:]), [B(u, "rs2")], [B(u, "rs2")])
                    c.stt(u["o"][:], u["o"][:], u["rs2"][:, 0:1], gnwr[:], ALU.mult, ALU.mult, [B(u, "o"), B(u, "rs2"), b_gnw], [B(u, "o")])
                    c.tt(u["y"][:], u["o"][:], gsil[:, tt, h * 128:(h + 1) * 128], ALU.mult, [B(u, "o"), b_gsil[tt]], [B(u, "y")])
                    yield
                    py, bpy = pg.get()
                    pyb = py.bitcast(BF16)
                    c.tr(pyb[:, 0:128], u["y"][:], identb[:], [B(u, "y"), b_identb], [bpy])
                    c.cp(yTg[:, h, ti * 128:(ti + 1) * 128], pyb[:, 0:128], [bpy], [b_yTg[blk]], eng="act")

                def run_rr(gens):
                    gens = list(gens)
                    while gens:
                        for g_ in list(gens):
                            try:
                                next(g_)
                            except StopIteration:
                                gens.remove(g_)

                for step in range(5):
                    gens = []
                    if step > 0:
                        gens += [g_seq(step - 1, 0), g_seq(step - 1, 1)]
                    if step < 4:
                        gens += [g_pre(step, 0), g_pre(step, 1)]
                    run_rr(gens)
            if debug:
                bd = Buf(); dbg_bufs.append(bd)
                c.dma("sp", yTg_d, yTg[:], R=b_yTg, W=[bd])
        c.barrier()
        c.es = es0
        if stop == "A":
            c.drain()
            return nc

        with ExitStack() as es:
            c.es = es
            wfm = c.sb("wBfm", [128, 16, 1024], BF16)
            wtm = c.sb("wBtm", [128, 16, 262], BF16)
            b_wfm, b_wtm = Buf(), Buf()
            c.dma("pool", wfm[:], wB_fm.rearrange("(kc p) n -> p kc n", p=128), W=[b_wfm])
            c.dma("pool", wtm[:], wB_tm.rearrange("(kc p) n -> p kc n", p=128), W=[b_wtm])
            w1 = [c.sb("w1", [128, 32, 256], BF16) for _ in range(2)]
            b_w1 = [Buf(), Buf()]
            for i, src in enumerate((w1k, w1v)):
                for half in range(2):
                    c.dma("pool", w1[i][:, half * 16:(half + 1) * 16, :],
                          src[half * 2048:(half + 1) * 2048, :].rearrange("(l p) n -> p l n", p=128), W=[b_w1[i]])
            w2 = [c.sb("w2", [128, 2, 128], BF16) for _ in range(2)]
            b_w2 = [Buf(), Buf()]
            for i, src in enumerate((w2k, w2v)):
                c.dma("pool", w2[i][:], src.rearrange("(a p) n -> p a n", p=128), W=[b_w2[i]])
            pe_ = [c.sb("pe", [128, 32], BF16) for _ in range(2)]
            b_pe = [Buf(), Buf()]
            for i, src in enumerate((pek, pev)):
                c.dma("pool", pe_[i][:], src, W=[b_pe[i]])
            wo = c.sb("wo", [128, 4, D], BF16)
            b_wo = Buf()
            c.dma("pool", wo[:], wout.rearrange("(a p) n -> p a n", p=128), W=[b_wo])
            triU, b_triU = load_const("triU", k_triU, [128, 256], BF16)
            triL, b_triL = load_const("triL", k_triL, [128, 256], BF16)
            Ec, b_E = load_const("Ec", k_E, [64, 32, 128], BF16)
            cb0, b_cb0 = load_const("cb0", k_cb0, [128, 17, 128], BF16)
            fb, b_fb = load_const("fb", k_fb, [128, 32, 64], BF16)
            ovl, b_ovl = load_const("ovl", k_ovl, [128, 2, 65], F32)

            kcmp = [c.sb("kcmp", [128, 16 + T], BF16) for _ in range(2)]
            b_kcmp = [Buf(), Buf()]
            for i in range(2):
                c.op("pool", lambda e: e.memset(kcmp[i][:], 0.0), [], [b_kcmp[i]])
            kslc = c.sb("kslc", [128, T], BF16)
            kwin = c.sb("kwin", [128, T], BF16)
            b_kslc, b_kwin = Buf(), Buf()
            vslc = c.sb("vslc", [128, 32, 130], BF16)
            vwin = c.sb("vwin", [128, 32, 130], BF16)
            b_vslc, b_vwin = Buf(), Buf()
            c.op("pool", lambda e: e.memset(vslc[:, :, 128:130], 1.0), [], [b_vslc])
            c.op("pool", lambda e: e.memset(vwin[:, :, 128:130], 1.0), [], [b_vwin])
            kcT = c.sb("kcT", [128, 256], BF16)
            b_kcT = Buf()
            c.op("pool", lambda e: e.memset(kcT[:], 0.0), [], [b_kcT])
            vcx = c.sb("vcx", [128, 2, 194], BF16)
            b_vcx = Buf()
            c.op("pool", lambda e: e.memset(vcx[:], 0.0), [], [b_vcx])
            c.cp(vcx[:, :, 128:193], ovl[:], [b_ovl, b_vcx], [b_vcx])

            hT = c.sb("hTb", [128, 16, 512], BF16)
            b_hT = Buf()
            qT = c.sb("qT", [128, 4, 512], BF16)
            b_qT = Buf()
            gn = c.sb("gn", [128, 4, 6], F32)
            b_gn = [Buf() for _ in range(4)]
            hs = [c.sb("hs", [128, 2, 32], BF16) for _ in range(2)]
            b_hs = [Buf(), Buf()]
            peb = [c.sb("peb", [128, 2], F32) for _ in range(2)]
            b_peb = [Buf(), Buf()]
            NET = 5
            ET = [c.sb("ET", [128, 256], BF16) for _ in range(NET)]
            b_ET = [Buf() for _ in range(NET)]
            eti = [0]
            imp = c.sb("imp", [128, 64], F32)
            imp2 = c.sb("imp2", [128, 64], F32)
            mx = c.sb("mx", [128, 16], F32)
            b_imp, b_imp2, b_mx = Buf(), Buf(), Buf()
            selb = c.sb("selb", [128, 64], BF16)
            b_selb = Buf()
            selbT = c.sb("selbT", [64, 2, 128], BF16)
            b_selbT = Buf()
            oacc = c.sb("oacc", [128, 2, 128], F32)
            b_oacc = Buf()
            rsm = c.sb("rsm", [128, 8], F32)
            b_rsm = Buf()
            ynb = c.sb("ynb", [128, 2, 128], BF16)
            b_ynb = Buf()
            yTn = c.sb("yTn", [128, 2, 512], BF16)
            b_yTn = Buf()
            pout = [c.sb("pout", [128, 512], F32) for _ in range(2)]
            b_pout = [Buf(), Buf()]

            pproj = PSlots(c, "pprojB", 2, 1)
            pst = PSlots(c, "pst", 2, 1)
            pst_attn = PSlots(c, "pst_attn", 0, 1)
            pst_attn.slots = [pst.slots[0], pproj.slots[0], pst.slots[1], pproj.slots[1]]
            pacc = [c.ps("pacc", [128, 512]) for _ in range(4)]
            b_pacc = [Buf(excl=True) for _ in range(4)]

            for i in range(2):
                for half in range(2):
                    pp, b_pp = pproj.get()
                    for l in range(32):
                        c.mm(pp[:, 0:1], w1[i][:, l, half * 128:(half + 1) * 128], pe_[i][:, l:l + 1],
                             [b_w1[i], b_pe[i]], [b_pp], start=(l == 0), stop=(l == 31), inc=(l == 31))
                    c.cp(peb[i][:, half:half + 1], pp[:, 0:1], [b_pp], [b_peb[i]])

            for blk in range(8):
                if blk == 0:
                    c.dma("sp", hT[:], hT_d[:, :, 0:512], R=[b_hTd[0]], W=[b_hT])
                for u in range(8):
                    pp, b_pp = pproj.get()
                    for kc in range(16):
                        c.mm(pp, wfm[:, kc, u * 128:(u + 1) * 128], hT[:, kc, :], [b_wfm, b_hT], [b_pp],
                             start=(kc == 0), stop=(kc == 15), inc=(kc == 15))
                    eng = "act" if u % 2 else "dve"
                    if u < 4:
                        c.cp(qT[:, u, :], pp, [b_pp], [b_qT], eng=eng)
                    elif u < 6:
                        c.cp(kcmp[u - 4][:, 16 + blk * 512:16 + (blk + 1) * 512], pp, [b_pp], [b_kcmp[u - 4]], eng=eng)
                    elif u == 6:
                        c.cp(kslc[:, blk * 512:(blk + 1) * 512], pp, [b_pp], [b_kslc], eng=eng)
                    else:
                        c.cp(kwin[:, blk * 512:(blk + 1) * 512], pp, [b_pp], [b_kwin], eng=eng)
                for tt in range(4):
                    ti = blk * 4 + tt
                    pp, b_pp = pproj.get()
                    for kc in range(16):
                        c.mm(pp[:, 0:262], hT[:, kc, tt * 128:(tt + 1) * 128], wtm[:, kc, :], [b_wtm, b_hT], [b_pp],
                             start=(kc == 0), stop=(kc == 15), inc=(kc == 15))
                    c.cp(vslc[:, ti, 0:128], pp[:, 0:128], [b_pp], [b_vslc], eng="act")
                    c.cp(vwin[:, ti, 0:128], pp[:, 128:256], [b_pp], [b_vwin])
                    c.act(gn[:, tt, :], pp[:, 256:262], AF.Sigmoid, [b_pp], [b_gn[tt]])
                if blk < 7:
                    c.dma("sp", hT[:], hT_d[:, :, (blk + 1) * 512:(blk + 2) * 512], R=[b_hTd[blk + 1]], W=[b_hT])
                m0 = 32 * blk
                for i in range(2):
                    for half in range(2):
                        pp, b_pp = pproj.get()
                        for l in range(32):
                            rhs = kcmp[i][:, 16 * m0 + l: 16 * m0 + l + 16 * 31 + 1: 16]
                            c.mm(pp[:, 0:32], w1[i][:, l, half * 128:(half + 1) * 128], rhs, [b_w1[i], b_kcmp[i]], [b_pp],
                                 start=(l == 0), stop=(l == 31), inc=(l == 31))
                        c.act(hs[i][:, half, :], pp[:, 0:32], AF.Silu, [b_pp, b_peb[i]], [b_hs[i]], bias=peb[i][:, half:half + 1], scale=1.0)
                pp, b_pp = pproj.get()
                for half in range(2):
                    c.mm(pp[:, 0:32], w2[0][:, half, :], hs[0][:, half, :], [b_w2[0], b_hs[0]], [b_pp], start=(half == 0), stop=(half == 1), inc=(half == 1))
                c.cp(kcT[:, m0:m0 + 32], pp[:, 0:32], [b_pp], [b_kcT])
                pp, b_pp = pproj.get()
                for half in range(2):
                    c.mm(pp[0:32, 0:128], hs[1][:, half, :], w2[1][:, half, :], [b_w2[1], b_hs[1]], [b_pp], start=(half == 0), stop=(half == 1), inc=(half == 1))
                mc_, mp = m0 // 128, m0 % 128
                c.cp(vcx[mp:mp + 32, mc_, 0:128], pp[0:32, 0:128], [b_pp], [b_vcx], eng="act")
                if blk == 0:
                    c.op("pool", lambda e: e.memset(vcx[0:1, 0, :], 0.0), [], [b_vcx])

                for tt in range(4):
                    qb = blk * 4 + tt
                    tsl = slice(tt * 128, (tt + 1) * 128)
                    pairs = []
                    SK = 3

                    def mk_s1(lhs_k, rb_k, ncol, rhs_q, extra, pre=None):
                        def s1(slot):
                            if pre is not None:
                                pre()
                            st, b_st = pst_attn.get()
                            c.mm(st[:, 0:ncol], lhs_k, rhs_q, rb_k + [b_qT], [b_st],
                                 start=True, stop=(len(extra) == 0), inc=(len(extra) == 0))
                            for xi, (o_, l_, r_, rb) in enumerate(extra):
                                last = xi == len(extra) - 1
                                c.mm(o_(st), l_, r_, rb, [b_st], start=False, stop=last, inc=last)
                            e = eti[0] % NET
                            eti[0] += 1
                            slot["e"] = e
                            c.act(ET[e][:, 0:ncol], st[:, 0:ncol], AF.Exp, [b_st], [b_ET[e]], scale=SCALE)
                        return s1

                    mcs = [0] + ([1] if qb >= 16 else [])

                    def fin_cmp():
                        for g in range(4):
                            pa = pacc[g]
                            c.ts(rsm[:, g:g + 1], pa[:, 192:193], 1e-30, None, ALU.max, None, [b_pacc[g]], [b_rsm])
                            c.op("dve", lambda e: e.reciprocal(out=rsm[:, g:g + 1], in_=rsm[:, g:g + 1]), [b_rsm], [b_rsm])
                            c.stt(imp[:], pa[:, 128:192], rsm[:, g:g + 1], (fb[:, qb, :] if g == 0 else imp[:]), ALU.mult, ALU.add,
                                  [b_pacc[g], b_rsm, b_fb, b_imp], [b_imp])
                            if g < 2:
                                c.tt(rsm[:, 4 + g:5 + g], rsm[:, g:g + 1], gn[:, tt, g:g + 1], ALU.mult, [b_rsm, b_gn[tt]], [b_rsm])
                                c.ts(oacc[:, g, :], pa[:, 0:128], rsm[:, 4 + g:5 + g], None, ALU.mult, None, [b_pacc[g], b_rsm], [b_oacc])
                        c.op("dve", lambda e: e.max(out=mx[:, 0:8], in_=imp[:]), [b_imp], [b_mx])
                        c.op("dve", lambda e: e.match_replace(out=imp2[:], in_to_replace=mx[:, 0:8], in_values=imp[:], imm_value=-3.0e4), [b_imp, b_mx], [b_imp2])
                        c.op("dve", lambda e: e.max(out=mx[:, 8:16], in_=imp2[:]), [b_imp2], [b_mx])
                        c.ts(selb[:], imp[:], mx[:, 15:16], NEG, ALU.is_lt, ALU.mult, [b_imp, b_mx], [b_selb])

                    for k, mc in enumerate(mcs):
                        delta = qb - 16 * mc
                        di = min(delta, 16) if mc == 0 else delta
                        for hf in range(2):
                            extra = [((lambda st, gg=gg: st[:, gg * 128:(gg + 1) * 128]), identb[:], cb0[:, di, :], [b_identb, b_cb0]) for gg in range(2)]
                            s1 = mk_s1(kcT[:, mc * 128:(mc + 1) * 128], [b_kcT], 256, qT[:, 2 * hf:2 * hf + 2, tsl], extra)

                            def s2(slot, k=k, mc=mc, hf=hf):
                                e = slot["e"]
                                lastk = k == len(mcs) - 1
                                for gg in range(2):
                                    g = 2 * hf + gg
                                    c.mm(pacc[g][:, 0:193], ET[e][:, gg * 128:(gg + 1) * 128], vcx[:, mc, 0:193], [b_ET[e], b_vcx], [b_pacc[g]],
                                         start=(k == 0), stop=lastk)
                                if lastk and hf == 1:
                                    fin_cmp()
                            pairs.append((s1, s2))
                    i_cmp_last = len(pairs) - 1

                    def b4_pe():
                        st, b_st = pst.get()
                        stb = st.bitcast(BF16)
                        c.tr(stb[0:64, 0:128], selb[:], identb[:], [b_selb, b_identb], [b_st])
                        c.cp(selbT[:, 0, :], stb[0:64, 0:128], [b_st], [b_selbT])
                        c.cp(selbT[:, 1, :], stb[0:64, 0:128], [b_st], [b_selbT])

                    for br in (1, 0):
                        if br == 0:
                            kcs = list(range(0, qb + 1))
                            KT_, b_KT, V_, b_V = kslc, b_kslc, vslc, b_vslc
                            pg0 = 0
                        else:
                            kcs = list(range(max(0, qb - 4), qb + 1))
                            KT_, b_KT, V_, b_V = kwin, b_kwin, vwin, b_vwin
                            pg0 = 2

                        def fin_br(br=br, pg0=pg0):
                            for g in range(2):
                                pa, bpa = pacc[pg0 + g], b_pacc[pg0 + g]
                                c.op("dve", lambda e: e.reciprocal(out=rsm[:, g:g + 1], in_=pa[:, 128:129]), [bpa], [b_rsm])
                                gi = (1 + br) * 2 + g
                                c.tt(rsm[:, 4 + g:5 + g], rsm[:, g:g + 1], gn[:, tt, gi:gi + 1], ALU.mult, [b_rsm, b_gn[tt]], [b_rsm])
                                c.stt(oacc[:, g, :], pa[:, 0:128], rsm[:, 4 + g:5 + g], oacc[:, g, :], ALU.mult, ALU.add,
                                      [bpa, b_rsm, b_oacc], [b_oacc])

                        if br == 0:
                            while len(pairs) < i_cmp_last + SK + 1:
                                pairs.append((lambda slot: None, lambda slot: None))
                        for k, kc in enumerate(kcs):
                            extra = []
                            full = (lambda st: st[:, 0:256])
                            if br == 0:
                                extra.append((full, Ec[:, kc, :], selbT[:].rearrange("p a b -> p (a b)"), [b_E, b_selbT]))
                            if kc == qb:
                                extra.append((full, identb[:], triU[:], [b_identb, b_triU]))
                            if br == 1 and kc == qb - 4:
                                extra.append((full, identb[:], triL[:], [b_identb, b_triL]))
                            s1 = mk_s1(KT_[:, kc * 128:(kc + 1) * 128], [b_KT], 256, qT[:, 0:2, tsl], extra,
                                       pre=(b4_pe if (br == 0 and k == 0) else None))

                            def s2(slot, k=k, kc=kc, n=len(kcs), V_=V_, b_V=b_V, pg0=pg0, fin=fin_br):
                                e = slot["e"]
                                for g in range(2):
                                    c.mm(pacc[pg0 + g][:, 0:129], ET[e][:, g * 128:(g + 1) * 128], V_[:, kc, 0:129], [b_ET[e], b_V], [b_pacc[pg0 + g]],
                                         start=(k == 0), stop=(k == n - 1))
                                if k == n - 1:
                                    fin()
                            pairs.append((s1, s2))

                    slots = [dict() for _ in pairs]
                    for i in range(len(pairs) + SK):
                        if i < len(pairs):
                            pairs[i][0](slots[i])
                        if i >= SK:
                            pairs[i - SK][1](slots[i - SK])
                    c.cp(ynb[:], oacc[:], [b_oacc], [b_ynb], eng="act")
                    for g in range(2):
                        st, b_st = pst.get()
                        stb = st.bitcast(BF16)
                        c.tr(stb[:, 0:128], ynb[:, g, :], identb[:], [b_ynb, b_identb], [b_st])
                        c.cp(yTn[:, g, tsl], stb[:, 0:128], [b_st], [b_yTn])
                if debug:
                    bd = Buf(); dbg_bufs.append(bd)
                    c.dma("sp", yTn_d[:, :, blk * 512:(blk + 1) * 512], yTn[:], R=[b_yTn], W=[bd])
                for tt in range(4):
                    ti = blk * 4 + tt
                    for cbk in range(4):
                        po, b_po = pout[cbk % 2], b_pout[cbk % 2]
                        pp, b_pp = pproj.get()
                        for fc in range(4):
                            lhs = yTg[:, fc, ti * 128:(ti + 1) * 128] if fc < 2 else yTn[:, fc - 2, tt * 128:(tt + 1) * 128]
                            rb = [b_yTg[blk]] if fc < 2 else [b_yTn]
                            c.mm(pp, lhs, wo[:, fc, cbk * 512:(cbk + 1) * 512], rb + [b_wo], [b_pp], start=(fc == 0), stop=(fc == 3), inc=(fc == 3))
                        c.cp(po[:], pp, [b_pp], [b_po], eng=("act" if cbk % 2 else "dve"))
                        b_part = Buf()
                        b_parts.append(b_part)
                        c.dma("sp", partial.ap()[ti * 128:(ti + 1) * 128, cbk * 512:(cbk + 1) * 512], po[:], R=[b_po], W=[b_part])
        c.barrier()
        c.es = es0
        if stop == "B":
            c.drain()
            return nc
        if debug:
            bd = Buf(); dbg_bufs.append(bd)
            c.dma("sp", part_d, partial.ap(), R=b_parts, W=[bd])
        b_rs = Buf()
        b_rs0 = Buf()
        c.op("pool", lambda e: e.collective_compute("ReduceScatter", ALU.add, replica_groups=[[0, 1, 2, 3], [4, 5, 6, 7]],
                                                    ins=[partial.ap().opt()], outs=[rs_out.ap().opt()]), b_parts, [b_rs0])
        c.op("pool", lambda e: e.collective_compute("AllReduce", ALU.add, replica_groups=[[0, 1, 2, 3], [4, 5, 6, 7]],
                                                    ins=[fence_in.ap().opt()], outs=[fence_out.ap().opt()]), [b_rs0], [b_rs])

        ada_part(32, 96, True)
        if stop == "rs":
            c.drain()
            return nc
        with ExitStack() as es:
            c.es = es
            h2T = c.sb("h2T", [128, 16, 1024], BF16)
            b_h2T = Buf()
            acc = c.sb("acc", [128, 8, D], F32)
            b_acc = [Buf() for _ in range(8)]
            comb = c.sb("comb", [128, 8, 65], F32)
            b_comb = Buf()
            ptr = PSlots(c, "ptr2", 1, 1)
            pgu = PSlots(c, "pgu", 4, 1)
            pd = PSlots(c, "pd", 3, 1)
            ptr0 = ptr.slots[0]
            ptr.slots += pd.slots[1:3]
            b_x1d = [Buf() for _ in range(8)]
            with ExitStack() as es2:
                c.es = es2
                ga = c.sb("ga", [128, D], F32)
                b_ga = Buf()
                c.dma("sp", ga[:], modD[32:48, :].rearrange("a b -> (a b)").partition_broadcast(128), R=[b_modD], W=[b_ga])
                rwt = c.sb("rwt", [128, 16, 64], BF16)
                b_rwt = Buf()
                c.dma("pool", rwt[:], rw.rearrange("(kc p) n -> p kc n", p=128), W=[b_rwt])
                rbr, b_rbr = load_const("rbr", rbias, [128, 64], F32)
                xts = [c.sb("xt2", [128, D], F32) for _ in range(2)]
                b_xts = [Buf(), Buf()]
                ats = [c.sb("at2", [128, D], F32) for _ in range(2)]
                b_ats = [Buf(), Buf()]
                ntmp = norm_tmp()
                sc = c.sb("sc", [128, 64], F32)
                sv = c.sb("sv", [128, 64], F32)
                mx8 = c.sb("mx8", [128, 8], F32)
                den = c.sb("den", [128, 1], F32)
                b_sc, b_sv, b_mx8, b_den = Buf(), Buf(), Buf(), Buf()
                c.op("pool", lambda e: e.memset(comb[:, :, 64:65], 1.0), [], [b_comb])
                for tt in range(8):
                    xt, b_xt = xts[tt % 2], b_xts[tt % 2]
                    at, b_at = ats[tt % 2], b_ats[tt % 2]
                    c.dma("sp", xt[:], xq[tt * 128:(tt + 1) * 128, :], W=[b_xt])
                    c.dma("sp", at[:], rs_out.ap()[tt * 128:(tt + 1) * 128, :], R=[b_rs], W=[b_at])
                    c.tt(at[:], at[:], ga[:], ALU.mult, [b_at, b_ga], [b_at])
                    c.tt(xt[:], xt[:], at[:], ALU.add, [b_xt, b_at], [b_xt])
                    c.dma("sp", x1_d[tt * 128:(tt + 1) * 128, :], xt[:], R=[b_xt], W=[b_x1d[tt]])
                    norm_tile(xt, b_xt, A2, B2, h2T, b_h2T, tt * 128, ntmp, ptr)
                    pp, b_pp = pd.get()
                    for kc in range(16):
                        c.mm(pp[:, 0:64], h2T[:, kc, tt * 128:(tt + 1) * 128], rwt[:, kc, :], [b_h2T, b_rwt], [b_pp],
                             start=(kc == 0), stop=(kc == 15), inc=(kc == 15))
                    c.act(sc[:], pp[:, 0:64], AF.Sigmoid, [b_pp], [b_sc])
                    c.tt(sv[:], sc[:], rbr[:], ALU.add, [b_sc, b_rbr], [b_sv])
                    c.op("dve", lambda e: e.max(out=mx8[:], in_=sv[:]), [b_sv], [b_mx8])
                    c.ts(sv[:], sv[:], mx8[:, 7:8], None, ALU.is_ge, None, [b_sv, b_mx8], [b_sv])
                    c.tt(sv[:], sv[:], sc[:], ALU.mult, [b_sv, b_sc], [b_sv])
                    c.op("dve", lambda e: e.reduce_sum(out=den[:], in_=sv[:], axis=mybir.AxisListType.X), [b_sv], [b_den])
                    c.op("dve", lambda e: e.reciprocal(out=den[:], in_=den[:]), [b_den], [b_den])
                    c.ts(comb[:, tt, 0:64], sv[:], den[:, 0:1], 2.5, ALU.mult, ALU.mult, [b_sv, b_den, b_comb], [b_comb])
            c.barrier()
            pd.slots.append(ptr0)
            es3 = ExitStack()
            es3.__enter__()
            c.es = es3
            wgu = [[c.sb("wg", [128, 16, 128], BF16), c.sb("wu", [128, 16, 128], BF16)] for _ in range(4)]
            b_wgu = [[Buf(), Buf()] for _ in range(4)]
            wdt = [c.sb("wd", [128, 4, D], BF16) for _ in range(2)]
            b_wdt = [Buf(), Buf()]
            actT = [c.sb("actT", [128, 4, 1024], BF16) for _ in range(2)]
            b_actT = [Buf(), Buf()]
            sgt = [c.sb("sgt", [128, 512], BF16) for _ in range(2)]
            b_sgt = [Buf(), Buf()]
            ui = 0
            for e_ in list(range(n_exp)) + [64]:
                if e_ < 64:
                    sg_, su_, sd_ = ewg[e_], ewu[e_], ewd[e_]
                else:
                    sg_, su_, sd_ = swg, swu, swd
                wd_t, b_wd = wdt[e_ % 2], b_wdt[e_ % 2]
                for half in range(2):
                    c.dma("pool", wd_t[:, half * 2:(half + 1) * 2, :], sd_[half * 256:(half + 1) * 256, :].rearrange("(a p) n -> p a n", p=128), W=[b_wd])
                aT, b_aT = actT[e_ % 2], b_actT[e_ % 2]
                for fc in range(4):
                    (wg_t, wu_t), (b_wg, b_wu) = wgu[ui % 4], b_wgu[ui % 4]
                    ui += 1
                    c.dma("pool", wg_t[:], sg_[:, fc * 128:(fc + 1) * 128].rearrange("(kc p) n -> p kc n", p=128), W=[b_wg])
                    c.dma("pool", wu_t[:], su_[:, fc * 128:(fc + 1) * 128].rearrange("(kc p) n -> p kc n", p=128), W=[b_wu])
                    for tb in range(2):
                        pG, b_pG = pgu.get()
                        pU, b_pU = pgu.get()
                        for kc in range(16):
                            c.mm(pG, wg_t[:, kc, :], h2T[:, kc, tb * 512:(tb + 1) * 512], [b_wg, b_h2T], [b_pG], start=(kc == 0), stop=(kc == 15), inc=(kc == 15))
                        for kc in range(16):
                            c.mm(pU, wu_t[:, kc, :], h2T[:, kc, tb * 512:(tb + 1) * 512], [b_wu, b_h2T], [b_pU], start=(kc == 0), stop=(kc == 15), inc=(kc == 15))
                        k2 = (fc * 2 + tb) % 2
                        c.act(sgt[k2][:], pG, AF.Silu, [b_pG], [b_sgt[k2]])
                        c.tt(aT[:, fc, tb * 512:(tb + 1) * 512], sgt[k2][:], pU, ALU.mult, [b_sgt[k2], b_pU], [b_aT])
                for tt in range(8):
                    for cbk in range(4):
                        pD, b_pD = pd.get()
                        for fc in range(4):
                            c.mm(pD, aT[:, fc, tt * 128:(tt + 1) * 128], wd_t[:, fc, cbk * 512:(cbk + 1) * 512], [b_aT, b_wd], [b_pD],
                                 start=(fc == 0), stop=(fc == 3), inc=(fc == 3))
                        dst = acc[:, tt, cbk * 512:(cbk + 1) * 512]
                        if e_ == (0 if n_exp else 64):
                            c.ts(dst, pD, comb[:, tt, e_:e_ + 1], None, ALU.mult, None, [b_pD, b_comb], [b_acc[tt]])
                        else:
                            c.stt(dst, pD, comb[:, tt, e_:e_ + 1], dst, ALU.mult, ALU.add, [b_pD, b_comb, b_acc[tt]], [b_acc[tt]])
            es3.__exit__(None, None, None)
            c.barrier()
            c.es = es
            gm = c.sb("gm", [128, D], F32)
            b_gm = Buf()
            c.dma("sp", gm[:], modD[80:96, :].rearrange("a b -> (a b)").partition_broadcast(128), R=[b_modD], W=[b_gm])
            nf = c.sb("nf", [128, D], F32)
            b_nf = Buf()
            c.dma("sp", nf[:], nrm_f.partition_broadcast(128), W=[b_nf])
            xts = [c.sb("xt3", [128, D], F32) for _ in range(2)]
            b_xts = [Buf(), Buf()]
            sq3 = c.sb("sq3", [128, D], BF16)
            ss3 = c.sb("ss3", [128, 1], F32)
            b_sq3, b_ss3 = Buf(), Buf()
            b_out = []
            for tt in range(8):
                xt, b_xt = xts[tt % 2], b_xts[tt % 2]
                c.dma("sp", xt[:], x1_d[tt * 128:(tt + 1) * 128, :], R=[b_x1d[tt]], W=[b_xt])
                z = acc[:, tt, :]
                c.tt(z, z, gm[:], ALU.mult, [b_acc[tt], b_gm], [b_acc[tt]])
                c.tt(z, z, xt[:], ALU.add, [b_acc[tt], b_xt], [b_acc[tt]])
                c.act(sq3[:], z, AF.Square, [b_acc[tt]], [b_sq3, b_ss3], accum_out=ss3[:])
                c.ts(ss3[:], ss3[:], 1.0 / D, EPS, ALU.mult, ALU.add, [b_ss3], [b_ss3])
                c.act(ss3[:], ss3[:], AF.Sqrt, [b_ss3], [b_ss3])
                c.op("dve", lambda e: e.reciprocal(out=ss3[:], in_=ss3[:]), [b_ss3], [b_ss3])
                c.stt(xt[:], z, ss3[:, 0:1], nf[:], ALU.mult, ALU.mult, [b_acc[tt], b_ss3, b_nf, b_xt], [b_xt])
                bo = Buf()
                b_out.append(bo)
                c.dma("sp", y[tt * 128:(tt + 1) * 128, :], xt[:], R=[b_xt], W=[bo])
            c.finish(b_out + dbg_bufs)
        c.barrier()
        c.es = es0
        return nc


_NC = None


def _consts():
    bf = ml_dtypes.bfloat16
    p = np.arange(128)
    k = {}
    k["k_identf"] = np.eye(128, dtype=np.float32)
    k["k_identb"] = np.eye(128).astype(bf)
    k["k_U"] = (p[:, None] <= p[None, :]).astype(np.float32)
    k["k_SL"] = (p[:, None] > p[None, :]).astype(np.float32)
    k["k_ILT"] = (p[None, :] >= p[:, None]).astype(np.float32)
    k["k_ones"] = np.ones((128, 128), np.float32)
    triU = np.where(p[:, None] <= p[None, :], 0.0, NEG).astype(np.float32)
    triL = np.where(p[:, None] > p[None, :], 0.0, NEG).astype(np.float32)
    k["k_triU"] = np.concatenate([triU, triU], axis=1).astype(bf)
    k["k_triL"] = np.concatenate([triL, triL], axis=1).astype(bf)
    E = np.zeros((64, 32, 128), np.float32)
    for kc in range(32):
        for pp in range(128):
            E[2 * kc + pp // 64, kc, pp] = 1.0
    k["k_E"] = E.astype(bf)
    ov = np.zeros((256, 65), np.float32)
    for m in range(1, 256):
        n = m - 1
        cs = n * 16
        for j in range(64):
            if cs < j * 64 + 64 and cs + 32 > j * 64:
                ov[m, j] = 1.0
        ov[m, 64] = 1.0
    k["k_ovl"] = np.ascontiguousarray(ov.reshape(2, 128, 65).transpose(1, 0, 2))
    cb0 = np.zeros((128, 17, 128), np.float32)
    col = np.arange(128)
    for d in range(16):
        valid = (16 * p[:, None] + 15) <= (128 * d + col[None, :])
        cb0[:, d, :] = np.where(valid, 0.0, NEG)
    k["k_cb0"] = cb0.astype(bf)
    fb = np.zeros((128, 32, 64), np.float32)
    for qb in range(32):
        for t in range(128):
            cur = 2 * qb + t // 64
            for j in range(64):
                if j == 0 or j == cur or j == cur - 1:
                    fb[t, qb, j] = 1.0e4
                elif j > cur:
                    fb[t, qb, j] = -1.0e4
    k["k_fb"] = fb.astype(bf)
    return k


def _prep(inp):
    f = lambda a: np.ascontiguousarray(np.asarray(a, dtype=np.float32))
    x = f(inp["x"]); cc = f(inp["c"])
    w_in = f(inp["w_in"])[0]
    conv = f(inp["gdn_conv_w"])[0]
    wo_full = f(inp["w_out"])[0]
    consts = _consts()
    shared = {
        "w_ada": f(inp["w_ada"])[0], "b_ada": f(inp["b_ada"])[0],
        "nrm_a": np.ascontiguousarray(f(inp["norm_attn_w"])[0].reshape(16, 128).T),
        "nrm_m": np.ascontiguousarray(f(inp["norm_ffn_w"])[0].reshape(16, 128).T),
        "nrm_f": f(inp["norm_final_w"]),
        "gnw": np.ascontiguousarray(np.broadcast_to(f(inp["gdn_norm_w"])[0][None, :], (128, 128))),
        "w1k": f(inp["cmp_w1_k"])[0], "w1v": f(inp["cmp_w1_v"])[0],
        "w2k": f(inp["cmp_w2_k"])[0], "w2v": f(inp["cmp_w2_v"])[0],
        "pek": np.ascontiguousarray(f(inp["cmp_pe_k"])[0].T), "pev": np.ascontiguousarray(f(inp["cmp_pe_v"])[0].T),
        "rw": f(inp["router_w"])[0],
        "rbias": np.ascontiguousarray(np.broadcast_to(f(inp["router_bias"])[0][None, :], (128, 64))),
        "ewg": f(inp["expert_w_gate"])[0], "ewu": f(inp["expert_w_up"])[0], "ewd": f(inp["expert_w_down"])[0],
        "swg": f(inp["shared_w_gate"])[0], "swu": f(inp["shared_w_up"])[0], "swd": f(inp["shared_w_down"])[0],
    }
    shared.update(consts)
    maps = []
    for core in range(8):
        b, hg = core // 4, core % 4
        kvh = hg // 2
        g = [2 * hg, 2 * hg + 1]
        m = dict(shared)
        m["xb"] = x[b]
        m["xq"] = np.ascontiguousarray(x[b, hg * 1024:(hg + 1) * 1024])
        m["cvec"] = np.ascontiguousarray(cc[b].reshape(16, 128).T)
        cols = []
        for part in range(3):
            for h in g:
                cols += list(range(part * 1024 + h * 128, part * 1024 + h * 128 + 128))
        m["wA_fm"] = np.ascontiguousarray(w_in[:, cols])
        m["convw"] = np.ascontiguousarray(conv[:, cols].reshape(4, 6, 128).transpose(2, 1, 0))
        tcols = []
        for h in g:
            tcols += list(range(3088 + h * 128, 3088 + h * 128 + 128))
        tcols += [3072 + g[0], 3072 + g[1], 3080 + g[0], 3080 + g[1]]
        m["wA_tm"] = np.ascontiguousarray(w_in[:, tcols])
        al = f(inp["gdn_a_log"])[0][g]; db = f(inp["gdn_dt_bias"])[0][g]
        m["alog"] = np.ascontiguousarray(np.broadcast_to(al[None, :], (128, 2)))
        m["dtb"] = np.ascontiguousarray(np.broadcast_to(db[None, :], (128, 2)))
        grp = [4 * kvh + i for i in range(4)]
        qh = g + [h for h in grp if h not in g]
        bcols = []
        for h in qh:
            bcols += list(range(4112 + h * 128, 4112 + h * 128 + 128))
        for part in (0, 1, 2, 4):
            bcols += list(range(5136 + part * 256 + kvh * 128, 5136 + part * 256 + kvh * 128 + 128))
        m["wB_fm"] = np.ascontiguousarray(w_in[:, bcols])
        btc = []
        for part in (3, 5):
            btc += list(range(5136 + part * 256 + kvh * 128, 5136 + part * 256 + kvh * 128 + 128))
        for br in range(3):
            for h in g:
                btc.append(6672 + br * 8 + h)
        m["wB_tm"] = np.ascontiguousarray(w_in[:, btc])
        rows = []
        for h in g:
            rows += list(range(h * 128, h * 128 + 128))
        for h in g:
            rows += list(range(1024 + h * 128, 1024 + h * 128 + 128))
        m["wout"] = np.ascontiguousarray(wo_full[rows, :])
        maps.append(m)
    return maps


def kernel(**inputs):
    global _NC
    maps = _prep(inputs)
    if _NC is None:
        _NC = build()
    res = run_bass_kernel_spmd(_NC, maps, core_ids=list(range(8)))
    out = np.zeros((2, T, D), np.float32)
    for core in range(8):
        b, q = core // 4, core % 4
        out[b, q * 1024:(q + 1) * 1024] = res.results[core]["y"]
    return out
```
